# Optimizing a Trainium2 kernel written in Bass

```python
import math
import jax, jax.numpy as jnp
from jax import lax
import numpy as np

D_MODEL = 1024
BATCH = 8
SEQ = 4096
DEPTH = 1

ATT_HEADS = 8
ATT_HEAD_DIM = 64
ATT_WIDTH = ATT_HEADS * ATT_HEAD_DIM
IDX_HEADS = 8
IDX_HEAD_DIM = 64
TOPK_MAX = 256
Q_BLOCK = 128
MLSTM_HEADS = 4
MLSTM_QK_DIM = 64
MLSTM_V_DIM = 128
MLSTM_WIDTH = MLSTM_HEADS * MLSTM_V_DIM
MLSTM_CHUNK = 64
CONV_WIDTH = 4
N_BUCKETS = 32
MAX_DISTANCE = 128
N_EXPERTS = 32
TOP_K = 4
D_FF = 1024
SWIGLU_ALPHA = 1.702
SWIGLU_LIMIT = 7.0
MOE_BLOCK = 256
LN_EPS = 1e-5
DEEPNORM_ALPHA = (2 * DEPTH) ** 0.25
DEEPNORM_BETA = (8 * DEPTH) ** -0.25

SPLIT_SIZES = (
    ATT_WIDTH, ATT_WIDTH, ATT_WIDTH,
    IDX_HEADS * IDX_HEAD_DIM, IDX_HEAD_DIM, IDX_HEADS,
    MLSTM_HEADS * MLSTM_QK_DIM, MLSTM_HEADS * MLSTM_QK_DIM, MLSTM_WIDTH,
    MLSTM_HEADS, MLSTM_HEADS, MLSTM_WIDTH,
    D_MODEL, D_MODEL,
)
IN_WIDTH = sum(SPLIT_SIZES)

kernel_name = "hybrid_dsa_mlstm_moe_deepnorm"


def layer_norm(x, g, b):
    xf = x.astype(jnp.float32)
    mu = jnp.mean(xf, axis=-1, keepdims=True)
    var = jnp.mean(jnp.square(xf - mu), axis=-1, keepdims=True)
    y = (xf - mu) * lax.rsqrt(var + LN_EPS)
    return (y * g.astype(jnp.float32) + b.astype(jnp.float32)).astype(x.dtype)


def t5_bucket(rel):
    n = jnp.maximum(rel, 0)
    max_exact = N_BUCKETS // 2
    n_f = jnp.maximum(n, 1).astype(jnp.float32)
    large = max_exact + (jnp.log(n_f / max_exact) / math.log(MAX_DISTANCE / max_exact)
                         * (N_BUCKETS - max_exact)).astype(jnp.int32)
    large = jnp.minimum(large, N_BUCKETS - 1)
    return jnp.where(n < max_exact, n, large)


def dsa_attention(q, k, v, q_idx, k_idx, w_idx, rel_bias):
    B, S, H, Dh = q.shape
    n_sel = min(TOPK_MAX, S // 4)
    n_blk = S // Q_BLOCK
    key_pos = jnp.arange(S, dtype=jnp.int32)

    def to_blocks(a):
        return jnp.moveaxis(a.reshape(B, n_blk, Q_BLOCK, *a.shape[2:]), 1, 0)

    def block(args):
        q_b, qi_b, wi_b, start = args
        t_pos = start + jnp.arange(Q_BLOCK, dtype=jnp.int32)
        dots = jnp.einsum('bqhd,bsd->bqhs', qi_b, k_idx).astype(jnp.float32) * IDX_HEAD_DIM ** -0.5
        score = jnp.einsum('bqh,bqhs->bqs', wi_b.astype(jnp.float32) * IDX_HEADS ** -0.5,
                           jax.nn.relu(dots))
        score = jnp.where(key_pos[None, None, :] <= t_pos[None, :, None], score, -jnp.inf)
        _, sel = lax.top_k(score, n_sel)
        k_g = jax.vmap(lambda kk, ii: kk[ii])(k, sel)
        v_g = jax.vmap(lambda vv, ii: vv[ii])(v, sel)
        rel = t_pos[None, :, None] - sel
        logits = jnp.einsum('bqhd,bqjhd->bqhj', q_b, k_g).astype(jnp.float32) * ATT_HEAD_DIM ** -0.5
        bias = jnp.moveaxis(rel_bias[:, t5_bucket(rel)], 0, 2).astype(jnp.float32)
        logits = jnp.where((rel >= 0)[:, :, None, :], logits + bias, -jnp.inf)
        p = jax.nn.softmax(logits, axis=-1).astype(v.dtype)
        return jnp.einsum('bqhj,bqjhd->bqhd', p, v_g)

    starts = jnp.arange(n_blk, dtype=jnp.int32) * Q_BLOCK
    out = lax.map(block, (to_blocks(q), to_blocks(q_idx), to_blocks(w_idx), starts))
    return jnp.moveaxis(out, 0, 1).reshape(B, S, H * Dh)


def causal_depthwise_conv(x, w, b):
    C = x.shape[-1]
    y = lax.conv_general_dilated(x, w[:, None, :].astype(x.dtype), window_strides=(1,),
                                 padding=[(CONV_WIDTH - 1, 0)],
                                 dimension_numbers=('NWC', 'WIO', 'NWC'),
                                 feature_group_count=C)
    return y + b


def mlstm_chunkwise(q, k, v, i_pre, f_pre):
    B, S, H, Dk = q.shape
    Dv = v.shape[-1]
    L = MLSTM_CHUNK
    NC = S // L
    q = q.reshape(B, NC, L, H, Dk)
    k = k.reshape(B, NC, L, H, Dk)
    v = v.reshape(B, NC, L, H, Dv)
    log_f = jax.nn.log_sigmoid(f_pre).reshape(B, NC, L, H)
    log_i = i_pre.reshape(B, NC, L, H)
    b = jnp.cumsum(log_f, axis=2)
    b_end = b[:, :, -1, :]
    a = b_end[:, :, None, :] - b + log_i
    m_loc = jnp.max(a, axis=2)
    w_loc = jnp.exp(a - m_loc[:, :, None, :])
    c_loc = jnp.einsum('bclh,bclhk,bclhv->bchkv', w_loc, k, v)
    n_loc = jnp.einsum('bclh,bclhk->bchk', w_loc, k)

    def step(carry, inp):
        c, n, m = carry
        cl, nl, ml, be = inp
        m_new = jnp.maximum(be + m, ml)
        decay = jnp.exp(be + m - m_new)
        inject = jnp.exp(ml - m_new)
        c_new = decay[..., None, None] * c + inject[..., None, None] * cl
        n_new = decay[..., None] * n + inject[..., None] * nl
        return (c_new, n_new, m_new), (c, n, m)

    init = (jnp.zeros((B, H, Dk, Dv), jnp.float32), jnp.zeros((B, H, Dk), jnp.float32),
            jnp.zeros((B, H), jnp.float32))
    xs = (jnp.moveaxis(c_loc, 1, 0), jnp.moveaxis(n_loc, 1, 0),
          jnp.moveaxis(m_loc, 1, 0), jnp.moveaxis(b_end, 1, 0))
    _, (c0, n0, m0) = lax.scan(step, init, xs)
    c0 = jnp.moveaxis(c0, 0, 1)
    n0 = jnp.moveaxis(n0, 0, 1)
    m0 = jnp.moveaxis(m0, 0, 1)

    d = b[:, :, :, None, :] - b[:, :, None, :, :] + log_i[:, :, None, :, :]
    causal = jnp.tril(jnp.ones((L, L), dtype=bool))
    d = jnp.where(causal[None, None, :, :, None], d, -jnp.inf)
    m_inter = b + m0[:, :, None, :]
    m_t = jnp.maximum(m_inter, jnp.max(d, axis=3))
    p = jnp.einsum('bcthk,bcshk->bctsh', q, k) * jnp.exp(d - m_t[:, :, :, None, :])
    scale_inter = jnp.exp(m_inter - m_t)
    num = (jnp.einsum('bctsh,bcshv->bcthv', p, v)
           + scale_inter[..., None] * jnp.einsum('bcthk,bchkv->bcthv', q, c0))
    den = jnp.sum(p, axis=3) + scale_inter * jnp.einsum('bcthk,bchk->bcth', q, n0)
    h = num / jnp.maximum(jnp.abs(den), jnp.exp(-m_t))[..., None]
    return h.reshape(B, S, H, Dv)


def token_mixers(x, w_in, conv_w, conv_b, i_bias, f_bias, norm_g,
                 w_branch_attn, w_branch_mlstm, w_out, rel_bias):
    B, S, _ = x.shape
    proj = x @ w_in
    split_points = np.cumsum(SPLIT_SIZES)[:-1].tolist()
    (q_a, k_a, v_a, q_i, k_i, w_i, q_m, k_m, v_m, i_m, f_m, o_m, g_a, g_m) = jnp.split(
        proj, split_points, axis=-1)

    y_att = dsa_attention(q_a.reshape(B, S, ATT_HEADS, ATT_HEAD_DIM),
                          k_a.reshape(B, S, ATT_HEADS, ATT_HEAD_DIM),
                          v_a.reshape(B, S, ATT_HEADS, ATT_HEAD_DIM),
                          q_i.reshape(B, S, IDX_HEADS, IDX_HEAD_DIM), k_i, w_i, rel_bias)

    qk_m = jax.nn.silu(causal_depthwise_conv(jnp.concatenate([q_m, k_m], axis=-1), conv_w, conv_b))
    q_m, k_m = jnp.split(qk_m, 2, axis=-1)
    h_m = mlstm_chunkwise(
        q_m.reshape(B, S, MLSTM_HEADS, MLSTM_QK_DIM).astype(jnp.float32),
        k_m.reshape(B, S, MLSTM_HEADS, MLSTM_QK_DIM).astype(jnp.float32) * MLSTM_QK_DIM ** -0.5,
        v_m.reshape(B, S, MLSTM_HEADS, MLSTM_V_DIM).astype(jnp.float32),
        (i_m + i_bias).astype(jnp.float32),
        (f_m + f_bias).astype(jnp.float32))
    mu = jnp.mean(h_m, axis=-1, keepdims=True)
    var = jnp.mean(jnp.square(h_m - mu), axis=-1, keepdims=True)
    h_m = (h_m - mu) * lax.rsqrt(var + LN_EPS) * norm_g.reshape(MLSTM_HEADS, MLSTM_V_DIM).astype(jnp.float32)
    h_m = h_m * jax.nn.sigmoid(o_m.reshape(B, S, MLSTM_HEADS, MLSTM_V_DIM).astype(jnp.float32))
    y_m = h_m.reshape(B, S, MLSTM_WIDTH).astype(x.dtype)

    mix = jax.nn.sigmoid(g_a) * (y_att @ w_branch_attn) + jax.nn.sigmoid(g_m) * (y_m @ w_branch_mlstm)
    return mix @ w_out


def moe_ffn(h, w_router, b_router, w_gate_up, b_gate_up, w_down, b_down):
    B, S, D = h.shape
    T = B * S
    xt = h.reshape(T, D)
    logits = (xt @ w_router + b_router).astype(jnp.float32)
    top_vals, top_idx = lax.top_k(logits, TOP_K)
    gates = jax.nn.softmax(top_vals, axis=-1)
    P = T * TOP_K
    e_flat = top_idx.reshape(P)
    tok_flat = jnp.arange(P, dtype=jnp.int32) // TOP_K
    g_flat = gates.reshape(P)
    order = jnp.argsort(e_flat)
    e_sorted = e_flat[order]
    counts = jnp.bincount(e_flat, length=N_EXPERTS)
    start = jnp.cumsum(counts) - counts
    padded = ((counts + MOE_BLOCK - 1) // MOE_BLOCK) * MOE_BLOCK
    pend = jnp.cumsum(padded)
    pstart = pend - padded
    dest = pstart[e_sorted] + (jnp.arange(P, dtype=jnp.int32) - start[e_sorted])
    cap = ((P + MOE_BLOCK - 1) // MOE_BLOCK) * MOE_BLOCK + N_EXPERTS * MOE_BLOCK
    n_blocks = cap // MOE_BLOCK
    buf_tok = jnp.zeros((cap,), jnp.int32).at[dest].set(tok_flat[order])
    buf_gate = jnp.zeros((cap,), jnp.float32).at[dest].set(g_flat[order])
    block_expert = jnp.searchsorted(pend, jnp.arange(n_blocks, dtype=jnp.int32) * MOE_BLOCK, side='right')
    block_expert = jnp.minimum(block_expert, N_EXPERTS - 1)

    def expert_block(args):
        tok, e = args
        xb = xt[tok]
        gu = xb @ w_gate_up[e] + b_gate_up[e]
        gate, up = gu[:, :D_FF], gu[:, D_FF:]
        gate = jnp.minimum(gate, SWIGLU_LIMIT)
        up = jnp.clip(up, -SWIGLU_LIMIT, SWIGLU_LIMIT)
        act = (up + 1) * (gate * jax.nn.sigmoid(SWIGLU_ALPHA * gate))
        return act @ w_down[e] + b_down[e]

    y_blocks = lax.map(expert_block, (buf_tok.reshape(n_blocks, MOE_BLOCK), block_expert))
    y_buf = y_blocks.reshape(cap, D) * buf_gate[:, None].astype(h.dtype)
    y = jnp.zeros((T, D), h.dtype).at[buf_tok].add(y_buf)
    return y.reshape(B, S, D)


def setup_inputs(seed: int = 0) -> dict:
    key = jax.random.key(seed)
    ks = jax.random.split(key, 21)
    nrm = jax.random.normal
    qk_cols = 2 * MLSTM_HEADS * MLSTM_QK_DIM
    return {
        "x": nrm(ks[0], (BATCH, SEQ, D_MODEL), jnp.float32),
        "w_in": nrm(ks[1], (DEPTH, D_MODEL, IN_WIDTH), jnp.float32) * D_MODEL ** -0.5,
        "conv_w": nrm(ks[2], (DEPTH, CONV_WIDTH, qk_cols), jnp.float32) * CONV_WIDTH ** -0.5,
        "conv_b": 0.02 * nrm(ks[3], (DEPTH, qk_cols), jnp.float32),
        "mlstm_i_bias": -2.0 + 0.1 * nrm(ks[4], (DEPTH, MLSTM_HEADS), jnp.float32),
        "mlstm_f_bias": jnp.linspace(3.0, 6.0, MLSTM_HEADS, dtype=jnp.float32)
                        + 0.1 * nrm(ks[5], (DEPTH, MLSTM_HEADS), jnp.float32),
        "mlstm_norm_g": 1.0 + 0.02 * nrm(ks[6], (DEPTH, MLSTM_WIDTH), jnp.float32),
        "w_branch_attn": nrm(ks[7], (DEPTH, ATT_WIDTH, D_MODEL), jnp.float32) * ATT_WIDTH ** -0.5,
        "w_branch_mlstm": nrm(ks[8], (DEPTH, MLSTM_WIDTH, D_MODEL), jnp.float32) * MLSTM_WIDTH ** -0.5,
        "w_out": nrm(ks[9], (DEPTH, D_MODEL, D_MODEL), jnp.float32) * (D_MODEL ** -0.5 * DEEPNORM_BETA),
        "ln1_g": 1.0 + 0.02 * nrm(ks[10], (DEPTH, D_MODEL), jnp.float32),
        "ln1_b": 0.02 * nrm(ks[11], (DEPTH, D_MODEL), jnp.float32),
        "w_router": nrm(ks[12], (DEPTH, D_MODEL, N_EXPERTS), jnp.float32) * D_MODEL ** -0.5,
        "b_router": 0.01 * nrm(ks[13], (DEPTH, N_EXPERTS), jnp.float32),
        "w_gate_up": nrm(ks[14], (DEPTH, N_EXPERTS, D_MODEL, 2 * D_FF), jnp.float32) * D_MODEL ** -0.5,
        "b_gate_up": 0.02 * nrm(ks[15], (DEPTH, N_EXPERTS, 2 * D_FF), jnp.float32),
        "w_down": nrm(ks[16], (DEPTH, N_EXPERTS, D_FF, D_MODEL), jnp.float32) * (D_FF ** -0.5 * DEEPNORM_BETA),
        "b_down": 0.02 * nrm(ks[17], (DEPTH, N_EXPERTS, D_MODEL), jnp.float32),
        "ln2_g": 1.0 + 0.02 * nrm(ks[18], (DEPTH, D_MODEL), jnp.float32),
        "ln2_b": 0.02 * nrm(ks[19], (DEPTH, D_MODEL), jnp.float32),
        "rel_bias": 0.5 * nrm(ks[20], (ATT_HEADS, N_BUCKETS), jnp.float32),
    }


def reference(x, w_in, conv_w, conv_b, mlstm_i_bias, mlstm_f_bias, mlstm_norm_g,
              w_branch_attn, w_branch_mlstm, w_out, ln1_g, ln1_b, w_router, b_router,
              w_gate_up, b_gate_up, w_down, b_down, ln2_g, ln2_b, rel_bias):
    for l in range(DEPTH):
        y = token_mixers(x, w_in[l], conv_w[l], conv_b[l], mlstm_i_bias[l], mlstm_f_bias[l],
                         mlstm_norm_g[l], w_branch_attn[l], w_branch_mlstm[l], w_out[l], rel_bias)
        x = layer_norm(DEEPNORM_ALPHA * x + y, ln1_g[l], ln1_b[l])
        y = moe_ffn(x, w_router[l], b_router[l], w_gate_up[l], b_gate_up[l], w_down[l], b_down[l])
        x = layer_norm(DEEPNORM_ALPHA * x + y, ln2_g[l], ln2_b[l])
    return x
```

```python
import numpy as np
import ml_dtypes
from contextlib import ExitStack
import concourse.bass as bass
import concourse.mybir as mybir
from concourse.bass_utils import run_bass_kernel_spmd

F32 = mybir.dt.float32
BF16 = mybir.dt.bfloat16
I32 = mybir.dt.int32
AF = mybir.ActivationFunctionType
ALU = mybir.AluOpType
AX = mybir.AxisListType

NCORES = 8
T = 4096
NT = 32
DM = 1024
E = 32
CAP = 224
SGM = (128, CAP - 128)
NG = 4
ALPHA = 2.0 ** 0.25
LN_EPS = 1e-5


DBG = {}


class Tok:
    __slots__ = ("w", "r")

    def __init__(self):
        self.w = None
        self.r = {}


class K:
    NDS = 24

    def __init__(self, nc, es):
        self.nc = nc
        self.es = es
        self.eng = {"pe": nc.tensor, "act": nc.scalar, "dve": nc.vector, "pool": nc.gpsimd, "sp": nc.sync}
        self.semh = {}
        for n in self.eng:
            self.semh[n] = es.enter_context(nc.semaphore("s_" + n))
        for i in range(self.NDS):
            self.semh[("d", i)] = es.enter_context(nc.semaphore("d%d" % i))
        self.cnt = {k: 0 for k in self.semh}
        self.seen = {n: {} for n in self.eng}
        self.dnext = 0
        self.dq = {}
        self.nins = 0
        self.pe_last = None

    def _deps(self, reads, writes):
        evs = {}
        for t in reads:
            if t.w is not None and evs.get(t.w[0], 0) < t.w[1]:
                evs[t.w[0]] = t.w[1]
        for t in writes:
            if t.w is not None and evs.get(t.w[0], 0) < t.w[1]:
                evs[t.w[0]] = t.w[1]
            for kk, v in t.r.items():
                if evs.get(kk, 0) < v:
                    evs[kk] = v
        return evs

    def _resolve_pe(self):
        if self.pe_last is not None:
            self.pe_last.then_inc(self.semh["pe"], 1)
            self.cnt["pe"] += 1
            self.pe_last = None

    def _wait(self, en, evs, skip_self=False):
        seen = self.seen[en]
        for kk, v in evs.items():
            if skip_self and kk == en:
                continue
            if seen.get(kk, 0) >= v:
                continue
            if kk == "pe" and v > self.cnt["pe"]:
                self._resolve_pe()
            self.eng[en].wait_ge(self.semh[kk], v)
            seen[kk] = v

    def _record(self, ev, reads, writes):
        kk, v = ev
        for t in reads:
            if t.r.get(kk, 0) < v:
                t.r[kk] = v
        for t in writes:
            t.w = ev
            t.r = {}

    def op(self, en, fn, reads=(), writes=(), self_ok=False):
        evs = self._deps(reads, writes)
        self._wait(en, evs, skip_self=(en == "pe" or self_ok))
        ins = fn(self.eng[en])
        if en == "pe" and not DBG.get("eager_pe"):
            self.pe_last = ins
            self._record((en, self.cnt[en] + 1), reads, writes)
        else:
            self.cnt[en] += 1
            ins.then_inc(self.semh[en], 1)
            self._record((en, self.cnt[en]), reads, writes)
        self.nins += 1

    def dma(self, q, fn, reads=(), writes=()):
        if q == "pool" and DBG.get("simsem"):
            key = ("x", len(self.semh))
            self.semh[key] = self.es.enter_context(self.nc.semaphore("x%d" % len(self.semh)))
            self.cnt[key] = 0
        else:
            lo_, hi_ = (0, 8) if q == "pool" else (8, self.NDS)
            i = self.dq.get(q, lo_)
            self.dq[q] = lo_ + (i + 1 - lo_) % (hi_ - lo_)
            key = ("d", i)
        evs = self._deps(reads, writes)
        if self.cnt[key] > 0 and evs.get(key, 0) < self.cnt[key]:
            evs[key] = self.cnt[key]
        self._wait(q, evs)
        ins = fn(self.eng[q])
        self.cnt[key] += 16
        ins.then_inc(self.semh[key], 16)
        self._record((key, self.cnt[key]), reads, writes)
        self.nins += 1

    def barrier(self):
        self._resolve_pe()
        snap = dict(self.cnt)
        for en in self.eng:
            self._wait(en, {kk: v for kk, v in snap.items() if v > 0 and kk != en})

    def finish(self, q="sp"):
        self._resolve_pe()
        for key in list(self.semh):
            if isinstance(key, tuple) and self.cnt[key] > 0:
                self.eng[q].wait_ge(self.semh[key], self.cnt[key])
        for n in ("pe", "act", "dve", "pool"):
            if self.cnt[n] > 0:
                self.eng[q].wait_ge(self.semh[n], self.cnt[n])


def dram_bcast(handle, offset, n, parts=128):
    return bass.AP(handle, offset, [[0, parts], [1, n]])


def moe_phase(nc, k, D):
    x1 = D["x1"]
    out = D["out"]
    yscr = D["yscr"]
    C = D["consts"]
    tk = {}

    def tok(name):
        if name not in tk:
            tk[name] = Tok()
        return tk[name]

    es = ExitStack()
    with es:
        def sbp(name, shape, dt):
            return es.enter_context(nc.sbuf_tensor(name, shape, dt))

        x1b = sbp("x1b", [128, NT, DM], BF16)
        maskf = sbp("maskf", [128, NT, E], F32)
        gates = sbp("gates", [128, NT, E], F32)
        pos = sbp("pos", [128, NT, E], F32)
        ghl = sbp("ghl", [128, NT, E, 2], BF16)
        idx4 = sbp("idx4", [128, NT, 4], I32)
        ident = sbp("identf", [128, 128], F32)
        iot = sbp("iot", [128, CAP], F32)
        bgu = sbp("bgu", [128, E, 16], F32)
        tc_ = tok("const")
        k.dma("sp", lambda e: e.dma_start(out=ident[:], in_=C["identf"]), writes=[tc_])
        k.dma("sp", lambda e: e.dma_start(out=iot[:], in_=C["iota"]), writes=[tc_])
        k.dma("sp", lambda e: e.dma_start(out=bgu[:], in_=D["b_gate_up"]), writes=[tc_])
        x1v = x1.rearrange("(j p) d -> p j d", p=128)
        for q4 in range(4):
            k.dma("pool", lambda e: e.dma_start(out=x1b[:, q4 * 8:(q4 + 1) * 8, :], in_=x1v[:, q4 * 8:(q4 + 1) * 8, :]),
                  reads=[tok("x1dram")], writes=[tok("x1b%d" % q4)])

        with ExitStack() as es2:
            def sb(name, shape, dt):
                return es2.enter_context(nc.sbuf_tensor("r_" + name, shape, dt))

            def ps(name):
                return es2.enter_context(nc.psum_tensor("r_" + name, [128, 512], F32))
            wr = sb("wr", [128, 8, E], F32)
            brt = sb("brt", [128, E], F32)
            logits = sb("logits", [128, NT, E], F32)
            maskb = sb("maskb", [128, NT, E], BF16)
            rid = sb("rid", [128, NT, E], F32)
            top8 = sb("top8", [128, NT, 8], F32)
            top8r = sb("top8r", [128, NT, 8], F32)
            sm = sb("sm", [128, NT, 4], F32)
            tri = sb("tri", [128, 128], BF16)
            ones = sb("onesb", [128, 128], BF16)
            rowbase = sb("rowbase", [128, NT, E], F32)
            xt = [sb("xt%d" % i, [128, DM], F32) for i in range(2)]
            xT = [sb("xT%d" % i, [128, 8 * 128], F32) for i in range(2)]
            psA = [ps("psA%d" % i) for i in range(2)]
            psP = ps("psP")
            psS = ps("psS")
            k.dma("sp", lambda e: e.dma_start(out=rowbase[:], in_=C["rowbase"]), writes=[tc_])
            k.dma("sp", lambda e: e.dma_start(out=tri[:], in_=C["tri"]), writes=[tc_])
            k.dma("sp", lambda e: e.dma_start(out=ones[:], in_=C["ones"]), writes=[tc_])
            k.dma("sp", lambda e: e.dma_start(out=wr[:], in_=D["w_router"]), writes=[tc_])
            k.dma("sp", lambda e: e.dma_start(out=brt[:], in_=D["b_router"]), writes=[tc_])

            lgt = [tok("lg%d" % j) for j in range(NT)]
            for j in range(NT):
                b = j % 2
                txt, txT = tok("xt%d" % b), tok("xT%d" % b)
                k.dma("sp", lambda e: e.dma_start(out=xt[b][:], in_=x1[j * 128:(j + 1) * 128, :]),
                      reads=[tok("x1dram")], writes=[txt])
                for c in range(8):
                    k.op("pe", lambda e: e.transpose(psA[c // 4][:, (c % 4) * 128:(c % 4 + 1) * 128],
                                                     xt[b][:, c * 128:(c + 1) * 128], ident[:]),
                         reads=[txt, tc_], writes=[tok("psA%d" % (c // 4))])
                k.op("act", lambda e: e.copy(out=xT[b][:, 0:512], in_=psA[0][:]), reads=[tok("psA0")], writes=[txT])
                k.op("act", lambda e: e.copy(out=xT[b][:, 512:1024], in_=psA[1][:]), reads=[tok("psA1")], writes=[txT])
                pp = [psP, psS][j % 2]
                for c in range(8):
                    k.op("pe", lambda e: e.matmul(pp[:, 0:E], xT[b][:, c * 128:(c + 1) * 128], wr[:, c, :],
                                                  start=(c == 0), stop=(c == 7)),
                         reads=[txT, tc_], writes=[tok(pp.name)])
                k.op("dve", lambda e: e.tensor_tensor(out=logits[:, j, :], in0=pp[:, 0:E], in1=brt[:], op=ALU.add),
                     reads=[tok(pp.name), tc_], writes=[lgt[j]])
                k.op("dve", lambda e: e.max(out=top8[:, j, :], in_=logits[:, j, :]), reads=[lgt[j]], writes=[lgt[j]])
            rt = tok("rtall")
            for j in range(NT):
                tk["rt%d" % j] = rt

            def fl(t_):
                return t_[:].rearrange("p a b -> p (a b)")
            k.op("dve", lambda e: e.tensor_tensor(out=maskf[:], in0=logits[:], in1=bc_in(top8[:, :, 3], E), op=ALU.is_ge),
                 reads=lgt, writes=[rt])
            k.op("dve", lambda e: e.tensor_tensor(out=gates[:], in0=logits[:], in1=bc_in(top8[:, :, 0], E), op=ALU.subtract),
                 reads=lgt, writes=[rt])
            k.op("act", lambda e: e.activation(out=fl(gates), in_=fl(gates), func=AF.Exp), reads=[rt], writes=[rt])
            k.op("dve", lambda e: e.tensor_tensor(out=fl(gates), in0=fl(gates), in1=fl(maskf), op=ALU.mult), reads=[rt], writes=[rt])
            k.op("dve", lambda e: e.tensor_reduce(out=sm[:, :, 1], in_=gates[:], axis=AX.X, op=ALU.add), reads=[rt], writes=[rt])
            k.op("dve", lambda e: e.reciprocal(out=sm[:, :, 2], in_=sm[:, :, 1]), reads=[rt], writes=[rt])
            k.op("dve", lambda e: e.tensor_tensor(out=gates[:], in0=gates[:], in1=bc_in(sm[:, :, 2], E), op=ALU.mult), reads=[rt], writes=[rt])
            k.op("dve", lambda e: e.tensor_copy(out=ghl[:, :, :, 0], in_=gates[:]), reads=[rt], writes=[rt])
            k.op("dve", lambda e: e.tensor_tensor(out=ghl[:, :, :, 1], in0=gates[:], in1=ghl[:, :, :, 0], op=ALU.subtract), reads=[rt], writes=[rt])
            k.op("dve", lambda e: e.tensor_copy(out=fl(maskb), in_=fl(maskf)), reads=[rt], writes=[rt])

            pst = [tok("pos%d" % j) for j in range(NT)]
            for j in range(NT):
                g = j // 8
                pp = [psP, psS][j % 2]
                first = True
                for i in range(g * 8, j):
                    k.op("pe", lambda e: e.matmul(pp[:, 0:E], ones[:], maskb[:, i, :], start=first, stop=False),
                         reads=[rt, tc_], writes=[tok(pp.name)])
                    first = False
                k.op("pe", lambda e: e.matmul(pp[:, 0:E], tri[:], maskb[:, j, :], start=first, stop=True),
                     reads=[rt, tc_], writes=[tok(pp.name)])
                k.op("act", lambda e: e.copy(out=pos[:, j, :], in_=pp[:, 0:E]), reads=[tok(pp.name)], writes=[pst[j]])
            k.op("dve", lambda e: e.tensor_tensor(out=fl(rid), in0=fl(pos), in1=fl(rowbase), op=ALU.add), reads=pst + [tc_], writes=[rt])
            k.op("dve", lambda e: e.tensor_tensor(out=fl(rid), in0=fl(rid), in1=fl(maskf), op=ALU.mult), reads=[rt], writes=[rt])
            rdt = [tok("rid%d" % j) for j in range(NT)]
            for j in range(NT):
                k.op("dve", lambda e: e.max(out=top8r[:, j, :], in_=rid[:, j, :]), reads=[rt], writes=[rdt[j]])
            k.op("dve", lambda e: e.tensor_scalar(out=idx4[:], in0=top8r[:, :, 0:4], scalar1=-1.0, scalar2=None, op0=ALU.add),
                 reads=rdt + [rt], writes=[rt])
            k.barrier()

        with ExitStack() as es2:
            def sb(name, shape, dt):
                return es2.enter_context(nc.sbuf_tensor("x_" + name, shape, dt))

            def ps(name):
                return es2.enter_context(nc.psum_tensor("x_" + name, [128, 512], F32))
            wgu = [sb("wgu%d" % i, [128, 8, 2048], BF16) for i in range(2)]
            wd = [sb("wd%d" % i, [128, 8, DM], BF16) for i in range(2)]
            sel = [sb("sel%d" % i, [128, 8, CAP], BF16) for i in range(2)]
            xeT = [sb("xeT%d" % i, [128, 8 * CAP], BF16) for i in range(2)]
            aT = sb("aT", [128, 8 * CAP], BF16)
            gc = [sb("gc%d" % i, [128, CAP], F32) for i in range(1)]
            sg_ = [sb("sg%d" % i, [128, CAP], F32) for i in range(1)]
            uc = [sb("uc%d" % i, [128, CAP], F32) for i in range(1)]
            gsl = [sb("gsl%d" % i, [128, 2], F32) for i in range(2)]
            ysb = [sb("ysb%d" % i, [128, 512], F32) for i in range(2)]
            psA = [ps("psA%d" % i) for i in range(2)]
            psU = [ps("psU%d" % i) for i in range(2)]
            psD = [ps("psD%d" % i) for i in range(2)]
            psS = ps("psS")
            for n in ("psA0", "psA1", "psS", "psP"):
                tk.pop(n, None)

            def load_wgu(e_, c0=0, c1=8):
                s = e_ % 2
                src_ = D["w_gate_up"][e_].rearrange("(c p) f -> p c f", p=128)
                k.dma("pool", lambda e: e.dma_start(out=wgu[s][:, c0:c1, :], in_=src_[:, c0:c1, :]),
                      writes=[tok("wgu%d" % s)])

            def load_wd(e_, c0=0, c1=8):
                s = e_ % 2
                src_ = D["w_down"][e_].rearrange("(c p) f -> p c f", p=128)
                k.dma("pool", lambda e: e.dma_start(out=wd[s][:, c0:c1, :], in_=src_[:, c0:c1, :]),
                      writes=[tok("wd%d" % s)])

            def build_sel(e_, g, sb_):
                for jj in range(8):
                    j = g * 8 + jj
                    k.op("dve", lambda e: e.tensor_scalar(out=sel[sb_][:, jj, :], in0=iot[:], scalar1=pos[:, j, e_:e_ + 1],
                                                          scalar2=maskf[:, j, e_:e_ + 1], op0=ALU.is_equal, op1=ALU.mult),
                         reads=[tok("rt%d" % j), tc_], writes=[tok("sel%d" % sb_)])

            items = [(e_, g) for e_ in range(E) for g in range(NG)]

            def gather(it):
                e_, g = items[it]
                sb_ = it % 2
                tsel, txe = tok("sel%d" % sb_), tok("xeT%d" % sb_)
                for c2 in range(4):
                    pb = c2 % 2
                    for h in range(2):
                        c = 2 * c2 + h
                        for jj in range(8):
                            j = g * 8 + jj
                            k.op("pe", lambda e: e.matmul(psA[pb][:, h * CAP:(h + 1) * CAP],
                                                          x1b[:, j, c * 128:(c + 1) * 128], sel[sb_][:, jj, :],
                                                          start=(jj == 0), stop=(jj == 7)),
                                 reads=[tok("x1b%d" % g), tsel], writes=[tok("psA%d" % pb)])
                    if c2 % 2 == 0:
                        k.op("act", lambda e: e.copy(out=xeT[sb_][:, c2 * 2 * CAP:(c2 + 1) * 2 * CAP], in_=psA[pb][:, 0:2 * CAP]),
                             reads=[tok("psA%d" % pb)], writes=[txe])
                    else:
                        k.op("dve", lambda e: e.tensor_copy(out=xeT[sb_][:, c2 * 2 * CAP:(c2 + 1) * 2 * CAP], in_=psA[pb][:, 0:2 * CAP]),
                             reads=[tok("psA%d" % pb)], writes=[txe])
                for sg in range(2):
                    for jj in range(8):
                        j = g * 8 + jj
                        k.op("pe", lambda e: e.matmul(psS[0:SGM[sg], sb_ * 4 + sg * 2:sb_ * 4 + sg * 2 + 2], sel[sb_][:, jj, sg * 128:sg * 128 + SGM[sg]],
                                                      ghl[:, j, e_, :], start=(jj == 0), stop=(jj == 7)),
                             reads=[tsel, tok("rt%d" % j)], writes=[tok("psS")])
                tgs = tok("gsl%d" % sb_)
                for sg in range(2):
                    k.op("dve", lambda e: e.tensor_reduce(out=gsl[sb_][0:SGM[sg], sg:sg + 1], in_=psS[0:SGM[sg], sb_ * 4 + sg * 2:sb_ * 4 + sg * 2 + 2],
                                                          axis=AX.X, op=ALU.add),
                         reads=[tok("psS")], writes=[tgs])

            def gate_up(it):
                e_, g = items[it]
                s = e_ % 2
                sb_ = it % 2
                twgu, txe, tat = tok("wgu%d" % s), tok("xeT%d" % sb_), tok("aT")
                for kk in range(8):
                    pb = kk % 2
                    tb = 0
                    for half in range(2):
                        for c in range(8):
                            k.op("pe", lambda e: e.matmul(psU[pb][:, half * CAP:(half + 1) * CAP],
                                                          wgu[s][:, c, half * 1024 + kk * 128: half * 1024 + (kk + 1) * 128],
                                                          xeT[sb_][:, c * CAP:(c + 1) * CAP], start=(c == 0), stop=(c == 7)),
                                 reads=[twgu, txe], writes=[tok("psU%d" % pb)])
                    tg, ts_, tu = tok("gc%d" % tb), tok("sg%d" % tb), tok("uc%d" % tb)
                    k.op("dve", lambda e: e.tensor_scalar(out=gc[tb][:], in0=psU[pb][:, 0:CAP], scalar1=bgu[:, e_, kk:kk + 1],
                                                          scalar2=7.0, op0=ALU.add, op1=ALU.min),
                         reads=[tok("psU%d" % pb), tc_], writes=[tg])
                    k.op("act", lambda e: e.activation(out=sg_[tb][:], in_=gc[tb][:], func=AF.Sigmoid, scale=1.702),
                         reads=[tg], writes=[ts_])
                    k.op("act", lambda e: e.activation(out=uc[tb][:], in_=psU[pb][:, CAP:2 * CAP], func=AF.Identity,
                                                       bias=bgu[:, e_, 8 + kk:9 + kk], scale=1.0),
                         reads=[tok("psU%d" % pb), tc_], writes=[tu])
                    k.op("dve", lambda e: e.tensor_scalar(out=uc[tb][:], in0=uc[tb][:], scalar1=7.0, scalar2=-7.0,
                                                          op0=ALU.min, op1=ALU.max), reads=[tu], writes=[tu])
                    k.op("dve", lambda e: e.tensor_tensor(out=sg_[tb][:], in0=gc[tb][:], in1=sg_[tb][:], op=ALU.mult),
                         reads=[tg, ts_], writes=[ts_])
                    k.op("dve", lambda e: e.scalar_tensor_tensor(out=aT[:, kk * CAP:(kk + 1) * CAP], in0=uc[tb][:],
                                                                 scalar=1.0, in1=sg_[tb][:], op0=ALU.add, op1=ALU.mult),
                         reads=[tu, ts_], writes=[tat])

            def down(it):
                e_, g = items[it]
                s = e_ % 2
                sb_ = it % 2
                twd, tat, tgs = tok("wd%d" % s), tok("aT"), tok("gsl%d" % sb_)
                for sg in range(2):
                    for half in range(2):
                        pb = half
                        tys = tok("ysb%d" % half)
                        for kk in range(8):
                            k.op("pe", lambda e: e.matmul(psD[pb][0:SGM[sg], :], aT[:, kk * CAP + sg * 128: kk * CAP + sg * 128 + SGM[sg]],
                                                          wd[s][:, kk, half * 512:(half + 1) * 512], start=(kk == 0), stop=(kk == 7)),
                                 reads=[tat, twd], writes=[tok("psD%d" % pb)])
                        k.op("act", lambda e: e.activation(out=ysb[half][0:SGM[sg], :], in_=psD[pb][0:SGM[sg], :],
                                                           func=AF.Copy, scale=gsl[sb_][0:SGM[sg], sg:sg + 1]),
                             reads=[tok("psD%d" % pb), tgs], writes=[tys])
                        row0 = e_ * (NG * CAP) + g * CAP + sg * 128
                        k.dma("sp", lambda e: e.dma_start(out=yscr[row0:row0 + SGM[sg], half * 512:(half + 1) * 512], in_=ysb[half][0:SGM[sg], :]),
                              reads=[tys], writes=[Tok()])

            for e0 in range(2):
                load_wgu(e0)
                load_wd(e0)
            build_sel(0, 0, 0)
            gather(0)
            for it, (e_, g) in enumerate(items):
                if it + 1 < len(items):
                    build_sel(items[it + 1][0], items[it + 1][1], (it + 1) % 2)
                gate_up(it)
                if e_ >= 1 and e_ + 1 < E:
                    load_wgu(e_ + 1, 2 * g, 2 * g + 2)
                if it + 1 < len(items):
                    gather(it + 1)
                down(it)
                if e_ >= 1 and e_ + 1 < E:
                    load_wd(e_ + 1, 2 * g, 2 * g + 2)
            k.barrier()

        with ExitStack() as es2:
            def sb(name, shape, dt):
                return es2.enter_context(nc.sbuf_tensor("c_" + name, shape, dt))

            def ps(name):
                return es2.enter_context(nc.psum_tensor("c_" + name, [128, 512], F32))
            for n in list(tk):
                if n.startswith("ps"):
                    tk.pop(n)
            lng = sb("lng", [128, DM], F32)
            lnb = sb("lnb", [128, DM], F32)
            bdn = sb("bdn", [E, DM], F32)
            xt = [sb("xt%d" % i, [128, DM], F32) for i in range(2)]
            acc = [sb("acc%d" % i, [128, DM], F32) for i in range(2)]
            gtt = [sb("gtt%d" % i, [128, 4, DM], F32) for i in range(2)]
            gT = [sb("gT%d" % i, [E, 128], F32) for i in range(2)]
            sq = sb("sq", [128, DM], F32)
            st = sb("st", [128, NT, 8], F32)
            psT = ps("psT")
            psB = [ps("psB%d" % i) for i in range(2)]
            tk.pop("xt0", None)
            tk.pop("xt1", None)
            k.dma("sp", lambda e: e.dma_start(out=lng[:], in_=D["ln2_g"]), writes=[tc_])
            k.dma("sp", lambda e: e.dma_start(out=lnb[:], in_=D["ln2_b"]), writes=[tc_])
            k.dma("sp", lambda e: e.dma_start(out=bdn[:], in_=D["b_down"]), writes=[tc_])
            nrows = E * NG * CAP
            def A(j):
                b = j % 2
                tgq, txt, tac = [tok("gtt%d_%d" % (b, q_)) for q_ in range(4)], tok("xt%d" % b), tok("acc%d" % b)
                trt = tok("rt%d" % j)
                for q in range(4):
                    k.dma("pool", lambda e: e.indirect_dma_start(
                        out=gtt[b][:, q, :], out_offset=None, in_=yscr[:, :],
                        in_offset=bass.IndirectOffsetOnAxis(ap=idx4[:, j, q:q + 1], axis=0)),
                        reads=[trt, tok("yscr")], writes=[tgq[q]])
                k.dma("sp", lambda e: e.dma_start(out=xt[b][:], in_=x1[j * 128:(j + 1) * 128, :]),
                      reads=[tok("x1dram")], writes=[txt])
                tgT = tok("gT%d" % b)
                k.op("pe", lambda e: e.transpose(psT[0:E, 0:128], gates[:, j, :], ident[:]), reads=[trt, tc_], writes=[tok("psT")])
                k.op("act", lambda e: e.copy(out=gT[b][:], in_=psT[0:E, 0:128]), reads=[tok("psT")], writes=[tgT])
                for half in range(2):
                    k.op("pe", lambda e: e.matmul(psB[half][:], gT[b][:], bdn[:, half * 512:(half + 1) * 512], start=True, stop=True),
                         reads=[tgT, tc_], writes=[tok("psB%d" % half)])
                yield
                k.op("dve", lambda e: e.scalar_tensor_tensor(out=acc[b][:], in0=xt[b][:], scalar=float(ALPHA), in1=gtt[b][:, 0, :],
                                                             op0=ALU.mult, op1=ALU.add), reads=[txt, tgq[0]], writes=[tac])
                k.op("dve", lambda e: e.tensor_tensor(out=acc[b][:], in0=acc[b][:], in1=gtt[b][:, 1, :], op=ALU.add),
                     reads=[tgq[1], tac], writes=[tac])
                yield
                for q in range(2, 4):
                    k.op("dve", lambda e: e.tensor_tensor(out=acc[b][:], in0=acc[b][:], in1=gtt[b][:, q, :], op=ALU.add),
                         reads=[tgq[q], tac], writes=[tac])
                yield
                for half in range(2):
                    k.op("dve", lambda e: e.tensor_tensor(out=acc[b][:, half * 512:(half + 1) * 512],
                                                          in0=acc[b][:, half * 512:(half + 1) * 512], in1=psB[half][:], op=ALU.add),
                         reads=[tok("psB%d" % half), tac], writes=[tac])
                yield

            def L(j):
                b = j % 2
                a_, ta, tst = acc[b], tok("acc%d" % b), tok("st%d" % j)
                k.op("dve", lambda e: e.reduce_sum(out=st[:, j, 0:1], in_=a_[:], axis=AX.X), reads=[ta], writes=[tst])
                k.op("dve", lambda e: e.tensor_scalar(out=st[:, j, 1:2], in0=st[:, j, 0:1], scalar1=-1.0 / DM, scalar2=None,
                                                      op0=ALU.mult), reads=[tst], writes=[tst])
                k.op("act", lambda e: e.activation(out=a_[:], in_=a_[:], func=AF.Identity, bias=st[:, j, 1:2], scale=1.0),
                     reads=[tst, ta], writes=[ta])
                k.op("act", lambda e: e.activation(out=sq[:], in_=a_[:], func=AF.Square), reads=[ta], writes=[tok("sq")])
                yield
                k.op("dve", lambda e: e.reduce_sum(out=st[:, j, 2:3], in_=sq[:], axis=AX.X), reads=[tok("sq")], writes=[tst])
                k.op("dve", lambda e: e.tensor_scalar(out=st[:, j, 3:4], in0=st[:, j, 2:3], scalar1=1.0 / DM, scalar2=LN_EPS,
                                                      op0=ALU.mult, op1=ALU.add), reads=[tst], writes=[tst])
                k.op("act", lambda e: e.activation(out=st[:, j, 4:5], in_=st[:, j, 3:4], func=AF.Sqrt), reads=[tst], writes=[tst])
                yield
                k.op("dve", lambda e: e.reciprocal(out=st[:, j, 5:6], in_=st[:, j, 4:5]), reads=[tst], writes=[tst])
                k.op("dve", lambda e: e.scalar_tensor_tensor(out=a_[:], in0=a_[:], scalar=st[:, j, 5:6], in1=lng[:],
                                                             op0=ALU.mult, op1=ALU.mult), reads=[tst, ta, tc_], writes=[ta])
                k.op("dve", lambda e: e.tensor_tensor(out=a_[:], in0=a_[:], in1=lnb[:], op=ALU.add), reads=[ta, tc_], writes=[ta])
                k.dma("sp", lambda e: e.dma_start(out=out[j * 128:(j + 1) * 128, :], in_=a_[:]),
                      reads=[ta], writes=[Tok()])
                yield

            for _ in A(0):
                pass
            for j in range(NT):
                gl = L(j)
                ga = A(j + 1) if j + 1 < NT else None
                while gl is not None or ga is not None:
                    if ga is not None and next(ga, "done") == "done":
                        ga = None
                    if gl is not None and next(gl, "done") == "done":
                        gl = None
            k.barrier()


def layer_norm_tile(k, a, ta, sq, tsq, st, j, tst, g_bc, b_bc, tc_):
    k.op("dve", lambda e: e.reduce_sum(out=st[:, j, 0:1], in_=a[:], axis=AX.X), reads=[ta], writes=[tst])
    k.op("dve", lambda e: e.tensor_scalar(out=st[:, j, 1:2], in0=st[:, j, 0:1], scalar1=-1.0 / DM, scalar2=None,
                                          op0=ALU.mult), reads=[tst], writes=[tst])
    k.op("act", lambda e: e.activation(out=a[:], in_=a[:], func=AF.Identity, bias=st[:, j, 1:2], scale=1.0),
         reads=[tst, ta], writes=[ta])
    k.op("act", lambda e: e.activation(out=sq[:], in_=a[:], func=AF.Square), reads=[ta], writes=[tsq])
    k.op("dve", lambda e: e.reduce_sum(out=st[:, j, 2:3], in_=sq[:], axis=AX.X), reads=[tsq], writes=[tst])
    k.op("dve", lambda e: e.tensor_scalar(out=st[:, j, 3:4], in0=st[:, j, 2:3], scalar1=1.0 / DM, scalar2=LN_EPS,
                                          op0=ALU.mult, op1=ALU.add), reads=[tst], writes=[tst])
    k.op("act", lambda e: e.activation(out=st[:, j, 4:5], in_=st[:, j, 3:4], func=AF.Sqrt), reads=[tst], writes=[tst])
    k.op("dve", lambda e: e.reciprocal(out=st[:, j, 5:6], in_=st[:, j, 4:5]), reads=[tst], writes=[tst])
    k.op("dve", lambda e: e.scalar_tensor_tensor(out=a[:], in0=a[:], scalar=st[:, j, 5:6], in1=g_bc[:],
                                                 op0=ALU.mult, op1=ALU.mult), reads=[tst, ta, tc_], writes=[ta])
    k.op("dve", lambda e: e.tensor_tensor(out=a[:], in0=a[:], in1=b_bc[:], op=ALU.add), reads=[ta, tc_], writes=[ta])


NIT = 20
A_QI, A_KI, A_VA, A_WI, A_COLS = 1024, 1536, 1664, 2176, 2184
B_GA, B_GM, B_VM, B_O, B_IF, B_COLS = 512, 1536, 2560, 3072, 3584, 3592


def bc_mid(x, n):
    return bass.AP(x.tensor, x.offset, [list(x.ap[0]), [0, n], list(x.ap[1])])


def bc_in(x, n):
    return bass.AP(x.tensor, x.offset, [list(x.ap[0]), list(x.ap[1]), [0, n]])


def mixer_phase(nc, k, D, dbg=None):
    C = D["consts"]
    tk = {}

    def tok(name):
        if name not in tk:
            tk[name] = Tok()
        return tk[name]

    tc_ = tok("const")
    xTd = D["xT"].rearrange("(c p) t -> p c t", p=128)
    with ExitStack() as es:
        identb = es.enter_context(nc.sbuf_tensor("identb", [128, 128], BF16))
        causT = es.enter_context(nc.sbuf_tensor("causT", [128, 128], BF16))
        k.dma("sp", lambda e: e.dma_start(out=identb[:], in_=C["identb"]), writes=[tc_])
        k.dma("sp", lambda e: e.dma_start(out=causT[:], in_=C["causT"]), writes=[tc_])

        with ExitStack() as es2:
            def sb(name, shape, dt):
                return es2.enter_context(nc.sbuf_tensor("a_" + name, shape, dt))

            def ps(name, dt=F32):
                return es2.enter_context(nc.psum_tensor("a_" + name, [128, 512] if dt == F32 else [128, 1024], dt))
            winA = sb("winA", [128, 8, A_COLS], BF16)
            kaT = sb("kaT", [128, 4, T], BF16)
            vaug = sb("vaug", [128, NT, 8, 66], BF16)
            kiT2 = sb("kiT2", [128, T], BF16)
            scores = [sb("scores%d" % i, [128, T], F32) for i in range(2)]
            mbuf = sb("mbuf", [128, T], BF16)
            maskT = sb("maskT", [128, NT, 128], BF16)
            onesb = sb("onesb", [128, 128], BF16)
            negmask = sb("negmask", [128, 128], F32)
            ebx = [sb("ebx%d" % i, [128, 8, 128], BF16) for i in range(2)]
            ebf = sb("ebf", [128, 8, 128], F32)
            cfx = sb("cfx", [128, 8], F32)
            ftab = sb("ftab", [128, NIT + 1], F32)
            xTa = [sb("xTa%d" % i, [128, 8, 128], BF16) for i in range(2)]
            qaT = [sb("qaT%d" % i, [128, 8, 128], BF16) for i in range(3)]
            qiT = sb("qiT", [128, 8, 128], BF16)
            wi = sb("wi", [128, 8], F32)
            diagw = sb("diagw", [128, 8, 128], BF16)
            R = [sb("R%d" % i, [128, 512], BF16) for i in range(2)]
            Eb = [sb("Eb%d" % i, [128, 8, 128], BF16) for i in range(2)]
            PT = [sb("PT%d" % i, [128, 8, 128], BF16) for i in range(2)]
            tmpE = sb("tmpE", [128, 8, 128], BF16)
            oc = sb("oc", [128, 8, 65], F32)
            bs = sb("bs", [128, 8], F32)
            stp = sb("stp", [128, NIT + 1], F32)
            dummy = sb("dummy", [128, 16], F32)
            self_ctr = [0]
            rden = sb("rden", [128, 8], F32)
            yat = [sb("yat%d" % i, [128, 512], BF16) for i in range(2)]
            psOf = [ps("psOf%d" % i) for i in range(2)]
            psOn = [ps("psOn%d" % i) for i in range(2)]
            Rk = [ps("K%d" % i) for i in range(3)]
            psT = ps("psT", BF16)
            if DBG.get("verbose"):
                print("sweepA sbuf remaining", nc.sbuf_bytes_remaining)

            for cb_, (c0, c1) in enumerate(((0, A_VA), (A_VA, A_COLS))):
                k.dma("pool", lambda e: e.dma_start(out=winA[:, :, c0:c1],
                                                    in_=D["winA"].rearrange("(c p) f -> p c f", p=128)[:, :, c0:c1]),
                      writes=[tok("winA")])
            k.dma("sp", lambda e: e.dma_start(out=onesb[:], in_=C["ones"]), writes=[tc_])
            k.dma("sp", lambda e: e.dma_start(out=negmask[:], in_=C["negmask"]), writes=[tc_])
            k.dma("sp", lambda e: e.dma_start(out=ftab[:], in_=C["ftab"]), writes=[tc_])
            k.dma("sp", lambda e: e.dma_start(out=cfx[:], in_=D["cfar"]), writes=[tc_])
            k.op("act", lambda e: e.activation(out=cfx[:], in_=cfx[:], func=AF.Exp), reads=[tc_], writes=[tc_])
            for i, nm in enumerate(("ebd", "ebp")):
                k.dma("sp", lambda e: e.dma_start(out=ebf[:], in_=D[nm]), writes=[tok("ebf")])
                k.op("act", lambda e: e.activation(out=ebx[i][:], in_=ebf[:], func=AF.Exp), reads=[tok("ebf")], writes=[tc_])
            for i in range(3):
                k.op("dve", lambda e: e.memset(qaT[i][:], 0.0), writes=[tok("qaT%d" % i)])
            k.op("dve", lambda e: e.memset(qiT[:], 0.0), writes=[tok("qiT")])
            tmk = tok("maskT")

            def S1(j):
                b = j % 2
                n = (j + 1) * 128
                txa = tok("xTa%d" % b)
                q3 = j % 3
                k.dma("pool", lambda e: e.dma_start(out=xTa[b][:], in_=xTd[:, :, j * 128:(j + 1) * 128]), writes=[txa])

                def fm_group(bank, chunk0, nch):
                    bv = bank[:, :].rearrange("p (a b) -> p a b", a=4)
                    for cc in range(nch):
                        for c in range(8):
                            k.op("pe", lambda e: e.matmul(bv[:, cc, :], winA[:, c, (chunk0 + cc) * 128:(chunk0 + cc + 1) * 128],
                                                          xTa[b][:, c, :], start=(c == 0), stop=(c == 7)),
                                 reads=[tok("winA"), txa], writes=[tok(bank.name)])
                    return bv
                bv = fm_group(Rk[0], 0, 4)
                k.op("act", lambda e: e.copy(out=qaT[q3][0:64, 0:8:2, :], in_=bv[0:64, :, :]), reads=[tok(Rk[0].name)], writes=[tok("qaT%d" % q3)])
                k.op("act", lambda e: e.copy(out=qaT[q3][64:128, 1:8:2, :], in_=bv[64:128, :, :]), reads=[tok(Rk[0].name)], writes=[tok("qaT%d" % q3)])
                bv = fm_group(Rk[1], 4, 4)
                k.op("act", lambda e: e.copy(out=kaT[:, :, j * 128:(j + 1) * 128], in_=bv[:, :, :]),
                     reads=[tok(Rk[1].name)], writes=[tok("kaT%d" % j)])
                bv = fm_group(Rk[2], 8, 4)
                k.op("act", lambda e: e.copy(out=qiT[0:64, 0:8:2, :], in_=bv[0:64, :, :]), reads=[tok(Rk[2].name)], writes=[tok("qiT")])
                k.op("act", lambda e: e.copy(out=qiT[64:128, 1:8:2, :], in_=bv[64:128, :, :]), reads=[tok(Rk[2].name)], writes=[tok("qiT")])
                bv = fm_group(Rk[0], 12, 1)
                k.op("act", lambda e: e.copy(out=kiT2[:, j * 128:(j + 1) * 128], in_=bv[:, 0, :]),
                     reads=[tok(Rk[0].name)], writes=[tok("kiT%d" % j)])
                for c in range(8):
                    k.op("pe", lambda e: e.matmul(Rk[1][:, :], xTa[b][:, c, :], winA[:, c, A_VA:A_WI], start=(c == 0), stop=(c == 7)),
                         reads=[tok("winA"), txa], writes=[tok(Rk[1].name)])
                k.op("pool", lambda e: e.memset(vaug[:, j, :, 64:66], 1.0), writes=[tok("vaug%d" % j)])
                k.op("act", lambda e: e.copy(out=vaug[:, j, :, 0:64], in_=Rk[1][:, :].rearrange("p (a b) -> p a b", a=8)),
                     reads=[tok(Rk[1].name)], writes=[tok("vaug%d" % j)])
                if j < 2:
                    return
                for c in range(8):
                    k.op("pe", lambda e: e.matmul(Rk[2][:, 0:8], xTa[b][:, c, :], winA[:, c, A_WI:A_COLS], start=(c == 0), stop=(c == 7)),
                         reads=[tok("winA"), txa], writes=[tok(Rk[2].name)])
                k.op("act", lambda e: e.activation(out=wi[:], in_=Rk[2][:, 0:8], func=AF.Copy, scale=float(0.125 * 8 ** -0.5)),
                     reads=[tok(Rk[2].name)], writes=[tok("wi")])
                for h in range(8):
                    k.op("act", lambda e: e.activation(out=diagw[:, h, :], in_=identb[:], func=AF.Copy, scale=wi[:, h:h + 1]),
                         reads=[tok("wi"), tc_], writes=[tok("diagw")])
                tsc = tok("scores%d" % b)
                for blk in range((n + 511) // 512):
                    ncl = min(512, n - blk * 512)
                    kread = [tok("kiT%d" % t_) for t_ in range(blk * 4, min(blk * 4 + 4, j + 1))]
                    def mm2(h):
                        hb = h % 2
                        k.op("pe", lambda e: e.matmul(Rk[2][:, 0:ncl], diagw[:, h, :], R[hb][:, 0:ncl], start=(h == 0), stop=(h == 7)),
                             reads=[tok("diagw"), tok("R%d" % hb)], writes=[tok(Rk[2].name)])
                    for h in range(8):
                        hb = h % 2
                        k.op("pe", lambda e: e.matmul(Rk[hb][:, 0:ncl], qiT[:, h, :],
                                                      kiT2[:, blk * 512:blk * 512 + ncl], start=True, stop=True),
                             reads=[tok("qiT")] + kread, writes=[tok(Rk[hb].name)])
                        k.op("act", lambda e: e.activation(out=R[hb][:, 0:ncl], in_=Rk[hb][:, 0:ncl], func=AF.Relu),
                             reads=[tok(Rk[hb].name)], writes=[tok("R%d" % hb)])
                        if h >= 1:
                            mm2(h - 1)
                    mm2(7)
                    k.op("act", lambda e: e.copy(out=scores[b][:, blk * 512:blk * 512 + ncl], in_=Rk[2][:, 0:ncl]),
                         reads=[tok(Rk[2].name)], writes=[tsc])

            def S2a(j):
                b = j % 2
                n = (j + 1) * 128
                sc = scores[b]
                tsc, tb_ = tok("scores%d" % b), tok("bs")
                k.op("dve", lambda e: e.tensor_reduce(out=bs[:, 0:1], in_=sc[:, 0:n], axis=AX.X, op=ALU.max), reads=[tsc], writes=[tb_])
                k.op("dve", lambda e: e.tensor_reduce(out=bs[:, 1:2], in_=sc[:, 0:256], axis=AX.X, op=ALU.min), reads=[tsc], writes=[tb_])
                k.op("dve", lambda e: e.tensor_tensor(out=bs[:, 2:3], in0=bs[:, 0:1], in1=bs[:, 1:2], op=ALU.subtract), reads=[tb_], writes=[tb_])
                k.op("dve", lambda e: e.tensor_tensor(out=sc[:, j * 128:(j + 1) * 128], in0=sc[:, j * 128:(j + 1) * 128],
                                                      in1=negmask[:], op=ALU.add), reads=[tsc, tc_], writes=[tsc])
                k.op("dve", lambda e: e.tensor_scalar(out=stp[:], in0=ftab[:], scalar1=bs[:, 2:3], scalar2=None, op0=ALU.mult),
                     reads=[tb_, tc_], writes=[tb_])
                k.op("dve", lambda e: e.tensor_tensor(out=bs[:, 3:4], in0=bs[:, 1:2], in1=stp[:, 0:1], op=ALU.add), reads=[tb_], writes=[tb_])
                yield
                nf = DBG.get("fillers", 0)
                so = nf > 0

                def fill():
                    for f_ in range(nf):
                        self_ctr[0] += 1
                        c_ = self_ctr[0] % 16
                        k.op("dve", lambda e: e.memset(dummy[:, c_:c_ + 1], 0.0), self_ok=True)
                for it in range(1, NIT + 1):
                    k.op("dve", lambda e: e.tensor_scalar(out=mbuf[:, 0:n], in0=sc[:, 0:n], scalar1=bs[:, 3:4], scalar2=0.0,
                                                          op0=ALU.is_ge, op1=ALU.add, accum_out=bs[:, 4:5]),
                         reads=[tsc, tb_], writes=[tok("mbuf"), tb_], self_ok=(so and it > 1))
                    fill()
                    k.op("dve", lambda e: e.tensor_scalar(out=bs[:, 5:6], in0=bs[:, 4:5], scalar1=256.0, scalar2=-0.5,
                                                          op0=ALU.is_ge, op1=ALU.add), reads=[tb_], writes=[tb_], self_ok=so)
                    fill()
                    k.op("dve", lambda e: e.scalar_tensor_tensor(out=bs[:, 3:4], in0=stp[:, it:it + 1], scalar=bs[:, 5:6], in1=bs[:, 3:4],
                                                                 op0=ALU.mult, op1=ALU.add), reads=[tb_], writes=[tb_], self_ok=so)
                    fill()
                    yield
                k.op("dve", lambda e: e.scalar_tensor_tensor(out=bs[:, 3:4], in0=stp[:, NIT:NIT + 1], scalar=-0.5, in1=bs[:, 3:4],
                                                             op0=ALU.mult, op1=ALU.add), reads=[tb_], writes=[tb_])
                k.op("dve", lambda e: e.tensor_scalar(out=mbuf[:, 0:n], in0=sc[:, 0:n], scalar1=bs[:, 3:4], scalar2=None,
                                                      op0=ALU.is_ge), reads=[tsc, tb_], writes=[tok("mbuf")])

            def S2b(j):
                for i0 in range(0, j + 1, 8):
                    cnt = min(8, j + 1 - i0)
                    pv = psT[:, :].rearrange("p (a b) -> p a b", a=8)
                    for i in range(i0, i0 + cnt):
                        k.op("pe", lambda e: e.transpose(pv[:, i - i0, :], mbuf[:, i * 128:(i + 1) * 128], identb[:]),
                             reads=[tok("mbuf"), tc_], writes=[tok("psT")])
                    k.op("act", lambda e: e.copy(out=maskT[:, i0:i0 + cnt, :], in_=pv[:, 0:cnt, :]), reads=[tok("psT")], writes=[tmk])

            def S3(j):
                q3 = j % 3

                def qk(i):
                    eb = i % 2
                    for hg in range(2):
                        qb = Rk[(2 * i + hg) % 3]
                        lv = qb[:, :].rearrange("p (a b) -> p a b", a=4)
                        for h4 in range(4):
                            h = hg * 4 + h4
                            k.op("pe", lambda e: e.matmul(lv[:, h4, :], kaT[:, h // 2, i * 128:(i + 1) * 128],
                                                          qaT[q3][:, h, :], start=True, stop=True),
                                 reads=[tok("kaT%d" % i), tok("qaT%d" % q3)], writes=[tok(qb.name)])
                        k.op("act", lambda e: e.activation(out=Eb[eb][:, hg * 4:(hg + 1) * 4, :], in_=lv[:, :, :], func=AF.Exp, scale=0.125),
                             reads=[tok(qb.name)], writes=[tok("Eb%d" % eb)])

                def pv(i):
                    eb = i % 2
                    far = (i <= j - 2)
                    if j >= 2:
                        m_ap, mreads = maskT[:, i, :], [tmk]
                    elif i == j:
                        m_ap, mreads = causT[:], [tc_]
                    else:
                        m_ap, mreads = onesb[:], [tc_]
                    tpt = tok("PT%d" % eb)
                    if far:
                        k.op("dve", lambda e: e.tensor_tensor(out=PT[eb][:], in0=Eb[eb][:], in1=bc_mid(m_ap, 8), op=ALU.mult),
                             reads=[tok("Eb%d" % eb)] + mreads, writes=[tpt])
                    else:
                        kind = 0 if i == j else 1
                        k.op("pool", lambda e: e.tensor_tensor(out=tmpE[:], in0=ebx[kind][:], in1=bc_mid(m_ap, 8), op=ALU.mult),
                             reads=[tc_] + mreads, writes=[tok("tmpE")])
                        k.op("dve", lambda e: e.tensor_tensor(out=PT[eb][:], in0=Eb[eb][:], in1=tmpE[:], op=ALU.mult),
                             reads=[tok("Eb%d" % eb), tok("tmpE")], writes=[tpt])
                    for h in range(8):
                        if far:
                            bank, st_, sp_ = psOf[h // 4], (i == 0 and h % 4 == 0), (i == j - 2 and h % 4 == 3)
                        else:
                            bank, st_, sp_ = psOn[h // 4], (i == max(j - 1, 0) and h % 4 == 0), (i == j and h % 4 == 3)
                        ov = bank[:, 0:260].rearrange("p (a b) -> p a b", a=4)
                        k.op("pe", lambda e: e.matmul(ov[:, h % 4, :], PT[eb][:, h, :], vaug[:, i, h, 0:65], start=st_, stop=sp_),
                             reads=[tpt, tok("vaug%d" % i)], writes=[tok(bank.name)])

                qk(0)
                for i in range(j + 1):
                    if i + 1 <= j:
                        qk(i + 1)
                    pv(i)
                    yield
                toc = tok("oc")
                for hg in range(2):
                    nv = psOn[hg][:, 0:260].rearrange("p (a b) -> p a b", a=4)
                    if j >= 2:
                        fv = psOf[hg][:, 0:260].rearrange("p (a b) -> p a b", a=4)
                        k.op("dve", lambda e: e.tensor_tensor(out=oc[:, hg * 4:(hg + 1) * 4, :], in0=fv[:, :, :],
                                                              in1=bc_in(cfx[:, hg * 4:(hg + 1) * 4], 65), op=ALU.mult),
                             reads=[tok(psOf[hg].name), tc_], writes=[toc])
                        k.op("dve", lambda e: e.tensor_tensor(out=oc[:, hg * 4:(hg + 1) * 4, :], in0=oc[:, hg * 4:(hg + 1) * 4, :],
                                                              in1=nv[:, :, :], op=ALU.add), reads=[tok(psOn[hg].name), toc], writes=[toc])
                    else:
                        k.op("dve", lambda e: e.tensor_copy(out=oc[:, hg * 4:(hg + 1) * 4, :], in_=nv[:, :, :]),
                             reads=[tok(psOn[hg].name)], writes=[toc])
                k.op("dve", lambda e: e.reciprocal(out=rden[:], in_=oc[:, :, 64]), reads=[toc], writes=[tok("rden")])
                yb_ = j % 2
                k.op("dve", lambda e: e.tensor_tensor(out=yat[yb_][:].rearrange("p (a b) -> p a b", a=8), in0=oc[:, :, 0:64],
                                                      in1=bc_in(rden[:], 64), op=ALU.mult),
                     reads=[toc, tok("rden")], writes=[tok("yat%d" % yb_)])
                k.dma("sp", lambda e: e.dma_start(out=D["yatt_scr"][j * 128:(j + 1) * 128, :], in_=yat[yb_][:]),
                      reads=[tok("yat%d" % yb_)], writes=[Tok()])

            def drain(g):
                if g is not None:
                    for _ in g:
                        pass

            ntA = DBG.get("ntA", NT)
            for t in range(-2, ntA):
                ga = S2a(t + 1) if 2 <= t + 1 < ntA else None
                gb = S3(t) if t >= 0 else None
                if ga is not None and gb is not None:
                    ppi = max(1, -(-(t + 1) // 5))
                    next(ga)
                    for it in range(NIT):
                        next(ga)
                        for _ in range(ppi):
                            if next(gb, "done") == "done":
                                gb = None
                                break
                        if gb is None:
                            break
                    drain(ga)
                    drain(gb)
                else:
                    drain(ga)
                    drain(gb)
                if t + 2 < ntA:
                    S1(t + 2)
                if 2 <= t + 1 < ntA:
                    S2b(t + 1)
            k.barrier()

        if DBG.get("noB"):
            return
        with ExitStack() as es2:
            def sb(name, shape, dt):
                return es2.enter_context(nc.sbuf_tensor("b_" + name, shape, dt))

            def ps(name, dt=F32):
                return es2.enter_context(nc.psum_tensor("b_" + name, [128, 512] if dt == F32 else [128, 1024], dt))
            for nme in list(tk):
                if nme.startswith("a_"):
                    tk.pop(nme)
            winB = sb("winB", [128, 8, B_COLS], BF16)
            wba = sb("wba", [128, 4, DM], BF16)
            wbm = sb("wbm", [128, 4, DM], BF16)
            wout = sb("wout", [128, 8, DM], BF16)
            cw = sb("cw", [128, 4, 4], F32)
            cbias = sb("cb", [128, 4], F32)
            ifb = sb("ifb", [128, 8], F32)
            ngb = sb("ngb", [128, 512], F32)
            l1g = sb("l1g", [128, DM], F32)
            l1b = sb("l1b", [128, DM], F32)
            triI = sb("triI", [128, 128], F32)
            onesf = sb("onesf", [128, 128], F32)
            xTa = [sb("xTa%d" % i, [128, 8, 128], BF16) for i in range(2)]
            xt = [sb("xt%d" % i, [128, DM], F32) for i in range(2)]
            qkraw = sb("qkraw", [128, 4, 131], F32)
            cacc = sb("cacc", [128, 4, 128], F32)
            qkm = sb("qkm", [128, 4, 128], BF16)
            qz = sb("qz", [128, 4, 128], BF16)
            sga = sb("sga", [128, 8, 128], F32)
            sgm = sb("sgm", [128, 8, 128], F32)
            vm = sb("vm", [128, 4, 130], BF16)
            so = sb("so", [128, 512], F32)
            gat = sb("gat", [128, 32], F32)
            PTm = sb("PTm", [128, 4, 128], BF16)
            kp2 = sb("kp2", [128, 2, 128], BF16)
            Cst = sb("Cst", [128, 2, 129], F32)
            Cb = sb("Cb", [128, 2, 130], BF16)
            ctmp = sb("ctmp", [128, 129], F32)
            hs = sb("hs", [128, 24], F32)
            hv = sb("hv", [128, 4, 128], F32)
            sq = sb("sq", [128, DM], F32)
            ym = sb("ym", [128, 512], BF16)
            yaT = sb("yaT", [128, 4, 128], BF16)
            ymT = sb("ymT", [128, 4, 128], BF16)
            t1 = sb("t1", [128, 512], F32)
            t2 = sb("t2", [128, 512], F32)
            mixT = sb("mixT", [128, 8, 128], BF16)
            acc = [sb("acc%d" % i, [128, DM], F32) for i in range(2)]
            yab = [sb("yab%d" % i, [128, 512], BF16) for i in range(2)]
            st = sb("st", [128, NT, 8], F32)
            Rb = [ps("K%d" % i) for i in range(7)]
            psTb = ps("psTb", BF16)

            wv = D["winB"].rearrange("(c p) f -> p c f", p=128)
            for (c0, c1) in ((0, 2048), (2048, B_COLS)):
                k.dma("pool", lambda e: e.dma_start(out=winB[:, :, c0:c1], in_=wv[:, :, c0:c1]), writes=[tok("winB")])
            k.dma("pool", lambda e: e.dma_start(out=wba[:], in_=D["wba"].rearrange("(c p) f -> p c f", p=128)), writes=[tc_])
            k.dma("pool", lambda e: e.dma_start(out=wbm[:], in_=D["wbm"].rearrange("(c p) f -> p c f", p=128)), writes=[tc_])
            k.dma("pool", lambda e: e.dma_start(out=wout[:], in_=D["wout"].rearrange("(c p) f -> p c f", p=128)), writes=[tc_])
            for t_, nm in ((cw, "cw"), (cbias, "cb"), (ifb, "ifb"), (ngb, "ng"), (l1g, "ln1_g"), (l1b, "ln1_b")):
                k.dma("sp", lambda e: e.dma_start(out=t_[:], in_=D[nm]), writes=[tc_])
            k.dma("sp", lambda e: e.dma_start(out=triI[:], in_=C["triI"]), writes=[tc_])
            k.dma("sp", lambda e: e.dma_start(out=onesf[:], in_=C["onesf"]), writes=[tc_])
            k.op("dve", lambda e: e.memset(qkraw[:], 0.0), writes=[tok("qkraw")])
            k.op("dve", lambda e: e.memset(vm[:], 1.0), writes=[tok("vm")])
            k.op("dve", lambda e: e.memset(Cst[:], 0.0), writes=[tok("Cst")])
            k.op("dve", lambda e: e.memset(Cb[:], 0.0), writes=[tok("Cb")])
            k.op("dve", lambda e: e.memset(qz[:], 0.0), writes=[tok("qkm")])

            def nm(t_):
                return tok(t_.name)

            for j in range(DBG.get("ntB", NT)):
                b = j % 2
                txa, txt = tok("bxTa%d" % b), tok("bxt%d" % b)
                k.dma("pool", lambda e: e.dma_start(out=xTa[b][:], in_=xTd[:, :, j * 128:(j + 1) * 128]), writes=[txa])
                k.dma("sp", lambda e: e.dma_start(out=xt[b][:], in_=D["x"][j * 128:(j + 1) * 128, :]), writes=[txt])
                k.dma("sp", lambda e: e.dma_start(out=yab[b][:], in_=D["yatt_scr"][j * 128:(j + 1) * 128, :]), writes=[tok("yab%d" % b)])

                def fm_group(bank, chunk0, nch):
                    bv = bank[:, :].rearrange("p (a b) -> p a b", a=4)
                    for cc in range(nch):
                        for c in range(8):
                            k.op("pe", lambda e: e.matmul(bv[:, cc, :], winB[:, c, (chunk0 + cc) * 128:(chunk0 + cc + 1) * 128],
                                                          xTa[b][:, c, :], start=(c == 0), stop=(c == 7)),
                                 reads=[tok("winB"), txa], writes=[nm(bank)])
                    return bv
                bv = fm_group(Rb[0], 0, 4)
                tqr = tok("qkraw")
                k.op("act", lambda e: e.copy(out=qkraw[:, :, 3:131], in_=bv[:, :, :]), reads=[nm(Rb[0])], writes=[tqr])
                for g_, (bk, dst) in enumerate(((Rb[1], sga), (Rb[2], sga), (Rb[3], sgm), (Rb[4], sgm))):
                    bv = fm_group(bk, 4 + 4 * g_, 4)
                    k.op("act", lambda e: e.activation(out=dst[:, (g_ % 2) * 4:(g_ % 2) * 4 + 4, :], in_=bv[:, :, :], func=AF.Sigmoid),
                         reads=[nm(bk)], writes=[tok("sg%d" % g_)])
                for (bk, c0, c1) in ((Rb[5], B_VM, B_O), (Rb[6], B_O, B_IF)):
                    for c in range(8):
                        k.op("pe", lambda e: e.matmul(bk[:, 0:c1 - c0], xTa[b][:, c, :], winB[:, c, c0:c1], start=(c == 0), stop=(c == 7)),
                             reads=[tok("winB"), txa], writes=[nm(bk)])
                    if bk is Rb[5]:
                        k.op("dve", lambda e: e.tensor_copy(out=vm[:, :, 0:128], in_=bk[:, :].rearrange("p (a b) -> p a b", a=4)),
                             reads=[nm(bk)], writes=[tok("vm")])
                    else:
                        k.op("act", lambda e: e.activation(out=so[:], in_=bk[:, :], func=AF.Sigmoid), reads=[nm(bk)], writes=[tok("so")])
                k.op("dve", lambda e: e.tensor_tensor(out=so[:], in0=so[:], in1=ngb[:], op=ALU.mult), reads=[tok("so"), tc_], writes=[tok("so")])
                for c in range(8):
                    k.op("pe", lambda e: e.matmul(Rb[6][:, 0:8], xTa[b][:, c, :], winB[:, c, B_IF:B_COLS], start=(c == 0), stop=(c == 7)),
                         reads=[tok("winB"), txa], writes=[nm(Rb[6])])
                tg = tok("gat")
                k.op("dve", lambda e: e.tensor_tensor(out=gat[:, 0:8], in0=Rb[6][:, 0:8], in1=ifb[:], op=ALU.add), reads=[nm(Rb[6]), tc_], writes=[tg])
                k.op("act", lambda e: e.activation(out=gat[:, 28:32], in_=gat[:, 4:8], func=AF.Exp, scale=-1.0), reads=[tg], writes=[tg])
                k.op("act", lambda e: e.activation(out=gat[:, 8:12], in_=gat[:, 28:32], func=AF.Ln, bias=1.0, scale=1.0), reads=[tg], writes=[tg])
                k.op("pe", lambda e: e.matmul(Rb[6][:, 16:20], triI[:], gat[:, 8:12], start=True, stop=True), reads=[tg, tc_], writes=[nm(Rb[6])])
                k.op("pe", lambda e: e.matmul(Rb[6][:, 24:28], onesf[:], gat[:, 8:12], start=True, stop=True), reads=[tg, tc_], writes=[nm(Rb[6])])
                k.op("act", lambda e: e.activation(out=gat[:, 16:20], in_=Rb[6][:, 16:20], func=AF.Exp, scale=-1.0), reads=[nm(Rb[6])], writes=[tg])
                k.op("act", lambda e: e.activation(out=gat[:, 24:28], in_=Rb[6][:, 24:28], func=AF.Exp, scale=-1.0), reads=[nm(Rb[6])], writes=[tg])
                k.op("dve", lambda e: e.tensor_tensor(out=gat[:, 12:16], in0=Rb[6][:, 16:20], in1=gat[:, 0:4], op=ALU.add), reads=[nm(Rb[6]), tg], writes=[tg])
                k.op("act", lambda e: e.activation(out=gat[:, 20:24], in_=gat[:, 12:16], func=AF.Exp, bias=float(np.log(0.125)), scale=1.0),
                     reads=[tg], writes=[tg])
                tca = tok("cacc")
                for cc in range(4):
                    k.op("dve", lambda e: e.tensor_scalar(out=cacc[:, cc, :], in0=qkraw[:, cc, 3:131], scalar1=cw[:, cc, 3:4], scalar2=cbias[:, cc:cc + 1],
                                                          op0=ALU.mult, op1=ALU.add), reads=[tqr, tc_], writes=[tca])
                    for tap in range(3):
                        k.op("dve", lambda e: e.scalar_tensor_tensor(out=cacc[:, cc, :], in0=qkraw[:, cc, tap:tap + 128], scalar=cw[:, cc, tap:tap + 1],
                                                                     in1=cacc[:, cc, :], op0=ALU.mult, op1=ALU.add), reads=[tqr, tc_, tca], writes=[tca])
                k.op("dve", lambda e: e.tensor_copy(out=qkraw[:, :, 0:3], in_=qkraw[:, :, 128:131]), reads=[tqr], writes=[tqr])
                tq = tok("qkm")
                k.op("act", lambda e: e.activation(out=qkm[:, 2:4, :], in_=cacc[:, 2:4, :], func=AF.Silu), reads=[tca], writes=[tq])
                k.op("act", lambda e: e.activation(out=qz[0:64, 0:4:2, :], in_=cacc[0:64, 0:2, :], func=AF.Silu), reads=[tca], writes=[tq])
                k.op("act", lambda e: e.activation(out=qz[64:128, 1:4:2, :], in_=cacc[64:128, 0:2, :], func=AF.Silu), reads=[tca], writes=[tq])
                sv = Rb[0][:, :].rearrange("p (a b) -> p a b", a=4)
                for h in range(4):
                    hp, hc = (h % 2) * 64, h // 2
                    k.op("pe", lambda e: e.matmul(sv[:, h, :], qkm[:, 2 + hc, :], qz[:, h, :], start=True, stop=True),
                         reads=[tq], writes=[nm(Rb[0])])
                for h in range(4):
                    k.op("dve", lambda e: e.scalar_tensor_tensor(out=PTm[:, h, :], in0=sv[:, h, :], scalar=gat[:, 20 + h:21 + h], in1=causT[:],
                                                                 op0=ALU.mult, op1=ALU.mult), reads=[nm(Rb[0]), tg, tc_], writes=[tok("PTm")])
                for h in range(4):
                    hp, hc = (h % 2) * 64, h // 2
                    bk = Rb[1 + h // 2]
                    ov = bk[:, 0:258].rearrange("p (a b) -> p a b", a=2)
                    k.op("pe", lambda e: e.matmul(ov[:, h % 2, :], PTm[:, h, :], vm[:, h, 0:129], start=True, stop=False),
                         reads=[tok("PTm"), tok("vm")], writes=[nm(bk)])
                    k.op("pe", lambda e: e.matmul(ov[:, h % 2, :], qz[:, h, :], Cb[:, hc, 0:129], start=False, stop=True),
                         reads=[tq, tok("Cb")], writes=[nm(bk)])
                kv = psTb[:, 0:256].rearrange("p (a b) -> p a b", a=2)
                for hc in range(2):
                    k.op("pe", lambda e: e.transpose(kv[:, hc, :], qkm[:, 2 + hc, :], identb[:]), reads=[tq, tc_], writes=[nm(psTb)])
                for h in range(4):
                    hq, hc = (h % 2) * 64, h // 2
                    k.op("dve", lambda e: e.tensor_scalar(out=kp2[:, hc, hq:hq + 64], in0=kv[:, hc, hq:hq + 64], scalar1=gat[:, 20 + h:21 + h],
                                                          scalar2=None, op0=ALU.mult), reads=[nm(psTb), tg], writes=[tok("kp2")])
                for h in range(4):
                    hp, hc = (h % 2) * 64, h // 2
                    bk = Rb[3 + h // 2]
                    cv = bk[:, 0:258].rearrange("p (a b) -> p a b", a=2)
                    k.op("pe", lambda e: e.matmul(cv[:, h % 2, :], kp2[:, hc, :], vm[:, h, 0:129], start=True, stop=True),
                         reads=[tok("kp2"), tok("vm")], writes=[nm(bk)])
                    k.op("dve", lambda e: e.tensor_tensor(out=ctmp[hp:hp + 64, :], in0=cv[hp:hp + 64, h % 2, :], in1=Cst[hp:hp + 64, hc, :], op=ALU.add),
                         reads=[nm(bk), tok("Cst")], writes=[tok("ctmp")])
                    k.op("dve", lambda e: e.tensor_scalar(out=Cst[hp:hp + 64, hc, :], in0=ctmp[hp:hp + 64, :], scalar1=gat[hp:hp + 64, 24 + h:25 + h],
                                                          scalar2=None, op0=ALU.mult), reads=[tok("ctmp"), tg], writes=[tok("Cst")])
                    k.op("act", lambda e: e.copy(out=Cb[hp:hp + 64, hc, 0:129], in_=Cst[hp:hp + 64, hc, :]), reads=[tok("Cst")], writes=[tok("Cb")])
                ths, thv = tok("hs"), tok("hv")
                for h in range(4):
                    bk = Rb[1 + h // 2]
                    ov = bk[:, 0:258].rearrange("p (a b) -> p a b", a=2)
                    k.op("dve", lambda e: e.tensor_scalar(out=hs[:, h:h + 1], in0=ov[:, h % 2, 128:129], scalar1=gat[:, 16 + h:17 + h], scalar2=None,
                                                          op0=ALU.mult), reads=[nm(bk), tg], writes=[ths])
                k.op("act", lambda e: e.activation(out=hs[:, 0:4], in_=hs[:, 0:4], func=AF.Abs), reads=[ths], writes=[ths])
                k.op("dve", lambda e: e.tensor_scalar(out=hs[:, 0:4], in0=hs[:, 0:4], scalar1=1.0, scalar2=None, op0=ALU.max), reads=[ths], writes=[ths])
                k.op("dve", lambda e: e.reciprocal(out=hs[:, 4:8], in_=hs[:, 0:4]), reads=[ths], writes=[ths])
                k.op("dve", lambda e: e.tensor_tensor(out=hs[:, 4:8], in0=hs[:, 4:8], in1=gat[:, 16:20], op=ALU.mult), reads=[ths, tg], writes=[ths])
                for h in range(4):
                    bk = Rb[1 + h // 2]
                    ov = bk[:, 0:258].rearrange("p (a b) -> p a b", a=2)
                    k.op("act", lambda e: e.activation(out=hv[:, h, :], in_=ov[:, h % 2, 0:128], func=AF.Copy, scale=hs[:, 4 + h:5 + h]),
                         reads=[nm(bk), ths], writes=[thv])
                k.op("dve", lambda e: e.tensor_reduce(out=hs[:, 8:12], in_=hv[:], axis=AX.X, op=ALU.add), reads=[thv], writes=[ths])
                k.op("dve", lambda e: e.tensor_scalar(out=hs[:, 8:12], in0=hs[:, 8:12], scalar1=-1.0 / 128, scalar2=None, op0=ALU.mult), reads=[ths], writes=[ths])
                for h in range(4):
                    k.op("act", lambda e: e.activation(out=hv[:, h, :], in_=hv[:, h, :], func=AF.Identity, bias=hs[:, 8 + h:9 + h], scale=1.0),
                         reads=[ths, thv], writes=[thv])
                k.op("act", lambda e: e.activation(out=sq[:, 0:512], in_=hv[:].rearrange("p a b -> p (a b)"), func=AF.Square), reads=[thv], writes=[tok("sq")])
                k.op("dve", lambda e: e.tensor_reduce(out=hs[:, 12:16], in_=sq[:, 0:512].rearrange("p (a b) -> p a b", a=4), axis=AX.X, op=ALU.add),
                     reads=[tok("sq")], writes=[ths])
                k.op("dve", lambda e: e.tensor_scalar(out=hs[:, 12:16], in0=hs[:, 12:16], scalar1=1.0 / 128, scalar2=LN_EPS, op0=ALU.mult, op1=ALU.add),
                     reads=[ths], writes=[ths])
                k.op("act", lambda e: e.activation(out=hs[:, 16:20], in_=hs[:, 12:16], func=AF.Sqrt), reads=[ths], writes=[ths])
                k.op("dve", lambda e: e.reciprocal(out=hs[:, 16:20], in_=hs[:, 16:20]), reads=[ths], writes=[ths])
                tym = tok("ym")
                for h in range(4):
                    k.op("dve", lambda e: e.scalar_tensor_tensor(out=ym[:, h * 128:(h + 1) * 128], in0=hv[:, h, :], scalar=hs[:, 16 + h:17 + h],
                                                                 in1=so[:, h * 128:(h + 1) * 128], op0=ALU.mult, op1=ALU.mult),
                         reads=[thv, ths, tok("so")], writes=[tym])
                if dbg is not None:
                    k.dma("sp", lambda e: e.dma_start(out=dbg["ym"][j * 128:(j + 1) * 128, :], in_=ym[:]), reads=[tym], writes=[tok("dbg")])
                tv = psTb[:, :].rearrange("p (a b) -> p a b", a=8)
                for c in range(4):
                    k.op("pe", lambda e: e.transpose(tv[:, c, :], yab[b][:, c * 128:(c + 1) * 128], identb[:]), reads=[tok("yab%d" % b), tc_], writes=[nm(psTb)])
                k.op("act", lambda e: e.copy(out=yaT[:], in_=tv[:, 0:4, :]), reads=[nm(psTb)], writes=[tok("yaT")])
                for c in range(4):
                    k.op("pe", lambda e: e.transpose(tv[:, 4 + c, :], ym[:, c * 128:(c + 1) * 128], identb[:]), reads=[tym, tc_], writes=[nm(psTb)])
                k.op("act", lambda e: e.copy(out=ymT[:], in_=tv[:, 4:8, :]), reads=[nm(psTb)], writes=[tok("ymT")])
                for half in range(2):
                    for (bk, w_, y_, tn) in ((Rb[half], wba, yaT, "yaT"), (Rb[2 + half], wbm, ymT, "ymT")):
                        mv = bk[:, :].rearrange("p (a b) -> p a b", a=4)
                        for c4 in range(4):
                            c8 = half * 4 + c4
                            for kc in range(4):
                                k.op("pe", lambda e: e.matmul(mv[:, c4, :], w_[:, kc, c8 * 128:(c8 + 1) * 128], y_[:, kc, :], start=(kc == 0), stop=(kc == 3)),
                                     reads=[tc_, tok(tn)], writes=[nm(bk)])
                    k.op("dve", lambda e: e.tensor_tensor(out=t1[:], in0=sga[:, half * 4:half * 4 + 4, :].rearrange("p a b -> p (a b)"), in1=Rb[half][:, :], op=ALU.mult),
                         reads=[tok("sg0"), tok("sg1"), nm(Rb[half])], writes=[tok("t1")])
                    k.op("dve", lambda e: e.tensor_tensor(out=t2[:], in0=sgm[:, half * 4:half * 4 + 4, :].rearrange("p a b -> p (a b)"), in1=Rb[2 + half][:, :], op=ALU.mult),
                         reads=[tok("sg2"), tok("sg3"), nm(Rb[2 + half])], writes=[tok("t2")])
                    k.op("dve", lambda e: e.tensor_tensor(out=mixT[:, half * 4:half * 4 + 4, :].rearrange("p a b -> p (a b)"), in0=t1[:], in1=t2[:], op=ALU.add),
                         reads=[tok("t1"), tok("t2")], writes=[tok("mixT")])
                tac = tok("bacc%d" % b)
                for half in range(2):
                    bk = Rb[4 + half]
                    for c8 in range(8):
                        k.op("pe", lambda e: e.matmul(bk[:, :], mixT[:, c8, :], wout[:, c8, half * 512:(half + 1) * 512], start=(c8 == 0), stop=(c8 == 7)),
                             reads=[tok("mixT"), tc_], writes=[nm(bk)])
                    k.op("dve", lambda e: e.scalar_tensor_tensor(out=acc[b][:, half * 512:(half + 1) * 512], in0=xt[b][:, half * 512:(half + 1) * 512],
                                                                 scalar=float(ALPHA), in1=bk[:, :], op0=ALU.mult, op1=ALU.add),
                         reads=[txt, nm(bk)], writes=[tac])
                layer_norm_tile(k, acc[b], tac, sq, tok("sq"), st, j, tok("bst%d" % j), l1g, l1b, tc_)
                k.dma("sp", lambda e: e.dma_start(out=D["x1"][j * 128:(j + 1) * 128, :], in_=acc[b][:]), reads=[tac], writes=[Tok()])
            k.barrier()


def t5_bucket_np(n):
    n = np.maximum(n, 0)
    nf = np.maximum(n, 1).astype(np.float32)
    large = 16 + (np.log(nf / np.float32(16)) / np.float32(np.log(128 / 16)) * np.float32(16)).astype(np.int32)
    large = np.minimum(large, 31)
    return np.where(n < 16, n, large)


def mixer_consts():
    c = {}
    kk = np.arange(128)
    c["identb"] = np.eye(128, dtype=np.float32).astype(ml_dtypes.bfloat16)
    c["causT"] = (kk[:, None] <= kk[None, :]).astype(ml_dtypes.bfloat16)
    c["ones"] = np.ones((128, 128), dtype=ml_dtypes.bfloat16)
    c["negmask"] = np.where(kk[None, :] <= kk[:, None], 0.0, -1e30).astype(np.float32)
    c["triI"] = (kk[:, None] <= kk[None, :]).astype(np.float32)
    c["onesf"] = np.ones((128, 128), dtype=np.float32)
    ft = np.array([0.5] + [2.0 ** -it for it in range(1, NIT + 1)], dtype=np.float32)
    c["ftab"] = np.tile(ft[None, :], (128, 1))
    return c


MIX_CONST_DT = {"ftab": F32, "identb": BF16, "causT": BF16, "ones": BF16, "negmask": F32, "triI": F32, "onesf": F32}

MIX_IN = {"xT": [DM, T], "x": [T, DM], "winA": [DM, A_COLS], "winB": [DM, B_COLS], "wba": [512, DM], "wbm": [512, DM],
          "wout": [DM, DM], "cw": [128, 4, 4], "cb": [128, 4], "ifb": [128, 8], "ng": [128, 512], "ln1_g": [128, DM],
          "ln1_b": [128, DM], "ebd": [128, 8, 128], "ebp": [128, 8, 128], "cfar": [128, 8]}


def declare_mixer_inputs(nc, D):
    for n, shp in MIX_IN.items():
        D[n] = nc.dram_tensor(n, shp, F32, kind="ExternalInput").ap()
    mc = mixer_consts()
    D.setdefault("consts", {})
    for n, a in mc.items():
        if n not in D["consts"]:
            D["consts"][n] = nc.dram_tensor("c_" + n, list(a.shape), MIX_CONST_DT[n], kind="ExternalInput").ap()


def mixer_host_shared(w_in, conv_w, conv_b, mlstm_i_bias, mlstm_f_bias, mlstm_norm_g, w_branch_attn, w_branch_mlstm,
                      w_out, ln1_g, ln1_b, rel_bias):
    m = {}
    for n, a in mixer_consts().items():
        m["c_" + n] = a
    w = w_in[0]
    sp = np.cumsum([0, 512, 512, 512, 512, 64, 8, 256, 256, 512, 4, 4, 512, 1024, 1024])
    seg = {n: w[:, sp[i]:sp[i + 1]] for i, n in enumerate(
        ["q_a", "k_a", "v_a", "q_i", "k_i", "w_i", "q_m", "k_m", "v_m", "i_m", "f_m", "o_m", "g_a", "g_m"])}
    m["winA"] = np.ascontiguousarray(np.concatenate([seg["q_a"], seg["k_a"], seg["q_i"], seg["k_i"], seg["k_i"], seg["v_a"], seg["w_i"]], axis=1))
    m["winB"] = np.ascontiguousarray(np.concatenate([seg["q_m"], seg["k_m"], seg["g_a"], seg["g_m"], seg["v_m"], seg["o_m"], seg["i_m"], seg["f_m"]], axis=1))
    m["wba"] = np.ascontiguousarray(w_branch_attn[0])
    m["wbm"] = np.ascontiguousarray(w_branch_mlstm[0])
    m["wout"] = np.ascontiguousarray(w_out[0])
    m["cw"] = np.ascontiguousarray(conv_w[0].reshape(4, 4, 128).transpose(2, 1, 0))
    m["cb"] = np.ascontiguousarray(conv_b[0].reshape(4, 128).T)
    m["ifb"] = np.ascontiguousarray(np.broadcast_to(np.concatenate([mlstm_i_bias[0], mlstm_f_bias[0]])[None, :], (128, 8)))
    m["ng"] = np.ascontiguousarray(np.broadcast_to(mlstm_norm_g[0][None, :], (128, 512)))
    m["ln1_g"] = np.ascontiguousarray(np.broadcast_to(ln1_g[0][None, :], (128, DM)))
    m["ln1_b"] = np.ascontiguousarray(np.broadcast_to(ln1_b[0][None, :], (128, DM)))
    kk = np.arange(128)
    dd = kk[None, :] - kk[:, None]
    bd = t5_bucket_np(dd)
    bp = t5_bucket_np(dd + 128)
    m["ebd"] = np.ascontiguousarray(rel_bias[:, bd].transpose(1, 0, 2))
    m["ebp"] = np.ascontiguousarray(rel_bias[:, bp].transpose(1, 0, 2))
    m["cfar"] = np.ascontiguousarray(np.broadcast_to(rel_bias[:, 31][None, :], (128, 8)))
    return m


def build_mixer_only():
    nc = bass.Bass("TRN2", target_bir_lowering=False)
    D = {}
    declare_mixer_inputs(nc, D)
    D["x1"] = nc.dram_tensor("x1", [T, DM], F32, kind="ExternalOutput").ap()
    dbg = {"yatt": nc.dram_tensor("dbg_yatt", [T, 512], BF16, kind="ExternalOutput").ap(),
           "ym": nc.dram_tensor("dbg_ym", [T, 512], BF16, kind="ExternalOutput").ap()}
    D["yatt_scr"] = dbg["yatt"]
    with ExitStack() as es:
        k = K(nc, es)
        mixer_phase(nc, k, D, dbg)
        k.finish("sp")
    return nc


def host_consts():
    c = {}
    c["identf"] = np.eye(128, dtype=np.float32)
    c["iota"] = np.tile(np.arange(CAP, dtype=np.float32)[None, :], (128, 1))
    rb = (np.arange(E, dtype=np.float32)[None, :] * (NG * CAP) + (np.arange(NT, dtype=np.float32)[:, None] // 8) * CAP + 1.0)
    c["rowbase"] = np.ascontiguousarray(np.broadcast_to(rb[None, :, :], (128, NT, E))).astype(np.float32)
    kk = np.arange(128)
    c["tri"] = (kk[:, None] < kk[None, :]).astype(ml_dtypes.bfloat16)
    c["ones"] = np.ones((128, 128), dtype=ml_dtypes.bfloat16)
    return c


CONST_DT = {"identf": F32, "iota": F32, "rowbase": F32, "tri": BF16, "ones": BF16}


def build_moe_only():
    nc = bass.Bass("TRN2", target_bir_lowering=False)
    D = {}
    D["x1"] = nc.dram_tensor("x1", [T, DM], F32, kind="ExternalInput").ap()
    D["out"] = nc.dram_tensor("out", [T, DM], F32, kind="ExternalOutput").ap()
    D["yscr"] = nc.dram_tensor("yscr", [E * NG * CAP, DM], F32, kind="Internal").ap()
    declare_moe_inputs(nc, D)
    es = ExitStack()
    with es:
        k = K(nc, es)
        moe_phase(nc, k, D)
        k.finish("sp")
    return nc


def declare_moe_inputs(nc, D):
    hc = host_consts()
    D["consts"] = {n: nc.dram_tensor("c_" + n, list(a.shape), CONST_DT[n], kind="ExternalInput").ap() for n, a in hc.items()}
    D["w_router"] = nc.dram_tensor("w_router", [128, 8, E], F32, kind="ExternalInput").ap()
    D["b_router"] = nc.dram_tensor("b_router", [128, E], F32, kind="ExternalInput").ap()
    D["b_gate_up"] = nc.dram_tensor("b_gate_up", [128, E, 16], F32, kind="ExternalInput").ap()
    D["ln2_g"] = nc.dram_tensor("ln2_g", [128, DM], F32, kind="ExternalInput").ap()
    D["ln2_b"] = nc.dram_tensor("ln2_b", [128, DM], F32, kind="ExternalInput").ap()
    D["w_gate_up"] = nc.dram_tensor("w_gate_up", [E, DM, 2048], F32, kind="ExternalInput").ap()
    D["w_down"] = nc.dram_tensor("w_down", [E, DM, DM], F32, kind="ExternalInput").ap()
    D["b_down"] = nc.dram_tensor("b_down", [E, DM], F32, kind="ExternalInput").ap()


def moe_host_inputs(w_router, b_router, w_gate_up, b_gate_up, w_down, b_down, ln2_g, ln2_b):
    m = {}
    for n, a in host_consts().items():
        m["c_" + n] = a
    m["w_router"] = np.ascontiguousarray(w_router[0].reshape(8, 128, E).transpose(1, 0, 2))
    m["b_router"] = np.ascontiguousarray(np.broadcast_to(b_router[0][None, :], (128, E)))
    m["b_gate_up"] = np.ascontiguousarray(b_gate_up[0].reshape(E, 16, 128).transpose(2, 0, 1))
    m["ln2_g"] = np.ascontiguousarray(np.broadcast_to(ln2_g[0][None, :], (128, DM)))
    m["ln2_b"] = np.ascontiguousarray(np.broadcast_to(ln2_b[0][None, :], (128, DM)))
    m["w_gate_up"] = np.ascontiguousarray(w_gate_up[0])
    m["w_down"] = np.ascontiguousarray(w_down[0])
    m["b_down"] = np.ascontiguousarray(b_down[0])
    return m


def build_full():
    nc = bass.Bass("TRN2", target_bir_lowering=False)
    D = {}
    declare_mixer_inputs(nc, D)
    hc = host_consts()
    for n, a in hc.items():
        if n not in D["consts"]:
            D["consts"][n] = nc.dram_tensor("c_" + n, list(a.shape), CONST_DT[n], kind="ExternalInput").ap()
    D["w_router"] = nc.dram_tensor("w_router", [128, 8, E], F32, kind="ExternalInput").ap()
    D["b_router"] = nc.dram_tensor("b_router", [128, E], F32, kind="ExternalInput").ap()
    D["b_gate_up"] = nc.dram_tensor("b_gate_up", [128, E, 16], F32, kind="ExternalInput").ap()
    D["ln2_g"] = nc.dram_tensor("ln2_g", [128, DM], F32, kind="ExternalInput").ap()
    D["ln2_b"] = nc.dram_tensor("ln2_b", [128, DM], F32, kind="ExternalInput").ap()
    D["w_gate_up"] = nc.dram_tensor("w_gate_up", [E, DM, 2048], F32, kind="ExternalInput").ap()
    D["w_down"] = nc.dram_tensor("w_down", [E, DM, DM], F32, kind="ExternalInput").ap()
    D["b_down"] = nc.dram_tensor("b_down", [E, DM], F32, kind="ExternalInput").ap()
    D["x1"] = nc.dram_tensor("x1", [T, DM], F32, kind="Internal").ap()
    D["yscr"] = nc.dram_tensor("yscr", [E * NG * CAP, DM], F32, kind="Internal").ap()
    D["yatt_scr"] = nc.dram_tensor("yatt_scr", [T, 512], BF16, kind="Internal").ap()
    D["out"] = nc.dram_tensor("out", [T, DM], F32, kind="ExternalOutput").ap()
    with ExitStack() as es:
        k = K(nc, es)
        mixer_phase(nc, k, D, None)
        moe_phase(nc, k, D)
        k.finish("sp")
    return nc


def kernel(x, w_in, conv_w, conv_b, mlstm_i_bias, mlstm_f_bias, mlstm_norm_g, w_branch_attn, w_branch_mlstm, w_out,
           ln1_g, ln1_b, w_router, b_router, w_gate_up, b_gate_up, w_down, b_down, ln2_g, ln2_b, rel_bias):
    f = lambda a: np.asarray(a, dtype=np.float32)
    x = f(x)
    shared = mixer_host_shared(f(w_in), f(conv_w), f(conv_b), f(mlstm_i_bias), f(mlstm_f_bias), f(mlstm_norm_g),
                               f(w_branch_attn), f(w_branch_mlstm), f(w_out), f(ln1_g), f(ln1_b), f(rel_bias))
    shared.update(moe_host_inputs(f(w_router), f(b_router), f(w_gate_up), f(b_gate_up), f(w_down), f(b_down), f(ln2_g), f(ln2_b)))
    nc = build_full()
    in_maps = []
    for c in range(NCORES):
        m = dict(shared)
        m["x"] = np.ascontiguousarray(x[c])
        m["xT"] = np.ascontiguousarray(x[c].T)
        in_maps.append(m)
    res = run_bass_kernel_spmd(nc, in_maps, core_ids=list(range(NCORES)))
    return np.stack([np.asarray(r["out"], dtype=np.float32) for r in res.results], axis=0)
```

```python
import numpy as np
import ml_dtypes
from contextlib import ExitStack
import concourse.bass as bass
import concourse.mybir as mybir
from concourse.bass_utils import run_bass_kernel_spmd

F32 = mybir.dt.float32
BF16 = mybir.dt.bfloat16
I32 = mybir.dt.int32
AF = mybir.ActivationFunctionType
ALU = mybir.AluOpType
AX = mybir.AxisListType

NCORES = 8
T = 4096
NT = 32
DM = 1024
E = 32
CAP = 224
SGM = (128, CAP - 128)
NG = 4
ALPHA = 2.0 ** 0.25
LN_EPS = 1e-5


DBG = {}


class Tok:
    __slots__ = ("w", "r")

    def __init__(self):
        self.w = None
        self.r = {}


class K:
    NDS = 24

    def __init__(self, nc, es):
        self.nc = nc
        self.es = es
        self.eng = {"pe": nc.tensor, "act": nc.scalar, "dve": nc.vector, "pool": nc.gpsimd, "sp": nc.sync}
        self.semh = {}
        for n in self.eng:
            self.semh[n] = es.enter_context(nc.semaphore("s_" + n))
        for i in range(self.NDS):
            self.semh[("d", i)] = es.enter_context(nc.semaphore("d%d" % i))
        self.cnt = {k: 0 for k in self.semh}
        self.seen = {n: {} for n in self.eng}
        self.dnext = 0
        self.dq = {}
        self.nins = 0
        self.pe_last = None

    def _deps(self, reads, writes):
        evs = {}
        for t in reads:
            if t.w is not None and evs.get(t.w[0], 0) < t.w[1]:
                evs[t.w[0]] = t.w[1]
        for t in writes:
            if t.w is not None and evs.get(t.w[0], 0) < t.w[1]:
                evs[t.w[0]] = t.w[1]
            for kk, v in t.r.items():
                if evs.get(kk, 0) < v:
                    evs[kk] = v
        return evs

    def _resolve_pe(self):
        if self.pe_last is not None:
            self.pe_last.then_inc(self.semh["pe"], 1)
            self.cnt["pe"] += 1
            self.pe_last = None

    def _wait(self, en, evs, skip_self=False):
        seen = self.seen[en]
        for kk, v in evs.items():
            if skip_self and kk == en:
                continue
            if seen.get(kk, 0) >= v:
                continue
            if kk == "pe" and v > self.cnt["pe"]:
                self._resolve_pe()
            self.eng[en].wait_ge(self.semh[kk], v)
            seen[kk] = v

    def _record(self, ev, reads, writes):
        kk, v = ev
        for t in reads:
            if t.r.get(kk, 0) < v:
                t.r[kk] = v
        for t in writes:
            t.w = ev
            t.r = {}

    def op(self, en, fn, reads=(), writes=(), self_ok=False):
        evs = self._deps(reads, writes)
        self._wait(en, evs, skip_self=(en == "pe" or self_ok))
        ins = fn(self.eng[en])
        if en == "pe" and not DBG.get("eager_pe"):
            self.pe_last = ins
            self._record((en, self.cnt[en] + 1), reads, writes)
        else:
            self.cnt[en] += 1
            ins.then_inc(self.semh[en], 1)
            self._record((en, self.cnt[en]), reads, writes)
        self.nins += 1

    def dma(self, q, fn, reads=(), writes=()):
        if q == "pool" and DBG.get("simsem"):
            key = ("x", len(self.semh))
            self.semh[key] = self.es.enter_context(self.nc.semaphore("x%d" % len(self.semh)))
            self.cnt[key] = 0
        else:
            lo_, hi_ = (0, 8) if q == "pool" else (8, self.NDS)
            i = self.dq.get(q, lo_)
            self.dq[q] = lo_ + (i + 1 - lo_) % (hi_ - lo_)
            key = ("d", i)
        evs = self._deps(reads, writes)
        if self.cnt[key] > 0 and evs.get(key, 0) < self.cnt[key]:
            evs[key] = self.cnt[key]
        self._wait(q, evs)
        ins = fn(self.eng[q])
        self.cnt[key] += 16
        ins.then_inc(self.semh[key], 16)
        self._record((key, self.cnt[key]), reads, writes)
        self.nins += 1

    def barrier(self):
        self._resolve_pe()
        snap = dict(self.cnt)
        for en in self.eng:
            self._wait(en, {kk: v for kk, v in snap.items() if v > 0 and kk != en})

    def finish(self, q="sp"):
        self._resolve_pe()
        for key in list(self.semh):
            if isinstance(key, tuple) and self.cnt[key] > 0:
                self.eng[q].wait_ge(self.semh[key], self.cnt[key])
        for n in ("pe", "act", "dve", "pool"):
            if self.cnt[n] > 0:
                self.eng[q].wait_ge(self.semh[n], self.cnt[n])


def dram_bcast(handle, offset, n, parts=128):
    return bass.AP(handle, offset, [[0, parts], [1, n]])


def moe_phase(nc, k, D):
    x1 = D["x1"]
    out = D["out"]
    yscr = D["yscr"]
    C = D["consts"]
    tk = {}

    def tok(name):
        if name not in tk:
            tk[name] = Tok()
        return tk[name]

    es = ExitStack()
    with es:
        def sbp(name, shape, dt):
            return es.enter_context(nc.sbuf_tensor(name, shape, dt))

        x1b = sbp("x1b", [128, NT, DM], BF16)
        maskf = sbp("maskf", [128, NT, E], F32)
        gates = sbp("gates", [128, NT, E], F32)
        pos = sbp("pos", [128, NT, E], F32)
        ghl = sbp("ghl", [128, NT, E, 2], BF16)
        idx4 = sbp("idx4", [128, NT, 4], I32)
        ident = sbp("identf", [128, 128], F32)
        iot = sbp("iot", [128, CAP], F32)
        bgu = sbp("bgu", [128, E, 16], F32)
        tc_ = tok("const")
        k.dma("sp", lambda e: e.dma_start(out=ident[:], in_=C["identf"]), writes=[tc_])
        k.dma("sp", lambda e: e.dma_start(out=iot[:], in_=C["iota"]), writes=[tc_])
        k.dma("sp", lambda e: e.dma_start(out=bgu[:], in_=D["b_gate_up"]), writes=[tc_])
        x1v = x1.rearrange("(j p) d -> p j d", p=128)
        for q4 in range(4):
            k.dma("pool", lambda e: e.dma_start(out=x1b[:, q4 * 8:(q4 + 1) * 8, :], in_=x1v[:, q4 * 8:(q4 + 1) * 8, :]),
                  reads=[tok("x1dram")], writes=[tok("x1b%d" % q4)])

        with ExitStack() as es2:
            def sb(name, shape, dt):
                return es2.enter_context(nc.sbuf_tensor("r_" + name, shape, dt))

            def ps(name):
                return es2.enter_context(nc.psum_tensor("r_" + name, [128, 512], F32))
            wr = sb("wr", [128, 8, E], F32)
            brt = sb("brt", [128, E], F32)
            logits = sb("logits", [128, NT, E], F32)
            maskb = sb("maskb", [128, NT, E], BF16)
            rid = sb("rid", [128, NT, E], F32)
            top8 = sb("top8", [128, NT, 8], F32)
            top8r = sb("top8r", [128, NT, 8], F32)
            sm = sb("sm", [128, NT, 4], F32)
            tri = sb("tri", [128, 128], BF16)
            ones = sb("onesb", [128, 128], BF16)
            rowbase = sb("rowbase", [128, NT, E], F32)
            xt = [sb("xt%d" % i, [128, DM], F32) for i in range(2)]
            xT = [sb("xT%d" % i, [128, 8 * 128], F32) for i in range(2)]
            psA = [ps("psA%d" % i) for i in range(2)]
            psP = ps("psP")
            psS = ps("psS")
            k.dma("sp", lambda e: e.dma_start(out=rowbase[:], in_=C["rowbase"]), writes=[tc_])
            k.dma("sp", lambda e: e.dma_start(out=tri[:], in_=C["tri"]), writes=[tc_])
            k.dma("sp", lambda e: e.dma_start(out=ones[:], in_=C["ones"]), writes=[tc_])
            k.dma("sp", lambda e: e.dma_start(out=wr[:], in_=D["w_router"]), writes=[tc_])
            k.dma("sp", lambda e: e.dma_start(out=brt[:], in_=D["b_router"]), writes=[tc_])

            lgt = [tok("lg%d" % j) for j in range(NT)]
            for j in range(NT):
                b = j % 2
                txt, txT = tok("xt%d" % b), tok("xT%d" % b)
                k.dma("sp", lambda e: e.dma_start(out=xt[b][:], in_=x1[j * 128:(j + 1) * 128, :]),
                      reads=[tok("x1dram")], writes=[txt])
                for c in range(8):
                    k.op("pe", lambda e: e.transpose(psA[c // 4][:, (c % 4) * 128:(c % 4 + 1) * 128],
                                                     xt[b][:, c * 128:(c + 1) * 128], ident[:]),
                         reads=[txt, tc_], writes=[tok("psA%d" % (c // 4))])
                k.op("act", lambda e: e.copy(out=xT[b][:, 0:512], in_=psA[0][:]), reads=[tok("psA0")], writes=[txT])
                k.op("act", lambda e: e.copy(out=xT[b][:, 512:1024], in_=psA[1][:]), reads=[tok("psA1")], writes=[txT])
                pp = [psP, psS][j % 2]
                for c in range(8):
                    k.op("pe", lambda e: e.matmul(pp[:, 0:E], xT[b][:, c * 128:(c + 1) * 128], wr[:, c, :],
                                                  start=(c == 0), stop=(c == 7)),
                         reads=[txT, tc_], writes=[tok(pp.name)])
                k.op("dve", lambda e: e.tensor_tensor(out=logits[:, j, :], in0=pp[:, 0:E], in1=brt[:], op=ALU.add),
                     reads=[tok(pp.name), tc_], writes=[lgt[j]])
                k.op("dve", lambda e: e.max(out=top8[:, j, :], in_=logits[:, j, :]), reads=[lgt[j]], writes=[lgt[j]])
            rt = tok("rtall")
            for j in range(NT):
                tk["rt%d" % j] = rt

            def fl(t_):
                return t_[:].rearrange("p a b -> p (a b)")
            k.op("dve", lambda e: e.tensor_tensor(out=maskf[:], in0=logits[:], in1=bc_in(top8[:, :, 3], E), op=ALU.is_ge),
                 reads=lgt, writes=[rt])
            k.op("dve", lambda e: e.tensor_tensor(out=gates[:], in0=logits[:], in1=bc_in(top8[:, :, 0], E), op=ALU.subtract),
                 reads=lgt, writes=[rt])
            k.op("act", lambda e: e.activation(out=fl(gates), in_=fl(gates), func=AF.Exp), reads=[rt], writes=[rt])
            k.op("dve", lambda e: e.tensor_tensor(out=fl(gates), in0=fl(gates), in1=fl(maskf), op=ALU.mult), reads=[rt], writes=[rt])
            k.op("dve", lambda e: e.tensor_reduce(out=sm[:, :, 1], in_=gates[:], axis=AX.X, op=ALU.add), reads=[rt], writes=[rt])
            k.op("dve", lambda e: e.reciprocal(out=sm[:, :, 2], in_=sm[:, :, 1]), reads=[rt], writes=[rt])
            k.op("dve", lambda e: e.tensor_tensor(out=gates[:], in0=gates[:], in1=bc_in(sm[:, :, 2], E), op=ALU.mult), reads=[rt], writes=[rt])
            k.op("dve", lambda e: e.tensor_copy(out=ghl[:, :, :, 0], in_=gates[:]), reads=[rt], writes=[rt])
            k.op("dve", lambda e: e.tensor_tensor(out=ghl[:, :, :, 1], in0=gates[:], in1=ghl[:, :, :, 0], op=ALU.subtract), reads=[rt], writes=[rt])
            k.op("dve", lambda e: e.tensor_copy(out=fl(maskb), in_=fl(maskf)), reads=[rt], writes=[rt])

            pst = [tok("pos%d" % j) for j in range(NT)]
            for j in range(NT):
                g = j // 8
                pp = [psP, psS][j % 2]
                first = True
                for i in range(g * 8, j):
                    k.op("pe", lambda e: e.matmul(pp[:, 0:E], ones[:], maskb[:, i, :], start=first, stop=False),
                         reads=[rt, tc_], writes=[tok(pp.name)])
                    first = False
                k.op("pe", lambda e: e.matmul(pp[:, 0:E], tri[:], maskb[:, j, :], start=first, stop=True),
                     reads=[rt, tc_], writes=[tok(pp.name)])
                k.op("act", lambda e: e.copy(out=pos[:, j, :], in_=pp[:, 0:E]), reads=[tok(pp.name)], writes=[pst[j]])
            k.op("dve", lambda e: e.tensor_tensor(out=fl(rid), in0=fl(pos), in1=fl(rowbase), op=ALU.add), reads=pst + [tc_], writes=[rt])
            k.op("dve", lambda e: e.tensor_tensor(out=fl(rid), in0=fl(rid), in1=fl(maskf), op=ALU.mult), reads=[rt], writes=[rt])
            rdt = [tok("rid%d" % j) for j in range(NT)]
            for j in range(NT):
                k.op("dve", lambda e: e.max(out=top8r[:, j, :], in_=rid[:, j, :]), reads=[rt], writes=[rdt[j]])
            k.op("dve", lambda e: e.tensor_scalar(out=idx4[:], in0=top8r[:, :, 0:4], scalar1=-1.0, scalar2=None, op0=ALU.add),
                 reads=rdt + [rt], writes=[rt])
            k.barrier()

        with ExitStack() as es2:
            def sb(name, shape, dt):
                return es2.enter_context(nc.sbuf_tensor("x_" + name, shape, dt))

            def ps(name):
                return es2.enter_context(nc.psum_tensor("x_" + name, [128, 512], F32))
            wgu = [sb("wgu%d" % i, [128, 8, 2048], BF16) for i in range(2)]
            wd = [sb("wd%d" % i, [128, 8, DM], BF16) for i in range(2)]
            sel = [sb("sel%d" % i, [128, 8, CAP], BF16) for i in range(2)]
            xeT = [sb("xeT%d" % i, [128, 8 * CAP], BF16) for i in range(2)]
            aT = sb("aT", [128, 8 * CAP], BF16)
            gc = [sb("gc%d" % i, [128, CAP], F32) for i in range(1)]
            sg_ = [sb("sg%d" % i, [128, CAP], F32) for i in range(1)]
            uc = [sb("uc%d" % i, [128, CAP], F32) for i in range(1)]
            gsl = [sb("gsl%d" % i, [128, 2], F32) for i in range(2)]
            ysb = [sb("ysb%d" % i, [128, 512], F32) for i in range(2)]
            psA = [ps("psA%d" % i) for i in range(2)]
            psU = [ps("psU%d" % i) for i in range(2)]
            psD = [ps("psD%d" % i) for i in range(2)]
            psS = ps("psS")
            for n in ("psA0", "psA1", "psS", "psP"):
                tk.pop(n, None)

            def load_wgu(e_, c0=0, c1=8):
                s = e_ % 2
                src_ = D["w_gate_up"][e_].rearrange("(c p) f -> p c f", p=128)
                k.dma("pool", lambda e: e.dma_start(out=wgu[s][:, c0:c1, :], in_=src_[:, c0:c1, :]),
                      writes=[tok("wgu%d" % s)])

            def load_wd(e_, c0=0, c1=8):
                s = e_ % 2
                src_ = D["w_down"][e_].rearrange("(c p) f -> p c f", p=128)
                k.dma("pool", lambda e: e.dma_start(out=wd[s][:, c0:c1, :], in_=src_[:, c0:c1, :]),
                      writes=[tok("wd%d" % s)])

            def build_sel(e_, g, sb_):
                for jj in range(8):
                    j = g * 8 + jj
                    k.op("dve", lambda e: e.tensor_scalar(out=sel[sb_][:, jj, :], in0=iot[:], scalar1=pos[:, j, e_:e_ + 1],
                                                          scalar2=maskf[:, j, e_:e_ + 1], op0=ALU.is_equal, op1=ALU.mult),
                         reads=[tok("rt%d" % j), tc_], writes=[tok("sel%d" % sb_)])

            items = [(e_, g) for e_ in range(E) for g in range(NG)]

            def gather(it):
                e_, g = items[it]
                sb_ = it % 2
                tsel, txe = tok("sel%d" % sb_), tok("xeT%d" % sb_)
                for c2 in range(4):
                    pb = c2 % 2
                    for h in range(2):
                        c = 2 * c2 + h
                        for jj in range(8):
                            j = g * 8 + jj
                            k.op("pe", lambda e: e.matmul(psA[pb][:, h * CAP:(h + 1) * CAP],
                                                          x1b[:, j, c * 128:(c + 1) * 128], sel[sb_][:, jj, :],
                                                          start=(jj == 0), stop=(jj == 7)),
                                 reads=[tok("x1b%d" % g), tsel], writes=[tok("psA%d" % pb)])
                    if c2 % 2 == 0:
                        k.op("act", lambda e: e.copy(out=xeT[sb_][:, c2 * 2 * CAP:(c2 + 1) * 2 * CAP], in_=psA[pb][:, 0:2 * CAP]),
                             reads=[tok("psA%d" % pb)], writes=[txe])
                    else:
                        k.op("dve", lambda e: e.tensor_copy(out=xeT[sb_][:, c2 * 2 * CAP:(c2 + 1) * 2 * CAP], in_=psA[pb][:, 0:2 * CAP]),
                             reads=[tok("psA%d" % pb)], writes=[txe])
                for sg in range(2):
                    for jj in range(8):
                        j = g * 8 + jj
                        k.op("pe", lambda e: e.matmul(psS[0:SGM[sg], sb_ * 4 + sg * 2:sb_ * 4 + sg * 2 + 2], sel[sb_][:, jj, sg * 128:sg * 128 + SGM[sg]],
                                                      ghl[:, j, e_, :], start=(jj == 0), stop=(jj == 7)),
                             reads=[tsel, tok("rt%d" % j)], writes=[tok("psS")])
                tgs = tok("gsl%d" % sb_)
                for sg in range(2):
                    k.op("dve", lambda e: e.tensor_reduce(out=gsl[sb_][0:SGM[sg], sg:sg + 1], in_=psS[0:SGM[sg], sb_ * 4 + sg * 2:sb_ * 4 + sg * 2 + 2],
                                                          axis=AX.X, op=ALU.add),
                         reads=[tok("psS")], writes=[tgs])

            def gate_up(it):
                e_, g = items[it]
                s = e_ % 2
                sb_ = it % 2
                twgu, txe, tat = tok("wgu%d" % s), tok("xeT%d" % sb_), tok("aT")
                for kk in range(8):
                    pb = kk % 2
                    tb = 0
                    for half in range(2):
                        for c in range(8):
                            k.op("pe", lambda e: e.matmul(psU[pb][:, half * CAP:(half + 1) * CAP],
                                                          wgu[s][:, c, half * 1024 + kk * 128: half * 1024 + (kk + 1) * 128],
                                                          xeT[sb_][:, c * CAP:(c + 1) * CAP], start=(c == 0), stop=(c == 7)),
                                 reads=[twgu, txe], writes=[tok("psU%d" % pb)])
                    tg, ts_, tu = tok("gc%d" % tb), tok("sg%d" % tb), tok("uc%d" % tb)
                    k.op("dve", lambda e: e.tensor_scalar(out=gc[tb][:], in0=psU[pb][:, 0:CAP], scalar1=bgu[:, e_, kk:kk + 1],
                                                          scalar2=7.0, op0=ALU.add, op1=ALU.min),
                         reads=[tok("psU%d" % pb), tc_], writes=[tg])
                    k.op("act", lambda e: e.activation(out=sg_[tb][:], in_=gc[tb][:], func=AF.Sigmoid, scale=1.702),
                         reads=[tg], writes=[ts_])
                    k.op("act", lambda e: e.activation(out=uc[tb][:], in_=psU[pb][:, CAP:2 * CAP], func=AF.Identity,
                                                       bias=bgu[:, e_, 8 + kk:9 + kk], scale=1.0),
                         reads=[tok("psU%d" % pb), tc_], writes=[tu])
                    k.op("dve", lambda e: e.tensor_scalar(out=uc[tb][:], in0=uc[tb][:], scalar1=7.0, scalar2=-7.0,
                                                          op0=ALU.min, op1=ALU.max), reads=[tu], writes=[tu])
                    k.op("dve", lambda e: e.tensor_tensor(out=sg_[tb][:], in0=gc[tb][:], in1=sg_[tb][:], op=ALU.mult),
                         reads=[tg, ts_], writes=[ts_])
                    k.op("dve", lambda e: e.scalar_tensor_tensor(out=aT[:, kk * CAP:(kk + 1) * CAP], in0=uc[tb][:],
                                                                 scalar=1.0, in1=sg_[tb][:], op0=ALU.add, op1=ALU.mult),
                         reads=[tu, ts_], writes=[tat])

            def down(it):
                e_, g = items[it]
                s = e_ % 2
                sb_ = it % 2
                twd, tat, tgs = tok("wd%d" % s), tok("aT"), tok("gsl%d" % sb_)
                for sg in range(2):
                    for half in range(2):
                        pb = half
                        tys = tok("ysb%d" % half)
                        for kk in range(8):
                            k.op("pe", lambda e: e.matmul(psD[pb][0:SGM[sg], :], aT[:, kk * CAP + sg * 128: kk * CAP + sg * 128 + SGM[sg]],
                                                          wd[s][:, kk, half * 512:(half + 1) * 512], start=(kk == 0), stop=(kk == 7)),
                                 reads=[tat, twd], writes=[tok("psD%d" % pb)])
                        k.op("act", lambda e: e.activation(out=ysb[half][0:SGM[sg], :], in_=psD[pb][0:SGM[sg], :],
                                                           func=AF.Copy, scale=gsl[sb_][0:SGM[sg], sg:sg + 1]),
                             reads=[tok("psD%d" % pb), tgs], writes=[tys])
                        row0 = e_ * (NG * CAP) + g * CAP + sg * 128
                        k.dma("sp", lambda e: e.dma_start(out=yscr[row0:row0 + SGM[sg], half * 512:(half + 1) * 512], in_=ysb[half][0:SGM[sg], :]),
                              reads=[tys], writes=[Tok()])

            for e0 in range(2):
                load_wgu(e0)
                load_wd(e0)
            build_sel(0, 0, 0)
            gather(0)
            for it, (e_, g) in enumerate(items):
                if it + 1 < len(items):
                    build_sel(items[it + 1][0], items[it + 1][1], (it + 1) % 2)
                gate_up(it)
                if e_ >= 1 and e_ + 1 < E:
                    load_wgu(e_ + 1, 2 * g, 2 * g + 2)
                if it + 1 < len(items):
                    gather(it + 1)
                down(it)
                if e_ >= 1 and e_ + 1 < E:
                    load_wd(e_ + 1, 2 * g, 2 * g + 2)
            k.barrier()

        with ExitStack() as es2:
            def sb(name, shape, dt):
                return es2.enter_context(nc.sbuf_tensor("c_" + name, shape, dt))

            def ps(name):
                return es2.enter_context(nc.psum_tensor("c_" + name, [128, 512], F32))
            for n in list(tk):
                if n.startswith("ps"):
                    tk.pop(n)
            lng = sb("lng", [128, DM], F32)
            lnb = sb("lnb", [128, DM], F32)
            bdn = sb("bdn", [E, DM], F32)
            xt = [sb("xt%d" % i, [128, DM], F32) for i in range(2)]
            acc = [sb("acc%d" % i, [128, DM], F32) for i in range(2)]
            gtt = [sb("gtt%d" % i, [128, 4, DM], F32) for i in range(2)]
            gT = [sb("gT%d" % i, [E, 128], F32) for i in range(2)]
            sq = sb("sq", [128, DM], F32)
            st = sb("st", [128, NT, 8], F32)
            psT = ps("psT")
            psB = [ps("psB%d" % i) for i in range(2)]
            tk.pop("xt0", None)
            tk.pop("xt1", None)
            k.dma("sp", lambda e: e.dma_start(out=lng[:], in_=D["ln2_g"]), writes=[tc_])
            k.dma("sp", lambda e: e.dma_start(out=lnb[:], in_=D["ln2_b"]), writes=[tc_])
            k.dma("sp", lambda e: e.dma_start(out=bdn[:], in_=D["b_down"]), writes=[tc_])
            nrows = E * NG * CAP
            def A(j):
                b = j % 2
                tgq, txt, tac = [tok("gtt%d_%d" % (b, q_)) for q_ in range(4)], tok("xt%d" % b), tok("acc%d" % b)
                trt = tok("rt%d" % j)
                for q in range(4):
                    k.dma("pool", lambda e: e.indirect_dma_start(
                        out=gtt[b][:, q, :], out_offset=None, in_=yscr[:, :],
                        in_offset=bass.IndirectOffsetOnAxis(ap=idx4[:, j, q:q + 1], axis=0)),
                        reads=[trt, tok("yscr")], writes=[tgq[q]])
                k.dma("sp", lambda e: e.dma_start(out=xt[b][:], in_=x1[j * 128:(j + 1) * 128, :]),
                      reads=[tok("x1dram")], writes=[txt])
                tgT = tok("gT%d" % b)
                k.op("pe", lambda e: e.transpose(psT[0:E, 0:128], gates[:, j, :], ident[:]), reads=[trt, tc_], writes=[tok("psT")])
                k.op("act", lambda e: e.copy(out=gT[b][:], in_=psT[0:E, 0:128]), reads=[tok("psT")], writes=[tgT])
                for half in range(2):
                    k.op("pe", lambda e: e.matmul(psB[half][:], gT[b][:], bdn[:, half * 512:(half + 1) * 512], start=True, stop=False),
                         reads=[tgT, tc_], writes=[tok("psB%d" % half)])
                    for q in range(4):
                        k.op("pe", lambda e: e.matmul(psB[half][:], ident[:], gtt[b][:, q, half * 512:(half + 1) * 512], start=False, stop=(q == 3)),
                             reads=[tgq[q], tc_], writes=[tok("psB%d" % half)])
                yield
                for half in range(2):
                    k.op("dve", lambda e: e.scalar_tensor_tensor(out=acc[b][:, half * 512:(half + 1) * 512], in0=xt[b][:, half * 512:(half + 1) * 512],
                                                                 scalar=float(ALPHA), in1=psB[half][:], op0=ALU.mult, op1=ALU.add),
                         reads=[txt, tok("psB%d" % half)], writes=[tac])
                    yield

            def L(j):
                b = j % 2
                a_, ta, tst = acc[b], tok("acc%d" % b), tok("st%d" % j)
                k.op("dve", lambda e: e.reduce_sum(out=st[:, j, 0:1], in_=a_[:], axis=AX.X), reads=[ta], writes=[tst])
                k.op("dve", lambda e: e.tensor_scalar(out=st[:, j, 1:2], in0=st[:, j, 0:1], scalar1=-1.0 / DM, scalar2=None,
                                                      op0=ALU.mult), reads=[tst], writes=[tst])
                k.op("act", lambda e: e.activation(out=a_[:], in_=a_[:], func=AF.Identity, bias=st[:, j, 1:2], scale=1.0),
                     reads=[tst, ta], writes=[ta])
                k.op("act", lambda e: e.activation(out=sq[:], in_=a_[:], func=AF.Square), reads=[ta], writes=[tok("sq")])
                yield
                k.op("dve", lambda e: e.reduce_sum(out=st[:, j, 2:3], in_=sq[:], axis=AX.X), reads=[tok("sq")], writes=[tst])
                k.op("dve", lambda e: e.tensor_scalar(out=st[:, j, 3:4], in0=st[:, j, 2:3], scalar1=1.0 / DM, scalar2=LN_EPS,
                                                      op0=ALU.mult, op1=ALU.add), reads=[tst], writes=[tst])
                k.op("act", lambda e: e.activation(out=st[:, j, 4:5], in_=st[:, j, 3:4], func=AF.Sqrt), reads=[tst], writes=[tst])
                yield
                k.op("dve", lambda e: e.reciprocal(out=st[:, j, 5:6], in_=st[:, j, 4:5]), reads=[tst], writes=[tst])
                k.op("dve", lambda e: e.scalar_tensor_tensor(out=a_[:], in0=a_[:], scalar=st[:, j, 5:6], in1=lng[:],
                                                             op0=ALU.mult, op1=ALU.mult), reads=[tst, ta, tc_], writes=[ta])
                k.op("dve", lambda e: e.tensor_tensor(out=a_[:], in0=a_[:], in1=lnb[:], op=ALU.add), reads=[ta, tc_], writes=[ta])
                k.dma("sp", lambda e: e.dma_start(out=out[j * 128:(j + 1) * 128, :], in_=a_[:]),
                      reads=[ta], writes=[Tok()])
                yield

            for _ in A(0):
                pass
            for j in range(NT):
                gl = L(j)
                ga = A(j + 1) if j + 1 < NT else None
                while gl is not None or ga is not None:
                    if ga is not None and next(ga, "done") == "done":
                        ga = None
                    if gl is not None and next(gl, "done") == "done":
                        gl = None
            k.barrier()


def layer_norm_tile(k, a, ta, sq, tsq, st, j, tst, g_bc, b_bc, tc_):
    k.op("dve", lambda e: e.reduce_sum(out=st[:, j, 0:1], in_=a[:], axis=AX.X), reads=[ta], writes=[tst])
    k.op("dve", lambda e: e.tensor_scalar(out=st[:, j, 1:2], in0=st[:, j, 0:1], scalar1=-1.0 / DM, scalar2=None,
                                          op0=ALU.mult), reads=[tst], writes=[tst])
    k.op("act", lambda e: e.activation(out=a[:], in_=a[:], func=AF.Identity, bias=st[:, j, 1:2], scale=1.0),
         reads=[tst, ta], writes=[ta])
    k.op("act", lambda e: e.activation(out=sq[:], in_=a[:], func=AF.Square), reads=[ta], writes=[tsq])
    k.op("dve", lambda e: e.reduce_sum(out=st[:, j, 2:3], in_=sq[:], axis=AX.X), reads=[tsq], writes=[tst])
    k.op("dve", lambda e: e.tensor_scalar(out=st[:, j, 3:4], in0=st[:, j, 2:3], scalar1=1.0 / DM, scalar2=LN_EPS,
                                          op0=ALU.mult, op1=ALU.add), reads=[tst], writes=[tst])
    k.op("act", lambda e: e.activation(out=st[:, j, 4:5], in_=st[:, j, 3:4], func=AF.Sqrt), reads=[tst], writes=[tst])
    k.op("dve", lambda e: e.reciprocal(out=st[:, j, 5:6], in_=st[:, j, 4:5]), reads=[tst], writes=[tst])
    k.op("dve", lambda e: e.scalar_tensor_tensor(out=a[:], in0=a[:], scalar=st[:, j, 5:6], in1=g_bc[:],
                                                 op0=ALU.mult, op1=ALU.mult), reads=[tst, ta, tc_], writes=[ta])
    k.op("dve", lambda e: e.tensor_tensor(out=a[:], in0=a[:], in1=b_bc[:], op=ALU.add), reads=[ta, tc_], writes=[ta])


NIT = 20
A_QI, A_KI, A_VA, A_WI, A_COLS = 1024, 1536, 1664, 2176, 2184
B_GA, B_GM, B_VM, B_O, B_IF, B_COLS = 512, 1536, 2560, 3072, 3584, 3592


def bc_mid(x, n):
    return bass.AP(x.tensor, x.offset, [list(x.ap[0]), [0, n], list(x.ap[1])])


def bc_in(x, n):
    return bass.AP(x.tensor, x.offset, [list(x.ap[0]), list(x.ap[1]), [0, n]])


def mixer_phase(nc, k, D, dbg=None):
    C = D["consts"]
    tk = {}

    def tok(name):
        if name not in tk:
            tk[name] = Tok()
        return tk[name]

    tc_ = tok("const")
    xTd = D["xT"].rearrange("(c p) t -> p c t", p=128)
    with ExitStack() as es:
        identb = es.enter_context(nc.sbuf_tensor("identb", [128, 128], BF16))
        causT = es.enter_context(nc.sbuf_tensor("causT", [128, 128], BF16))
        k.dma("sp", lambda e: e.dma_start(out=identb[:], in_=C["identb"]), writes=[tc_])
        k.dma("sp", lambda e: e.dma_start(out=causT[:], in_=C["causT"]), writes=[tc_])

        with ExitStack() as es2:
            def sb(name, shape, dt):
                return es2.enter_context(nc.sbuf_tensor("a_" + name, shape, dt))

            def ps(name, dt=F32):
                return es2.enter_context(nc.psum_tensor("a_" + name, [128, 512] if dt == F32 else [128, 1024], dt))
            winA = sb("winA", [128, 8, A_COLS], BF16)
            kaT = sb("kaT", [128, 4, T], BF16)
            vaug = sb("vaug", [128, NT, 8, 66], BF16)
            kiT2 = sb("kiT2", [128, T], BF16)
            scores = [sb("scores%d" % i, [128, T], F32) for i in range(2)]
            mbuf = sb("mbuf", [128, T], BF16)
            maskT = sb("maskT", [128, NT, 128], BF16)
            onesb = sb("onesb", [128, 128], BF16)
            negmask = sb("negmask", [128, 128], F32)
            ebx = [sb("ebx%d" % i, [128, 8, 128], BF16) for i in range(2)]
            ebf = sb("ebf", [128, 8, 128], F32)
            cfx = sb("cfx", [128, 8], F32)
            ftab = sb("ftab", [128, NIT + 1], F32)
            xTa = [sb("xTa%d" % i, [128, 8, 128], BF16) for i in range(2)]
            qaT = [sb("qaT%d" % i, [128, 8, 128], BF16) for i in range(3)]
            qiT = sb("qiT", [128, 8, 128], BF16)
            wi = sb("wi", [128, 8], F32)
            diagw = sb("diagw", [128, 8, 128], BF16)
            R = [sb("R%d" % i, [128, 512], BF16) for i in range(2)]
            Eb = [sb("Eb%d" % i, [128, 8, 128], BF16) for i in range(2)]
            PT = [sb("PT%d" % i, [128, 8, 128], BF16) for i in range(2)]
            tmpE = sb("tmpE", [128, 8, 128], BF16)
            oc = sb("oc", [128, 8, 65], F32)
            bs = sb("bs", [128, 8], F32)
            stp = sb("stp", [128, NIT + 1], F32)
            dummy = sb("dummy", [128, 16], F32)
            self_ctr = [0]
            rden = sb("rden", [128, 8], F32)
            yat = [sb("yat%d" % i, [128, 512], BF16) for i in range(2)]
            psOf = [ps("psOf%d" % i) for i in range(2)]
            psOn = [ps("psOn%d" % i) for i in range(2)]
            Rk = [ps("K%d" % i) for i in range(3)]
            psT = ps("psT", BF16)
            if DBG.get("verbose"):
                print("sweepA sbuf remaining", nc.sbuf_bytes_remaining)

            for cb_, (c0, c1) in enumerate(((0, A_VA), (A_VA, A_COLS))):
                k.dma("pool", lambda e: e.dma_start(out=winA[:, :, c0:c1],
                                                    in_=D["winA"].rearrange("(c p) f -> p c f", p=128)[:, :, c0:c1]),
                      writes=[tok("winA")])
            k.dma("sp", lambda e: e.dma_start(out=onesb[:], in_=C["ones"]), writes=[tc_])
            k.dma("sp", lambda e: e.dma_start(out=negmask[:], in_=C["negmask"]), writes=[tc_])
            k.dma("sp", lambda e: e.dma_start(out=ftab[:], in_=C["ftab"]), writes=[tc_])
            k.dma("sp", lambda e: e.dma_start(out=cfx[:], in_=D["cfar"]), writes=[tc_])
            k.op("act", lambda e: e.activation(out=cfx[:], in_=cfx[:], func=AF.Exp), reads=[tc_], writes=[tc_])
            for i, nm in enumerate(("ebd", "ebp")):
                k.dma("sp", lambda e: e.dma_start(out=ebf[:], in_=D[nm]), writes=[tok("ebf")])
                k.op("act", lambda e: e.activation(out=ebx[i][:], in_=ebf[:], func=AF.Exp), reads=[tok("ebf")], writes=[tc_])
            for i in range(3):
                k.op("dve", lambda e: e.memset(qaT[i][:], 0.0), writes=[tok("qaT%d" % i)])
            k.op("dve", lambda e: e.memset(qiT[:], 0.0), writes=[tok("qiT")])
            tmk = tok("maskT")

            def S1(j):
                b = j % 2
                n = (j + 1) * 128
                txa = tok("xTa%d" % b)
                q3 = j % 3
                k.dma("pool", lambda e: e.dma_start(out=xTa[b][:], in_=xTd[:, :, j * 128:(j + 1) * 128]), writes=[txa])

                def fm_group(bank, chunk0, nch):
                    bv = bank[:, :].rearrange("p (a b) -> p a b", a=4)
                    for cc in range(nch):
                        for c in range(8):
                            k.op("pe", lambda e: e.matmul(bv[:, cc, :], winA[:, c, (chunk0 + cc) * 128:(chunk0 + cc + 1) * 128],
                                                          xTa[b][:, c, :], start=(c == 0), stop=(c == 7)),
                                 reads=[tok("winA"), txa], writes=[tok(bank.name)])
                    return bv
                bv = fm_group(Rk[0], 0, 4)
                k.op("act", lambda e: e.copy(out=qaT[q3][0:64, 0:8:2, :], in_=bv[0:64, :, :]), reads=[tok(Rk[0].name)], writes=[tok("qaT%d" % q3)])
                k.op("act", lambda e: e.copy(out=qaT[q3][64:128, 1:8:2, :], in_=bv[64:128, :, :]), reads=[tok(Rk[0].name)], writes=[tok("qaT%d" % q3)])
                bv = fm_group(Rk[1], 4, 4)
                k.op("act", lambda e: e.copy(out=kaT[:, :, j * 128:(j + 1) * 128], in_=bv[:, :, :]),
                     reads=[tok(Rk[1].name)], writes=[tok("kaT%d" % j)])
                bv = fm_group(Rk[2], 8, 4)
                k.op("act", lambda e: e.copy(out=qiT[0:64, 0:8:2, :], in_=bv[0:64, :, :]), reads=[tok(Rk[2].name)], writes=[tok("qiT")])
                k.op("act", lambda e: e.copy(out=qiT[64:128, 1:8:2, :], in_=bv[64:128, :, :]), reads=[tok(Rk[2].name)], writes=[tok("qiT")])
                bv = fm_group(Rk[0], 12, 1)
                k.op("act", lambda e: e.copy(out=kiT2[:, j * 128:(j + 1) * 128], in_=bv[:, 0, :]),
                     reads=[tok(Rk[0].name)], writes=[tok("kiT%d" % j)])
                for c in range(8):
                    k.op("pe", lambda e: e.matmul(Rk[1][:, :], xTa[b][:, c, :], winA[:, c, A_VA:A_WI], start=(c == 0), stop=(c == 7)),
                         reads=[tok("winA"), txa], writes=[tok(Rk[1].name)])
                k.op("pool", lambda e: e.memset(vaug[:, j, :, 64:66], 1.0), writes=[tok("vaug%d" % j)])
                k.op("act", lambda e: e.copy(out=vaug[:, j, :, 0:64], in_=Rk[1][:, :].rearrange("p (a b) -> p a b", a=8)),
                     reads=[tok(Rk[1].name)], writes=[tok("vaug%d" % j)])
                if j < 2:
                    return
                for c in range(8):
                    k.op("pe", lambda e: e.matmul(Rk[2][:, 0:8], xTa[b][:, c, :], winA[:, c, A_WI:A_COLS], start=(c == 0), stop=(c == 7)),
                         reads=[tok("winA"), txa], writes=[tok(Rk[2].name)])
                k.op("act", lambda e: e.activation(out=wi[:], in_=Rk[2][:, 0:8], func=AF.Copy, scale=float(0.125 * 8 ** -0.5)),
                     reads=[tok(Rk[2].name)], writes=[tok("wi")])
                for h in range(8):
                    k.op("act", lambda e: e.activation(out=diagw[:, h, :], in_=identb[:], func=AF.Copy, scale=wi[:, h:h + 1]),
                         reads=[tok("wi"), tc_], writes=[tok("diagw")])
                tsc = tok("scores%d" % b)
                for blk in range((n + 511) // 512):
                    ncl = min(512, n - blk * 512)
                    kread = [tok("kiT%d" % t_) for t_ in range(blk * 4, min(blk * 4 + 4, j + 1))]
                    def mm2(h):
                        hb = h % 2
                        k.op("pe", lambda e: e.matmul(Rk[2][:, 0:ncl], diagw[:, h, :], R[hb][:, 0:ncl], start=(h == 0), stop=(h == 7)),
                             reads=[tok("diagw"), tok("R%d" % hb)], writes=[tok(Rk[2].name)])
                    for h in range(8):
                        hb = h % 2
                        k.op("pe", lambda e: e.matmul(Rk[hb][:, 0:ncl], qiT[:, h, :],
                                                      kiT2[:, blk * 512:blk * 512 + ncl], start=True, stop=True),
                             reads=[tok("qiT")] + kread, writes=[tok(Rk[hb].name)])
                        k.op("act", lambda e: e.activation(out=R[hb][:, 0:ncl], in_=Rk[hb][:, 0:ncl], func=AF.Relu),
                             reads=[tok(Rk[hb].name)], writes=[tok("R%d" % hb)])
                        if h >= 1:
                            mm2(h - 1)
                    mm2(7)
                    k.op("act", lambda e: e.copy(out=scores[b][:, blk * 512:blk * 512 + ncl], in_=Rk[2][:, 0:ncl]),
                         reads=[tok(Rk[2].name)], writes=[tsc])

            def S2a(j):
                b = j % 2
                n = (j + 1) * 128
                sc = scores[b]
                tsc, tb_ = tok("scores%d" % b), tok("bs")
                k.op("dve", lambda e: e.tensor_reduce(out=bs[:, 0:1], in_=sc[:, 0:n], axis=AX.X, op=ALU.max), reads=[tsc], writes=[tb_])
                k.op("dve", lambda e: e.tensor_reduce(out=bs[:, 1:2], in_=sc[:, 0:256], axis=AX.X, op=ALU.min), reads=[tsc], writes=[tb_])
                k.op("dve", lambda e: e.tensor_tensor(out=bs[:, 2:3], in0=bs[:, 0:1], in1=bs[:, 1:2], op=ALU.subtract), reads=[tb_], writes=[tb_])
                k.op("dve", lambda e: e.tensor_tensor(out=sc[:, j * 128:(j + 1) * 128], in0=sc[:, j * 128:(j + 1) * 128],
                                                      in1=negmask[:], op=ALU.add), reads=[tsc, tc_], writes=[tsc])
                k.op("dve", lambda e: e.tensor_scalar(out=stp[:], in0=ftab[:], scalar1=bs[:, 2:3], scalar2=None, op0=ALU.mult),
                     reads=[tb_, tc_], writes=[tb_])
                k.op("dve", lambda e: e.tensor_tensor(out=bs[:, 3:4], in0=bs[:, 1:2], in1=stp[:, 0:1], op=ALU.add), reads=[tb_], writes=[tb_])
                yield
                nf = DBG.get("fillers", 0)
                so = nf > 0

                def fill():
                    for f_ in range(nf):
                        self_ctr[0] += 1
                        c_ = self_ctr[0] % 16
                        k.op("dve", lambda e: e.memset(dummy[:, c_:c_ + 1], 0.0), self_ok=True)
                for it in range(1, NIT + 1):
                    k.op("dve", lambda e: e.tensor_scalar(out=mbuf[:, 0:n], in0=sc[:, 0:n], scalar1=bs[:, 3:4], scalar2=0.0,
                                                          op0=ALU.is_ge, op1=ALU.add, accum_out=bs[:, 4:5]),
                         reads=[tsc, tb_], writes=[tok("mbuf"), tb_], self_ok=(so and it > 1))
                    fill()
                    k.op("dve", lambda e: e.tensor_scalar(out=bs[:, 5:6], in0=bs[:, 4:5], scalar1=256.0, scalar2=-0.5,
                                                          op0=ALU.is_ge, op1=ALU.add), reads=[tb_], writes=[tb_], self_ok=so)
                    fill()
                    k.op("dve", lambda e: e.scalar_tensor_tensor(out=bs[:, 3:4], in0=stp[:, it:it + 1], scalar=bs[:, 5:6], in1=bs[:, 3:4],
                                                                 op0=ALU.mult, op1=ALU.add), reads=[tb_], writes=[tb_], self_ok=so)
                    fill()
                    yield
                k.op("dve", lambda e: e.scalar_tensor_tensor(out=bs[:, 3:4], in0=stp[:, NIT:NIT + 1], scalar=-0.5, in1=bs[:, 3:4],
                                                             op0=ALU.mult, op1=ALU.add), reads=[tb_], writes=[tb_])
                k.op("dve", lambda e: e.tensor_scalar(out=mbuf[:, 0:n], in0=sc[:, 0:n], scalar1=bs[:, 3:4], scalar2=None,
                                                      op0=ALU.is_ge), reads=[tsc, tb_], writes=[tok("mbuf")])

            def S2b(j):
                for i0 in range(0, j + 1, 8):
                    cnt = min(8, j + 1 - i0)
                    pv = psT[:, :].rearrange("p (a b) -> p a b", a=8)
                    for i in range(i0, i0 + cnt):
                        k.op("pe", lambda e: e.transpose(pv[:, i - i0, :], mbuf[:, i * 128:(i + 1) * 128], identb[:]),
                             reads=[tok("mbuf"), tc_], writes=[tok("psT")])
                    k.op("act", lambda e: e.copy(out=maskT[:, i0:i0 + cnt, :], in_=pv[:, 0:cnt, :]), reads=[tok("psT")], writes=[tmk])

            def S3(j):
                q3 = j % 3

                def qk(i):
                    eb = i % 2
                    for hg in range(2):
                        qb = Rk[(2 * i + hg) % 3]
                        lv = qb[:, :].rearrange("p (a b) -> p a b", a=4)
                        for h4 in range(4):
                            h = hg * 4 + h4
                            k.op("pe", lambda e: e.matmul(lv[:, h4, :], kaT[:, h // 2, i * 128:(i + 1) * 128],
                                                          qaT[q3][:, h, :], start=True, stop=True),
                                 reads=[tok("kaT%d" % i), tok("qaT%d" % q3)], writes=[tok(qb.name)])
                        k.op("act", lambda e: e.activation(out=Eb[eb][:, hg * 4:(hg + 1) * 4, :], in_=lv[:, :, :], func=AF.Exp, scale=0.125),
                             reads=[tok(qb.name)], writes=[tok("Eb%d" % eb)])

                def pv(i):
                    eb = i % 2
                    far = (i <= j - 2)
                    if j >= 2:
                        m_ap, mreads = maskT[:, i, :], [tmk]
                    elif i == j:
                        m_ap, mreads = causT[:], [tc_]
                    else:
                        m_ap, mreads = onesb[:], [tc_]
                    tpt = tok("PT%d" % eb)
                    if far:
                        k.op("dve", lambda e: e.tensor_tensor(out=PT[eb][:], in0=Eb[eb][:], in1=bc_mid(m_ap, 8), op=ALU.mult),
                             reads=[tok("Eb%d" % eb)] + mreads, writes=[tpt])
                    else:
                        kind = 0 if i == j else 1
                        k.op("pool", lambda e: e.tensor_tensor(out=tmpE[:], in0=ebx[kind][:], in1=bc_mid(m_ap, 8), op=ALU.mult),
                             reads=[tc_] + mreads, writes=[tok("tmpE")])
                        k.op("dve", lambda e: e.tensor_tensor(out=PT[eb][:], in0=Eb[eb][:], in1=tmpE[:], op=ALU.mult),
                             reads=[tok("Eb%d" % eb), tok("tmpE")], writes=[tpt])
                    for h in range(8):
                        if far:
                            bank, st_, sp_ = psOf[h // 4], (i == 0 and h % 4 == 0), (i == j - 2 and h % 4 == 3)
                        else:
                            bank, st_, sp_ = psOn[h // 4], (i == max(j - 1, 0) and h % 4 == 0), (i == j and h % 4 == 3)
                        ov = bank[:, 0:260].rearrange("p (a b) -> p a b", a=4)
                        k.op("pe", lambda e: e.matmul(ov[:, h % 4, :], PT[eb][:, h, :], vaug[:, i, h, 0:65], start=st_, stop=sp_),
                             reads=[tpt, tok("vaug%d" % i)], writes=[tok(bank.name)])

                qk(0)
                for i in range(j + 1):
                    if i + 1 <= j:
                        qk(i + 1)
                    pv(i)
                    yield
                toc = tok("oc")
                for hg in range(2):
                    nv = psOn[hg][:, 0:260].rearrange("p (a b) -> p a b", a=4)
                    if j >= 2:
                        fv = psOf[hg][:, 0:260].rearrange("p (a b) -> p a b", a=4)
                        k.op("dve", lambda e: e.tensor_tensor(out=oc[:, hg * 4:(hg + 1) * 4, :], in0=fv[:, :, :],
                                                              in1=bc_in(cfx[:, hg * 4:(hg + 1) * 4], 65), op=ALU.mult),
                             reads=[tok(psOf[hg].name), tc_], writes=[toc])
                        k.op("dve", lambda e: e.tensor_tensor(out=oc[:, hg * 4:(hg + 1) * 4, :], in0=oc[:, hg * 4:(hg + 1) * 4, :],
                                                              in1=nv[:, :, :], op=ALU.add), reads=[tok(psOn[hg].name), toc], writes=[toc])
                    else:
                        k.op("dve", lambda e: e.tensor_copy(out=oc[:, hg * 4:(hg + 1) * 4, :], in_=nv[:, :, :]),
                             reads=[tok(psOn[hg].name)], writes=[toc])
                k.op("dve", lambda e: e.reciprocal(out=rden[:], in_=oc[:, :, 64]), reads=[toc], writes=[tok("rden")])
                yb_ = j % 2
                k.op("dve", lambda e: e.tensor_tensor(out=yat[yb_][:].rearrange("p (a b) -> p a b", a=8), in0=oc[:, :, 0:64],
                                                      in1=bc_in(rden[:], 64), op=ALU.mult),
                     reads=[toc, tok("rden")], writes=[tok("yat%d" % yb_)])
                k.dma("sp", lambda e: e.dma_start(out=D["yatt_scr"][j * 128:(j + 1) * 128, :], in_=yat[yb_][:]),
                      reads=[tok("yat%d" % yb_)], writes=[Tok()])

            def drain(g):
                if g is not None:
                    for _ in g:
                        pass

            ntA = DBG.get("ntA", NT)
            for t in range(-2, ntA):
                ga = S2a(t + 1) if 2 <= t + 1 < ntA else None
                gb = S3(t) if t >= 0 else None
                if ga is not None and gb is not None:
                    ppi = max(1, -(-(t + 1) // 5))
                    next(ga)
                    for it in range(NIT):
                        next(ga)
                        for _ in range(ppi):
                            if next(gb, "done") == "done":
                                gb = None
                                break
                        if gb is None:
                            break
                    drain(ga)
                    drain(gb)
                else:
                    drain(ga)
                    drain(gb)
                if t + 2 < ntA:
                    S1(t + 2)
                if 2 <= t + 1 < ntA:
                    S2b(t + 1)
            k.barrier()

        if DBG.get("noB"):
            return
        with ExitStack() as es2:
            def sb(name, shape, dt):
                return es2.enter_context(nc.sbuf_tensor("b_" + name, shape, dt))

            def ps(name, dt=F32):
                return es2.enter_context(nc.psum_tensor("b_" + name, [128, 512] if dt == F32 else [128, 1024], dt))
            for nme in list(tk):
                if nme.startswith("a_"):
                    tk.pop(nme)
            winB = sb("winB", [128, 8, B_COLS], BF16)
            wba = sb("wba", [128, 4, DM], BF16)
            wbm = sb("wbm", [128, 4, DM], BF16)
            wout = sb("wout", [128, 8, DM], BF16)
            cw = sb("cw", [128, 4, 4], F32)
            cbias = sb("cb", [128, 4], F32)
            ifb = sb("ifb", [128, 8], F32)
            ngb = sb("ngb", [128, 512], F32)
            l1g = sb("l1g", [128, DM], F32)
            l1b = sb("l1b", [128, DM], F32)
            triI = sb("triI", [128, 128], F32)
            onesf = sb("onesf", [128, 128], F32)
            xTa = [sb("xTa%d" % i, [128, 8, 128], BF16) for i in range(2)]
            xt = [sb("xt%d" % i, [128, DM], F32) for i in range(2)]
            qkraw = sb("qkraw", [128, 4, 131], F32)
            cacc = sb("cacc", [128, 4, 128], F32)
            qkm = sb("qkm", [128, 4, 128], BF16)
            qz = sb("qz", [128, 4, 128], BF16)
            sga = sb("sga", [128, 8, 128], F32)
            sgm = sb("sgm", [128, 8, 128], F32)
            vm = sb("vm", [128, 4, 130], BF16)
            so = sb("so", [128, 512], F32)
            gat = sb("gat", [128, 32], F32)
            PTm = sb("PTm", [128, 4, 128], BF16)
            kp2 = sb("kp2", [128, 2, 128], BF16)
            Cst = sb("Cst", [128, 2, 129], F32)
            Cb = sb("Cb", [128, 2, 130], BF16)
            ctmp = sb("ctmp", [128, 129], F32)
            hs = sb("hs", [128, 24], F32)
            hv = sb("hv", [128, 4, 128], F32)
            sq = sb("sq", [128, DM], F32)
            ym = sb("ym", [128, 512], BF16)
            yaT = sb("yaT", [128, 4, 128], BF16)
            ymT = sb("ymT", [128, 4, 128], BF16)
            t1 = sb("t1", [128, 512], F32)
            t2 = sb("t2", [128, 512], F32)
            mixT = sb("mixT", [128, 8, 128], BF16)
            acc = [sb("acc%d" % i, [128, DM], F32) for i in range(2)]
            yab = [sb("yab%d" % i, [128, 512], BF16) for i in range(2)]
            st = sb("st", [128, NT, 8], F32)
            Rb = [ps("K%d" % i) for i in range(7)]
            psTb = ps("psTb", BF16)

            wv = D["winB"].rearrange("(c p) f -> p c f", p=128)
            for (c0, c1) in ((0, 2048), (2048, B_COLS)):
                k.dma("pool", lambda e: e.dma_start(out=winB[:, :, c0:c1], in_=wv[:, :, c0:c1]), writes=[tok("winB")])
            k.dma("pool", lambda e: e.dma_start(out=wba[:], in_=D["wba"].rearrange("(c p) f -> p c f", p=128)), writes=[tc_])
            k.dma("pool", lambda e: e.dma_start(out=wbm[:], in_=D["wbm"].rearrange("(c p) f -> p c f", p=128)), writes=[tc_])
            k.dma("pool", lambda e: e.dma_start(out=wout[:], in_=D["wout"].rearrange("(c p) f -> p c f", p=128)), writes=[tc_])
            for t_, nm in ((cw, "cw"), (cbias, "cb"), (ifb, "ifb"), (ngb, "ng"), (l1g, "ln1_g"), (l1b, "ln1_b")):
                k.dma("sp", lambda e: e.dma_start(out=t_[:], in_=D[nm]), writes=[tc_])
            k.dma("sp", lambda e: e.dma_start(out=triI[:], in_=C["triI"]), writes=[tc_])
            k.dma("sp", lambda e: e.dma_start(out=onesf[:], in_=C["onesf"]), writes=[tc_])
            k.op("dve", lambda e: e.memset(qkraw[:], 0.0), writes=[tok("qkraw")])
            k.op("dve", lambda e: e.memset(vm[:], 1.0), writes=[tok("vm")])
            k.op("dve", lambda e: e.memset(Cst[:], 0.0), writes=[tok("Cst")])
            k.op("dve", lambda e: e.memset(Cb[:], 0.0), writes=[tok("Cb")])
            k.op("dve", lambda e: e.memset(qz[:], 0.0), writes=[tok("qkm")])

            def nm(t_):
                return tok(t_.name)

            for j in range(DBG.get("ntB", NT)):
                b = j % 2
                txa, txt = tok("bxTa%d" % b), tok("bxt%d" % b)
                k.dma("pool", lambda e: e.dma_start(out=xTa[b][:], in_=xTd[:, :, j * 128:(j + 1) * 128]), writes=[txa])
                k.dma("sp", lambda e: e.dma_start(out=xt[b][:], in_=D["x"][j * 128:(j + 1) * 128, :]), writes=[txt])
                k.dma("sp", lambda e: e.dma_start(out=yab[b][:], in_=D["yatt_scr"][j * 128:(j + 1) * 128, :]), writes=[tok("yab%d" % b)])

                def fm_group(bank, chunk0, nch):
                    bv = bank[:, :].rearrange("p (a b) -> p a b", a=4)
                    for cc in range(nch):
                        for c in range(8):
                            k.op("pe", lambda e: e.matmul(bv[:, cc, :], winB[:, c, (chunk0 + cc) * 128:(chunk0 + cc + 1) * 128],
                                                          xTa[b][:, c, :], start=(c == 0), stop=(c == 7)),
                                 reads=[tok("winB"), txa], writes=[nm(bank)])
                    return bv
                bv = fm_group(Rb[0], 0, 4)
                tqr = tok("qkraw")
                k.op("act", lambda e: e.copy(out=qkraw[:, :, 3:131], in_=bv[:, :, :]), reads=[nm(Rb[0])], writes=[tqr])
                for g_, (bk, dst) in enumerate(((Rb[1], sga), (Rb[2], sga), (Rb[3], sgm), (Rb[4], sgm))):
                    bv = fm_group(bk, 4 + 4 * g_, 4)
                    k.op("act", lambda e: e.activation(out=dst[:, (g_ % 2) * 4:(g_ % 2) * 4 + 4, :], in_=bv[:, :, :], func=AF.Sigmoid),
                         reads=[nm(bk)], writes=[tok("sg%d" % g_)])
                for (bk, c0, c1) in ((Rb[5], B_VM, B_O), (Rb[6], B_O, B_IF)):
                    for c in range(8):
                        k.op("pe", lambda e: e.matmul(bk[:, 0:c1 - c0], xTa[b][:, c, :], winB[:, c, c0:c1], start=(c == 0), stop=(c == 7)),
                             reads=[tok("winB"), txa], writes=[nm(bk)])
                    if bk is Rb[5]:
                        k.op("dve", lambda e: e.tensor_copy(out=vm[:, :, 0:128], in_=bk[:, :].rearrange("p (a b) -> p a b", a=4)),
                             reads=[nm(bk)], writes=[tok("vm")])
                    else:
                        k.op("act", lambda e: e.activation(out=so[:], in_=bk[:, :], func=AF.Sigmoid), reads=[nm(bk)], writes=[tok("so")])
                k.op("dve", lambda e: e.tensor_tensor(out=so[:], in0=so[:], in1=ngb[:], op=ALU.mult), reads=[tok("so"), tc_], writes=[tok("so")])
                for c in range(8):
                    k.op("pe", lambda e: e.matmul(Rb[6][:, 0:8], xTa[b][:, c, :], winB[:, c, B_IF:B_COLS], start=(c == 0), stop=(c == 7)),
                         reads=[tok("winB"), txa], writes=[nm(Rb[6])])
                tg = tok("gat")
                k.op("dve", lambda e: e.tensor_tensor(out=gat[:, 0:8], in0=Rb[6][:, 0:8], in1=ifb[:], op=ALU.add), reads=[nm(Rb[6]), tc_], writes=[tg])
                k.op("act", lambda e: e.activation(out=gat[:, 28:32], in_=gat[:, 4:8], func=AF.Exp, scale=-1.0), reads=[tg], writes=[tg])
                k.op("act", lambda e: e.activation(out=gat[:, 8:12], in_=gat[:, 28:32], func=AF.Ln, bias=1.0, scale=1.0), reads=[tg], writes=[tg])
                k.op("pe", lambda e: e.matmul(Rb[6][:, 16:20], triI[:], gat[:, 8:12], start=True, stop=True), reads=[tg, tc_], writes=[nm(Rb[6])])
                k.op("pe", lambda e: e.matmul(Rb[6][:, 24:28], onesf[:], gat[:, 8:12], start=True, stop=True), reads=[tg, tc_], writes=[nm(Rb[6])])
                k.op("act", lambda e: e.activation(out=gat[:, 16:20], in_=Rb[6][:, 16:20], func=AF.Exp, scale=-1.0), reads=[nm(Rb[6])], writes=[tg])
                k.op("act", lambda e: e.activation(out=gat[:, 24:28], in_=Rb[6][:, 24:28], func=AF.Exp, scale=-1.0), reads=[nm(Rb[6])], writes=[tg])
                k.op("dve", lambda e: e.tensor_tensor(out=gat[:, 12:16], in0=Rb[6][:, 16:20], in1=gat[:, 0:4], op=ALU.add), reads=[nm(Rb[6]), tg], writes=[tg])
                k.op("act", lambda e: e.activation(out=gat[:, 20:24], in_=gat[:, 12:16], func=AF.Exp, bias=float(np.log(0.125)), scale=1.0),
                     reads=[tg], writes=[tg])
                tca = tok("cacc")
                for cc in range(4):
                    k.op("dve", lambda e: e.tensor_scalar(out=cacc[:, cc, :], in0=qkraw[:, cc, 3:131], scalar1=cw[:, cc, 3:4], scalar2=cbias[:, cc:cc + 1],
                                                          op0=ALU.mult, op1=ALU.add), reads=[tqr, tc_], writes=[tca])
                    for tap in range(3):
                        k.op("dve", lambda e: e.scalar_tensor_tensor(out=cacc[:, cc, :], in0=qkraw[:, cc, tap:tap + 128], scalar=cw[:, cc, tap:tap + 1],
                                                                     in1=cacc[:, cc, :], op0=ALU.mult, op1=ALU.add), reads=[tqr, tc_, tca], writes=[tca])
                k.op("dve", lambda e: e.tensor_copy(out=qkraw[:, :, 0:3], in_=qkraw[:, :, 128:131]), reads=[tqr], writes=[tqr])
                tq = tok("qkm")
                k.op("act", lambda e: e.activation(out=qkm[:, 2:4, :], in_=cacc[:, 2:4, :], func=AF.Silu), reads=[tca], writes=[tq])
                k.op("act", lambda e: e.activation(out=qz[0:64, 0:4:2, :], in_=cacc[0:64, 0:2, :], func=AF.Silu), reads=[tca], writes=[tq])
                k.op("act", lambda e: e.activation(out=qz[64:128, 1:4:2, :], in_=cacc[64:128, 0:2, :], func=AF.Silu), reads=[tca], writes=[tq])
                sv = Rb[0][:, :].rearrange("p (a b) -> p a b", a=4)
                for h in range(4):
                    hp, hc = (h % 2) * 64, h // 2
                    k.op("pe", lambda e: e.matmul(sv[:, h, :], qkm[:, 2 + hc, :], qz[:, h, :], start=True, stop=True),
                         reads=[tq], writes=[nm(Rb[0])])
                for h in range(4):
                    k.op("dve", lambda e: e.scalar_tensor_tensor(out=PTm[:, h, :], in0=sv[:, h, :], scalar=gat[:, 20 + h:21 + h], in1=causT[:],
                                                                 op0=ALU.mult, op1=ALU.mult), reads=[nm(Rb[0]), tg, tc_], writes=[tok("PTm")])
                for h in range(4):
                    hp, hc = (h % 2) * 64, h // 2
                    bk = Rb[1 + h // 2]
                    ov = bk[:, 0:258].rearrange("p (a b) -> p a b", a=2)
                    k.op("pe", lambda e: e.matmul(ov[:, h % 2, :], PTm[:, h, :], vm[:, h, 0:129], start=True, stop=False),
                         reads=[tok("PTm"), tok("vm")], writes=[nm(bk)])
                    k.op("pe", lambda e: e.matmul(ov[:, h % 2, :], qz[:, h, :], Cb[:, hc, 0:129], start=False, stop=True),
                         reads=[tq, tok("Cb")], writes=[nm(bk)])
                kv = psTb[:, 0:256].rearrange("p (a b) -> p a b", a=2)
                for hc in range(2):
                    k.op("pe", lambda e: e.transpose(kv[:, hc, :], qkm[:, 2 + hc, :], identb[:]), reads=[tq, tc_], writes=[nm(psTb)])
                for h in range(4):
                    hq, hc = (h % 2) * 64, h // 2
                    k.op("dve", lambda e: e.tensor_scalar(out=kp2[:, hc, hq:hq + 64], in0=kv[:, hc, hq:hq + 64], scalar1=gat[:, 20 + h:21 + h],
                                                          scalar2=None, op0=ALU.mult), reads=[nm(psTb), tg], writes=[tok("kp2")])
                for h in range(4):
                    hp, hc = (h % 2) * 64, h // 2
                    bk = Rb[3 + h // 2]
                    cv = bk[:, 0:258].rearrange("p (a b) -> p a b", a=2)
                    k.op("pe", lambda e: e.matmul(cv[:, h % 2, :], kp2[:, hc, :], vm[:, h, 0:129], start=True, stop=True),
                         reads=[tok("kp2"), tok("vm")], writes=[nm(bk)])
                    k.op("dve", lambda e: e.tensor_tensor(out=ctmp[hp:hp + 64, :], in0=cv[hp:hp + 64, h % 2, :], in1=Cst[hp:hp + 64, hc, :], op=ALU.add),
                         reads=[nm(bk), tok("Cst")], writes=[tok("ctmp")])
                    k.op("dve", lambda e: e.tensor_scalar(out=Cst[hp:hp + 64, hc, :], in0=ctmp[hp:hp + 64, :], scalar1=gat[hp:hp + 64, 24 + h:25 + h],
                                                          scalar2=None, op0=ALU.mult), reads=[tok("ctmp"), tg], writes=[tok("Cst")])
                    k.op("act", lambda e: e.copy(out=Cb[hp:hp + 64, hc, 0:129], in_=Cst[hp:hp + 64, hc, :]), reads=[tok("Cst")], writes=[tok("Cb")])
                ths, thv = tok("hs"), tok("hv")
                for h in range(4):
                    bk = Rb[1 + h // 2]
                    ov = bk[:, 0:258].rearrange("p (a b) -> p a b", a=2)
                    k.op("dve", lambda e: e.tensor_scalar(out=hs[:, h:h + 1], in0=ov[:, h % 2, 128:129], scalar1=gat[:, 16 + h:17 + h], scalar2=None,
                                                          op0=ALU.mult), reads=[nm(bk), tg], writes=[ths])
                k.op("act", lambda e: e.activation(out=hs[:, 0:4], in_=hs[:, 0:4], func=AF.Abs), reads=[ths], writes=[ths])
                k.op("dve", lambda e: e.tensor_scalar(out=hs[:, 0:4], in0=hs[:, 0:4], scalar1=1.0, scalar2=None, op0=ALU.max), reads=[ths], writes=[ths])
                k.op("dve", lambda e: e.reciprocal(out=hs[:, 4:8], in_=hs[:, 0:4]), reads=[ths], writes=[ths])
                k.op("dve", lambda e: e.tensor_tensor(out=hs[:, 4:8], in0=hs[:, 4:8], in1=gat[:, 16:20], op=ALU.mult), reads=[ths, tg], writes=[ths])
                for h in range(4):
                    bk = Rb[1 + h // 2]
                    ov = bk[:, 0:258].rearrange("p (a b) -> p a b", a=2)
                    k.op("act", lambda e: e.activation(out=hv[:, h, :], in_=ov[:, h % 2, 0:128], func=AF.Copy, scale=hs[:, 4 + h:5 + h]),
                         reads=[nm(bk), ths], writes=[thv])
                k.op("dve", lambda e: e.tensor_reduce(out=hs[:, 8:12], in_=hv[:], axis=AX.X, op=ALU.add), reads=[thv], writes=[ths])
                k.op("dve", lambda e: e.tensor_scalar(out=hs[:, 8:12], in0=hs[:, 8:12], scalar1=-1.0 / 128, scalar2=None, op0=ALU.mult), reads=[ths], writes=[ths])
                for h in range(4):
                    k.op("act", lambda e: e.activation(out=hv[:, h, :], in_=hv[:, h, :], func=AF.Identity, bias=hs[:, 8 + h:9 + h], scale=1.0),
                         reads=[ths, thv], writes=[thv])
                k.op("act", lambda e: e.activation(out=sq[:, 0:512], in_=hv[:].rearrange("p a b -> p (a b)"), func=AF.Square), reads=[thv], writes=[tok("sq")])
                k.op("dve", lambda e: e.tensor_reduce(out=hs[:, 12:16], in_=sq[:, 0:512].rearrange("p (a b) -> p a b", a=4), axis=AX.X, op=ALU.add),
                     reads=[tok("sq")], writes=[ths])
                k.op("dve", lambda e: e.tensor_scalar(out=hs[:, 12:16], in0=hs[:, 12:16], scalar1=1.0 / 128, scalar2=LN_EPS, op0=ALU.mult, op1=ALU.add),
                     reads=[ths], writes=[ths])
                k.op("act", lambda e: e.activation(out=hs[:, 16:20], in_=hs[:, 12:16], func=AF.Sqrt), reads=[ths], writes=[ths])
                k.op("dve", lambda e: e.reciprocal(out=hs[:, 16:20], in_=hs[:, 16:20]), reads=[ths], writes=[ths])
                tym = tok("ym")
                for h in range(4):
                    k.op("dve", lambda e: e.scalar_tensor_tensor(out=ym[:, h * 128:(h + 1) * 128], in0=hv[:, h, :], scalar=hs[:, 16 + h:17 + h],
                                                                 in1=so[:, h * 128:(h + 1) * 128], op0=ALU.mult, op1=ALU.mult),
                         reads=[thv, ths, tok("so")], writes=[tym])
                if dbg is not None:
                    k.dma("sp", lambda e: e.dma_start(out=dbg["ym"][j * 128:(j + 1) * 128, :], in_=ym[:]), reads=[tym], writes=[tok("dbg")])
                tv = psTb[:, :].rearrange("p (a b) -> p a b", a=8)
                for c in range(4):
                    k.op("pe", lambda e: e.transpose(tv[:, c, :], yab[b][:, c * 128:(c + 1) * 128], identb[:]), reads=[tok("yab%d" % b), tc_], writes=[nm(psTb)])
                k.op("act", lambda e: e.copy(out=yaT[:], in_=tv[:, 0:4, :]), reads=[nm(psTb)], writes=[tok("yaT")])
                for c in range(4):
                    k.op("pe", lambda e: e.transpose(tv[:, 4 + c, :], ym[:, c * 128:(c + 1) * 128], identb[:]), reads=[tym, tc_], writes=[nm(psTb)])
                k.op("act", lambda e: e.copy(out=ymT[:], in_=tv[:, 4:8, :]), reads=[nm(psTb)], writes=[tok("ymT")])
                for half in range(2):
                    for (bk, w_, y_, tn) in ((Rb[half], wba, yaT, "yaT"), (Rb[2 + half], wbm, ymT, "ymT")):
                        mv = bk[:, :].rearrange("p (a b) -> p a b", a=4)
                        for c4 in range(4):
                            c8 = half * 4 + c4
                            for kc in range(4):
                                k.op("pe", lambda e: e.matmul(mv[:, c4, :], w_[:, kc, c8 * 128:(c8 + 1) * 128], y_[:, kc, :], start=(kc == 0), stop=(kc == 3)),
                                     reads=[tc_, tok(tn)], writes=[nm(bk)])
                    k.op("dve", lambda e: e.tensor_tensor(out=t1[:], in0=sga[:, half * 4:half * 4 + 4, :].rearrange("p a b -> p (a b)"), in1=Rb[half][:, :], op=ALU.mult),
                         reads=[tok("sg0"), tok("sg1"), nm(Rb[half])], writes=[tok("t1")])
                    k.op("dve", lambda e: e.tensor_tensor(out=t2[:], in0=sgm[:, half * 4:half * 4 + 4, :].rearrange("p a b -> p (a b)"), in1=Rb[2 + half][:, :], op=ALU.mult),
                         reads=[tok("sg2"), tok("sg3"), nm(Rb[2 + half])], writes=[tok("t2")])
                    k.op("dve", lambda e: e.tensor_tensor(out=mixT[:, half * 4:half * 4 + 4, :].rearrange("p a b -> p (a b)"), in0=t1[:], in1=t2[:], op=ALU.add),
                         reads=[tok("t1"), tok("t2")], writes=[tok("mixT")])
                tac = tok("bacc%d" % b)
                for half in range(2):
                    bk = Rb[4 + half]
                    for c8 in range(8):
                        k.op("pe", lambda e: e.matmul(bk[:, :], mixT[:, c8, :], wout[:, c8, half * 512:(half + 1) * 512], start=(c8 == 0), stop=(c8 == 7)),
                             reads=[tok("mixT"), tc_], writes=[nm(bk)])
                    k.op("dve", lambda e: e.scalar_tensor_tensor(out=acc[b][:, half * 512:(half + 1) * 512], in0=xt[b][:, half * 512:(half + 1) * 512],
                                                                 scalar=float(ALPHA), in1=bk[:, :], op0=ALU.mult, op1=ALU.add),
                         reads=[txt, nm(bk)], writes=[tac])
                layer_norm_tile(k, acc[b], tac, sq, tok("sq"), st, j, tok("bst%d" % j), l1g, l1b, tc_)
                k.dma("sp", lambda e: e.dma_start(out=D["x1"][j * 128:(j + 1) * 128, :], in_=acc[b][:]), reads=[tac], writes=[Tok()])
            k.barrier()


def t5_bucket_np(n):
    n = np.maximum(n, 0)
    nf = np.maximum(n, 1).astype(np.float32)
    large = 16 + (np.log(nf / np.float32(16)) / np.float32(np.log(128 / 16)) * np.float32(16)).astype(np.int32)
    large = np.minimum(large, 31)
    return np.where(n < 16, n, large)


def mixer_consts():
    c = {}
    kk = np.arange(128)
    c["identb"] = np.eye(128, dtype=np.float32).astype(ml_dtypes.bfloat16)
    c["causT"] = (kk[:, None] <= kk[None, :]).astype(ml_dtypes.bfloat16)
    c["ones"] = np.ones((128, 128), dtype=ml_dtypes.bfloat16)
    c["negmask"] = np.where(kk[None, :] <= kk[:, None], 0.0, -1e30).astype(np.float32)
    c["triI"] = (kk[:, None] <= kk[None, :]).astype(np.float32)
    c["onesf"] = np.ones((128, 128), dtype=np.float32)
    ft = np.array([0.5] + [2.0 ** -it for it in range(1, NIT + 1)], dtype=np.float32)
    c["ftab"] = np.tile(ft[None, :], (128, 1))
    return c


MIX_CONST_DT = {"ftab": F32, "identb": BF16, "causT": BF16, "ones": BF16, "negmask": F32, "triI": F32, "onesf": F32}

MIX_IN = {"xT": [DM, T], "x": [T, DM], "winA": [DM, A_COLS], "winB": [DM, B_COLS], "wba": [512, DM], "wbm": [512, DM],
          "wout": [DM, DM], "cw": [128, 4, 4], "cb": [128, 4], "ifb": [128, 8], "ng": [128, 512], "ln1_g": [128, DM],
          "ln1_b": [128, DM], "ebd": [128, 8, 128], "ebp": [128, 8, 128], "cfar": [128, 8]}


def declare_mixer_inputs(nc, D):
    for n, shp in MIX_IN.items():
        D[n] = nc.dram_tensor(n, shp, F32, kind="ExternalInput").ap()
    mc = mixer_consts()
    D.setdefault("consts", {})
    for n, a in mc.items():
        if n not in D["consts"]:
            D["consts"][n] = nc.dram_tensor("c_" + n, list(a.shape), MIX_CONST_DT[n], kind="ExternalInput").ap()


def mixer_host_shared(w_in, conv_w, conv_b, mlstm_i_bias, mlstm_f_bias, mlstm_norm_g, w_branch_attn, w_branch_mlstm,
                      w_out, ln1_g, ln1_b, rel_bias):
    m = {}
    for n, a in mixer_consts().items():
        m["c_" + n] = a
    w = w_in[0]
    sp = np.cumsum([0, 512, 512, 512, 512, 64, 8, 256, 256, 512, 4, 4, 512, 1024, 1024])
    seg = {n: w[:, sp[i]:sp[i + 1]] for i, n in enumerate(
        ["q_a", "k_a", "v_a", "q_i", "k_i", "w_i", "q_m", "k_m", "v_m", "i_m", "f_m", "o_m", "g_a", "g_m"])}
    m["winA"] = np.ascontiguousarray(np.concatenate([seg["q_a"], seg["k_a"], seg["q_i"], seg["k_i"], seg["k_i"], seg["v_a"], seg["w_i"]], axis=1))
    m["winB"] = np.ascontiguousarray(np.concatenate([seg["q_m"], seg["k_m"], seg["g_a"], seg["g_m"], seg["v_m"], seg["o_m"], seg["i_m"], seg["f_m"]], axis=1))
    m["wba"] = np.ascontiguousarray(w_branch_attn[0])
    m["wbm"] = np.ascontiguousarray(w_branch_mlstm[0])
    m["wout"] = np.ascontiguousarray(w_out[0])
    m["cw"] = np.ascontiguousarray(conv_w[0].reshape(4, 4, 128).transpose(2, 1, 0))
    m["cb"] = np.ascontiguousarray(conv_b[0].reshape(4, 128).T)
    m["ifb"] = np.ascontiguousarray(np.broadcast_to(np.concatenate([mlstm_i_bias[0], mlstm_f_bias[0]])[None, :], (128, 8)))
    m["ng"] = np.ascontiguousarray(np.broadcast_to(mlstm_norm_g[0][None, :], (128, 512)))
    m["ln1_g"] = np.ascontiguousarray(np.broadcast_to(ln1_g[0][None, :], (128, DM)))
    m["ln1_b"] = np.ascontiguousarray(np.broadcast_to(ln1_b[0][None, :], (128, DM)))
    kk = np.arange(128)
    dd = kk[None, :] - kk[:, None]
    bd = t5_bucket_np(dd)
    bp = t5_bucket_np(dd + 128)
    m["ebd"] = np.ascontiguousarray(rel_bias[:, bd].transpose(1, 0, 2))
    m["ebp"] = np.ascontiguousarray(rel_bias[:, bp].transpose(1, 0, 2))
    m["cfar"] = np.ascontiguousarray(np.broadcast_to(rel_bias[:, 31][None, :], (128, 8)))
    return m


def build_mixer_only():
    nc = bass.Bass("TRN2", target_bir_lowering=False)
    D = {}
    declare_mixer_inputs(nc, D)
    D["x1"] = nc.dram_tensor("x1", [T, DM], F32, kind="ExternalOutput").ap()
    dbg = {"yatt": nc.dram_tensor("dbg_yatt", [T, 512], BF16, kind="ExternalOutput").ap(),
           "ym": nc.dram_tensor("dbg_ym", [T, 512], BF16, kind="ExternalOutput").ap()}
    D["yatt_scr"] = dbg["yatt"]
    with ExitStack() as es:
        k = K(nc, es)
        mixer_phase(nc, k, D, dbg)
        k.finish("sp")
    return nc


def host_consts():
    c = {}
    c["identf"] = np.eye(128, dtype=np.float32)
    c["iota"] = np.tile(np.arange(CAP, dtype=np.float32)[None, :], (128, 1))
    rb = (np.arange(E, dtype=np.float32)[None, :] * (NG * CAP) + (np.arange(NT, dtype=np.float32)[:, None] // 8) * CAP + 1.0)
    c["rowbase"] = np.ascontiguousarray(np.broadcast_to(rb[None, :, :], (128, NT, E))).astype(np.float32)
    kk = np.arange(128)
    c["tri"] = (kk[:, None] < kk[None, :]).astype(ml_dtypes.bfloat16)
    c["ones"] = np.ones((128, 128), dtype=ml_dtypes.bfloat16)
    return c


CONST_DT = {"identf": F32, "iota": F32, "rowbase": F32, "tri": BF16, "ones": BF16}


def build_moe_only():
    nc = bass.Bass("TRN2", target_bir_lowering=False)
    D = {}
    D["x1"] = nc.dram_tensor("x1", [T, DM], F32, kind="ExternalInput").ap()
    D["out"] = nc.dram_tensor("out", [T, DM], F32, kind="ExternalOutput").ap()
    D["yscr"] = nc.dram_tensor("yscr", [E * NG * CAP, DM], F32, kind="Internal").ap()
    declare_moe_inputs(nc, D)
    es = ExitStack()
    with es:
        k = K(nc, es)
        moe_phase(nc, k, D)
        k.finish("sp")
    return nc


def declare_moe_inputs(nc, D):
    hc = host_consts()
    D["consts"] = {n: nc.dram_tensor("c_" + n, list(a.shape), CONST_DT[n], kind="ExternalInput").ap() for n, a in hc.items()}
    D["w_router"] = nc.dram_tensor("w_router", [128, 8, E], F32, kind="ExternalInput").ap()
    D["b_router"] = nc.dram_tensor("b_router", [128, E], F32, kind="ExternalInput").ap()
    D["b_gate_up"] = nc.dram_tensor("b_gate_up", [128, E, 16], F32, kind="ExternalInput").ap()
    D["ln2_g"] = nc.dram_tensor("ln2_g", [128, DM], F32, kind="ExternalInput").ap()
    D["ln2_b"] = nc.dram_tensor("ln2_b", [128, DM], F32, kind="ExternalInput").ap()
    D["w_gate_up"] = nc.dram_tensor("w_gate_up", [E, DM, 2048], F32, kind="ExternalInput").ap()
    D["w_down"] = nc.dram_tensor("w_down", [E, DM, DM], F32, kind="ExternalInput").ap()
    D["b_down"] = nc.dram_tensor("b_down", [E, DM], F32, kind="ExternalInput").ap()


def moe_host_inputs(w_router, b_router, w_gate_up, b_gate_up, w_down, b_down, ln2_g, ln2_b):
    m = {}
    for n, a in host_consts().items():
        m["c_" + n] = a
    m["w_router"] = np.ascontiguousarray(w_router[0].reshape(8, 128, E).transpose(1, 0, 2))
    m["b_router"] = np.ascontiguousarray(np.broadcast_to(b_router[0][None, :], (128, E)))
    m["b_gate_up"] = np.ascontiguousarray(b_gate_up[0].reshape(E, 16, 128).transpose(2, 0, 1))
    m["ln2_g"] = np.ascontiguousarray(np.broadcast_to(ln2_g[0][None, :], (128, DM)))
    m["ln2_b"] = np.ascontiguousarray(np.broadcast_to(ln2_b[0][None, :], (128, DM)))
    m["w_gate_up"] = np.ascontiguousarray(w_gate_up[0])
    m["w_down"] = np.ascontiguousarray(w_down[0])
    m["b_down"] = np.ascontiguousarray(b_down[0])
    return m


def build_full():
    nc = bass.Bass("TRN2", target_bir_lowering=False)
    D = {}
    declare_mixer_inputs(nc, D)
    hc = host_consts()
    for n, a in hc.items():
        if n not in D["consts"]:
            D["consts"][n] = nc.dram_tensor("c_" + n, list(a.shape), CONST_DT[n], kind="ExternalInput").ap()
    D["w_router"] = nc.dram_tensor("w_router", [128, 8, E], F32, kind="ExternalInput").ap()
    D["b_router"] = nc.dram_tensor("b_router", [128, E], F32, kind="ExternalInput").ap()
    D["b_gate_up"] = nc.dram_tensor("b_gate_up", [128, E, 16], F32, kind="ExternalInput").ap()
    D["ln2_g"] = nc.dram_tensor("ln2_g", [128, DM], F32, kind="ExternalInput").ap()
    D["ln2_b"] = nc.dram_tensor("ln2_b", [128, DM], F32, kind="ExternalInput").ap()
    D["w_gate_up"] = nc.dram_tensor("w_gate_up", [E, DM, 2048], F32, kind="ExternalInput").ap()
    D["w_down"] = nc.dram_tensor("w_down", [E, DM, DM], F32, kind="ExternalInput").ap()
    D["b_down"] = nc.dram_tensor("b_down", [E, DM], F32, kind="ExternalInput").ap()
    D["x1"] = nc.dram_tensor("x1", [T, DM], F32, kind="Internal").ap()
    D["yscr"] = nc.dram_tensor("yscr", [E * NG * CAP, DM], F32, kind="Internal").ap()
    D["yatt_scr"] = nc.dram_tensor("yatt_scr", [T, 512], BF16, kind="Internal").ap()
    D["out"] = nc.dram_tensor("out", [T, DM], F32, kind="ExternalOutput").ap()
    with ExitStack() as es:
        k = K(nc, es)
        mixer_phase(nc, k, D, None)
        moe_phase(nc, k, D)
        k.finish("sp")
    return nc


def kernel(x, w_in, conv_w, conv_b, mlstm_i_bias, mlstm_f_bias, mlstm_norm_g, w_branch_attn, w_branch_mlstm, w_out,
           ln1_g, ln1_b, w_router, b_router, w_gate_up, b_gate_up, w_down, b_down, ln2_g, ln2_b, rel_bias):
    f = lambda a: np.asarray(a, dtype=np.float32)
    x = f(x)
    shared = mixer_host_shared(f(w_in), f(conv_w), f(conv_b), f(mlstm_i_bias), f(mlstm_f_bias), f(mlstm_norm_g),
                               f(w_branch_attn), f(w_branch_mlstm), f(w_out), f(ln1_g), f(ln1_b), f(rel_bias))
    shared.update(moe_host_inputs(f(w_router), f(b_router), f(w_gate_up), f(b_gate_up), f(w_down), f(b_down), f(ln2_g), f(ln2_b)))
    nc = build_full()
    in_maps = []
    for c in range(NCORES):
        m = dict(shared)
        m["x"] = np.ascontiguousarray(x[c])
        m["xT"] = np.ascontiguousarray(x[c].T)
        in_maps.append(m)
    res = run_bass_kernel_spmd(nc, in_maps, core_ids=list(range(NCORES)))
    return np.stack([np.asarray(r["out"], dtype=np.float32) for r in res.results], axis=0)
```

```python
import numpy as np
import ml_dtypes
from contextlib import ExitStack
import concourse.bass as bass
import concourse.mybir as mybir
from concourse.bass_utils import run_bass_kernel_spmd

F32 = mybir.dt.float32
BF16 = mybir.dt.bfloat16
I32 = mybir.dt.int32
AF = mybir.ActivationFunctionType
ALU = mybir.AluOpType
AX = mybir.AxisListType

NCORES = 8
T = 4096
NT = 32
DM = 1024
E = 32
CAP = 224
SGM = (128, CAP - 128)
NG = 4
ALPHA = 2.0 ** 0.25
LN_EPS = 1e-5


DBG = {}


class Tok:
    __slots__ = ("w", "r")

    def __init__(self):
        self.w = None
        self.r = {}


class K:
    NDS = 24

    def __init__(self, nc, es):
        self.nc = nc
        self.es = es
        self.eng = {"pe": nc.tensor, "act": nc.scalar, "dve": nc.vector, "pool": nc.gpsimd, "sp": nc.sync}
        self.semh = {}
        for n in self.eng:
            self.semh[n] = es.enter_context(nc.semaphore("s_" + n))
        for i in range(self.NDS):
            self.semh[("d", i)] = es.enter_context(nc.semaphore("d%d" % i))
        self.cnt = {k: 0 for k in self.semh}
        self.seen = {n: {} for n in self.eng}
        self.dnext = 0
        self.dq = {}
        self.nins = 0
        self.pe_last = None

    def _deps(self, reads, writes):
        evs = {}
        for t in reads:
            if t.w is not None and evs.get(t.w[0], 0) < t.w[1]:
                evs[t.w[0]] = t.w[1]
        for t in writes:
            if t.w is not None and evs.get(t.w[0], 0) < t.w[1]:
                evs[t.w[0]] = t.w[1]
            for kk, v in t.r.items():
                if evs.get(kk, 0) < v:
                    evs[kk] = v
        return evs

    def _resolve_pe(self):
        if self.pe_last is not None:
            self.pe_last.then_inc(self.semh["pe"], 1)
            self.cnt["pe"] += 1
            self.pe_last = None

    def _wait(self, en, evs, skip_self=False):
        seen = self.seen[en]
        for kk, v in evs.items():
            if skip_self and kk == en:
                continue
            if seen.get(kk, 0) >= v:
                continue
            if kk == "pe" and v > self.cnt["pe"]:
                self._resolve_pe()
            self.eng[en].wait_ge(self.semh[kk], v)
            seen[kk] = v

    def _record(self, ev, reads, writes):
        kk, v = ev
        for t in reads:
            if t.r.get(kk, 0) < v:
                t.r[kk] = v
        for t in writes:
            t.w = ev
            t.r = {}

    def op(self, en, fn, reads=(), writes=(), self_ok=False):
        evs = self._deps(reads, writes)
        self._wait(en, evs, skip_self=(en == "pe" or self_ok))
        ins = fn(self.eng[en])
        if en == "pe" and not DBG.get("eager_pe"):
            self.pe_last = ins
            self._record((en, self.cnt[en] + 1), reads, writes)
        else:
            self.cnt[en] += 1
            ins.then_inc(self.semh[en], 1)
            self._record((en, self.cnt[en]), reads, writes)
        self.nins += 1

    def dma(self, q, fn, reads=(), writes=()):
        if q == "pool" and DBG.get("simsem"):
            key = ("x", len(self.semh))
            self.semh[key] = self.es.enter_context(self.nc.semaphore("x%d" % len(self.semh)))
            self.cnt[key] = 0
        else:
            lo_, hi_ = (0, 8) if q == "pool" else (8, self.NDS)
            i = self.dq.get(q, lo_)
            self.dq[q] = lo_ + (i + 1 - lo_) % (hi_ - lo_)
            key = ("d", i)
        evs = self._deps(reads, writes)
        if self.cnt[key] > 0 and evs.get(key, 0) < self.cnt[key]:
            evs[key] = self.cnt[key]
        self._wait(q, evs)
        ins = fn(self.eng[q])
        self.cnt[key] += 16
        ins.then_inc(self.semh[key], 16)
        self._record((key, self.cnt[key]), reads, writes)
        self.nins += 1

    def barrier(self):
        self._resolve_pe()
        snap = dict(self.cnt)
        for en in self.eng:
            self._wait(en, {kk: v for kk, v in snap.items() if v > 0 and kk != en})

    def finish(self, q="sp"):
        self._resolve_pe()
        for key in list(self.semh):
            if isinstance(key, tuple) and self.cnt[key] > 0:
                self.eng[q].wait_ge(self.semh[key], self.cnt[key])
        for n in ("pe", "act", "dve", "pool"):
            if self.cnt[n] > 0:
                self.eng[q].wait_ge(self.semh[n], self.cnt[n])


def dram_bcast(handle, offset, n, parts=128):
    return bass.AP(handle, offset, [[0, parts], [1, n]])


def moe_phase(nc, k, D):
    x1 = D["x1"]
    out = D["out"]
    yscr = D["yscr"]
    C = D["consts"]
    tk = {}

    def tok(name):
        if name not in tk:
            tk[name] = Tok()
        return tk[name]

    es = ExitStack()
    with es:
        def sbp(name, shape, dt):
            return es.enter_context(nc.sbuf_tensor(name, shape, dt))

        x1b = sbp("x1b", [128, NT, DM], BF16)
        maskf = sbp("maskf", [128, NT, E], F32)
        gates = sbp("gates", [128, NT, E], F32)
        pos = sbp("pos", [128, NT, E], F32)
        ghl = sbp("ghl", [128, NT, E, 2], BF16)
        idx4 = sbp("idx4", [128, NT, 4], I32)
        ident = sbp("identf", [128, 128], F32)
        iot = sbp("iot", [128, CAP], F32)
        bgu = sbp("bgu", [128, E, 16], F32)
        tc_ = tok("const")
        k.dma("sp", lambda e: e.dma_start(out=ident[:], in_=C["identf"]), writes=[tc_])
        k.dma("sp", lambda e: e.dma_start(out=iot[:], in_=C["iota"]), writes=[tc_])
        k.dma("sp", lambda e: e.dma_start(out=bgu[:], in_=D["b_gate_up"]), writes=[tc_])
        x1v = x1.rearrange("(j p) d -> p j d", p=128)
        for q4 in range(4):
            k.dma("pool", lambda e: e.dma_start(out=x1b[:, q4 * 8:(q4 + 1) * 8, :], in_=x1v[:, q4 * 8:(q4 + 1) * 8, :]),
                  reads=[tok("x1dram")], writes=[tok("x1b%d" % q4)])

        with ExitStack() as es2:
            def sb(name, shape, dt):
                return es2.enter_context(nc.sbuf_tensor("r_" + name, shape, dt))

            def ps(name):
                return es2.enter_context(nc.psum_tensor("r_" + name, [128, 512], F32))
            wr = sb("wr", [128, 8, E], F32)
            brt = sb("brt", [128, E], F32)
            logits = sb("logits", [128, NT, E], F32)
            maskb = sb("maskb", [128, NT, E], BF16)
            rid = sb("rid", [128, NT, E], F32)
            top8 = sb("top8", [128, NT, 8], F32)
            top8r = sb("top8r", [128, NT, 8], F32)
            sm = sb("sm", [128, NT, 4], F32)
            tri = sb("tri", [128, 128], BF16)
            ones = sb("onesb", [128, 128], BF16)
            rowbase = sb("rowbase", [128, NT, E], F32)
            xt = [sb("xt%d" % i, [128, DM], F32) for i in range(2)]
            xT = [sb("xT%d" % i, [128, 8 * 128], F32) for i in range(2)]
            psA = [ps("psA%d" % i) for i in range(2)]
            psP = ps("psP")
            psS = ps("psS")
            k.dma("sp", lambda e: e.dma_start(out=rowbase[:], in_=C["rowbase"]), writes=[tc_])
            k.dma("sp", lambda e: e.dma_start(out=tri[:], in_=C["tri"]), writes=[tc_])
            k.dma("sp", lambda e: e.dma_start(out=ones[:], in_=C["ones"]), writes=[tc_])
            k.dma("sp", lambda e: e.dma_start(out=wr[:], in_=D["w_router"]), writes=[tc_])
            k.dma("sp", lambda e: e.dma_start(out=brt[:], in_=D["b_router"]), writes=[tc_])

            lgt = [tok("lg%d" % j) for j in range(NT)]
            for j in range(NT):
                b = j % 2
                txt, txT = tok("xt%d" % b), tok("xT%d" % b)
                k.dma("sp", lambda e: e.dma_start(out=xt[b][:], in_=x1[j * 128:(j + 1) * 128, :]),
                      reads=[tok("x1dram")], writes=[txt])
                for c in range(8):
                    k.op("pe", lambda e: e.transpose(psA[c // 4][:, (c % 4) * 128:(c % 4 + 1) * 128],
                                                     xt[b][:, c * 128:(c + 1) * 128], ident[:]),
                         reads=[txt, tc_], writes=[tok("psA%d" % (c // 4))])
                k.op("act", lambda e: e.copy(out=xT[b][:, 0:512], in_=psA[0][:]), reads=[tok("psA0")], writes=[txT])
                k.op("act", lambda e: e.copy(out=xT[b][:, 512:1024], in_=psA[1][:]), reads=[tok("psA1")], writes=[txT])
                pp = [psP, psS][j % 2]
                for c in range(8):
                    k.op("pe", lambda e: e.matmul(pp[:, 0:E], xT[b][:, c * 128:(c + 1) * 128], wr[:, c, :],
                                                  start=(c == 0), stop=(c == 7)),
                         reads=[txT, tc_], writes=[tok(pp.name)])
                k.op("dve", lambda e: e.tensor_tensor(out=logits[:, j, :], in0=pp[:, 0:E], in1=brt[:], op=ALU.add),
                     reads=[tok(pp.name), tc_], writes=[lgt[j]])
                k.op("dve", lambda e: e.max(out=top8[:, j, :], in_=logits[:, j, :]), reads=[lgt[j]], writes=[lgt[j]])
            rt = tok("rtall")
            for j in range(NT):
                tk["rt%d" % j] = rt

            def fl(t_):
                return t_[:].rearrange("p a b -> p (a b)")
            k.op("dve", lambda e: e.tensor_tensor(out=maskf[:], in0=logits[:], in1=bc_in(top8[:, :, 3], E), op=ALU.is_ge),
                 reads=lgt, writes=[rt])
            k.op("dve", lambda e: e.tensor_tensor(out=gates[:], in0=logits[:], in1=bc_in(top8[:, :, 0], E), op=ALU.subtract),
                 reads=lgt, writes=[rt])
            k.op("act", lambda e: e.activation(out=fl(gates), in_=fl(gates), func=AF.Exp), reads=[rt], writes=[rt])
            k.op("dve", lambda e: e.tensor_tensor(out=fl(gates), in0=fl(gates), in1=fl(maskf), op=ALU.mult), reads=[rt], writes=[rt])
            k.op("dve", lambda e: e.tensor_reduce(out=sm[:, :, 1], in_=gates[:], axis=AX.X, op=ALU.add), reads=[rt], writes=[rt])
            k.op("dve", lambda e: e.reciprocal(out=sm[:, :, 2], in_=sm[:, :, 1]), reads=[rt], writes=[rt])
            k.op("dve", lambda e: e.tensor_tensor(out=gates[:], in0=gates[:], in1=bc_in(sm[:, :, 2], E), op=ALU.mult), reads=[rt], writes=[rt])
            k.op("dve", lambda e: e.tensor_copy(out=ghl[:, :, :, 0], in_=gates[:]), reads=[rt], writes=[rt])
            k.op("dve", lambda e: e.tensor_tensor(out=ghl[:, :, :, 1], in0=gates[:], in1=ghl[:, :, :, 0], op=ALU.subtract), reads=[rt], writes=[rt])
            k.op("dve", lambda e: e.tensor_copy(out=fl(maskb), in_=fl(maskf)), reads=[rt], writes=[rt])

            pst = [tok("pos%d" % j) for j in range(NT)]
            for j in range(NT):
                g = j // 8
                pp = [psP, psS][j % 2]
                first = True
                for i in range(g * 8, j):
                    k.op("pe", lambda e: e.matmul(pp[:, 0:E], ones[:], maskb[:, i, :], start=first, stop=False),
                         reads=[rt, tc_], writes=[tok(pp.name)])
                    first = False
                k.op("pe", lambda e: e.matmul(pp[:, 0:E], tri[:], maskb[:, j, :], start=first, stop=True),
                     reads=[rt, tc_], writes=[tok(pp.name)])
                k.op("act", lambda e: e.copy(out=pos[:, j, :], in_=pp[:, 0:E]), reads=[tok(pp.name)], writes=[pst[j]])
            k.op("dve", lambda e: e.tensor_tensor(out=fl(rid), in0=fl(pos), in1=fl(rowbase), op=ALU.add), reads=pst + [tc_], writes=[rt])
            k.op("dve", lambda e: e.tensor_tensor(out=fl(rid), in0=fl(rid), in1=fl(maskf), op=ALU.mult), reads=[rt], writes=[rt])
            rdt = [tok("rid%d" % j) for j in range(NT)]
            for j in range(NT):
                k.op("dve", lambda e: e.max(out=top8r[:, j, :], in_=rid[:, j, :]), reads=[rt], writes=[rdt[j]])
            k.op("dve", lambda e: e.tensor_scalar(out=idx4[:], in0=top8r[:, :, 0:4], scalar1=-1.0, scalar2=None, op0=ALU.add),
                 reads=rdt + [rt], writes=[rt])
            k.barrier()

        with ExitStack() as es2:
            def sb(name, shape, dt):
                return es2.enter_context(nc.sbuf_tensor("x_" + name, shape, dt))

            def ps(name):
                return es2.enter_context(nc.psum_tensor("x_" + name, [128, 512], F32))
            wgu = [sb("wgu%d" % i, [128, 8, 2048], BF16) for i in range(2)]
            wd = [sb("wd%d" % i, [128, 8, DM], BF16) for i in range(2)]
            sel = [sb("sel%d" % i, [128, 8, CAP], BF16) for i in range(2)]
            xeT = [sb("xeT%d" % i, [128, 8 * CAP], BF16) for i in range(2)]
            aT = sb("aT", [128, 8 * CAP], BF16)
            gc = [sb("gc%d" % i, [128, CAP], F32) for i in range(1)]
            sg_ = [sb("sg%d" % i, [128, CAP], F32) for i in range(1)]
            uc = [sb("uc%d" % i, [128, CAP], F32) for i in range(1)]
            gsl = [sb("gsl%d" % i, [128, 2], F32) for i in range(2)]
            ysb = [sb("ysb%d" % i, [128, 512], F32) for i in range(2)]
            psA = [ps("psA%d" % i) for i in range(2)]
            psU = [ps("psU%d" % i) for i in range(2)]
            psD = [ps("psD%d" % i) for i in range(2)]
            psS = ps("psS")
            for n in ("psA0", "psA1", "psS", "psP"):
                tk.pop(n, None)

            def load_wgu(e_, c0=0, c1=8):
                s = e_ % 2
                src_ = D["w_gate_up"][e_].rearrange("(c p) f -> p c f", p=128)
                k.dma("pool", lambda e: e.dma_start(out=wgu[s][:, c0:c1, :], in_=src_[:, c0:c1, :]),
                      writes=[tok("wgu%d" % s)])

            def load_wd(e_, c0=0, c1=8):
                s = e_ % 2
                src_ = D["w_down"][e_].rearrange("(c p) f -> p c f", p=128)
                k.dma("pool", lambda e: e.dma_start(out=wd[s][:, c0:c1, :], in_=src_[:, c0:c1, :]),
                      writes=[tok("wd%d" % s)])

            def build_sel(e_, g, sb_):
                for jj in range(8):
                    j = g * 8 + jj
                    k.op("dve", lambda e: e.tensor_scalar(out=sel[sb_][:, jj, :], in0=iot[:], scalar1=pos[:, j, e_:e_ + 1],
                                                          scalar2=maskf[:, j, e_:e_ + 1], op0=ALU.is_equal, op1=ALU.mult),
                         reads=[tok("rt%d" % j), tc_], writes=[tok("sel%d" % sb_)])

            items = [(e_, g) for e_ in range(E) for g in range(NG)]

            def gather(it):
                e_, g = items[it]
                sb_ = it % 2
                tsel, txe = tok("sel%d" % sb_), tok("xeT%d" % sb_)
                for c2 in range(4):
                    pb = c2 % 2
                    for h in range(2):
                        c = 2 * c2 + h
                        for jj in range(8):
                            j = g * 8 + jj
                            k.op("pe", lambda e: e.matmul(psA[pb][:, h * CAP:(h + 1) * CAP],
                                                          x1b[:, j, c * 128:(c + 1) * 128], sel[sb_][:, jj, :],
                                                          start=(jj == 0), stop=(jj == 7)),
                                 reads=[tok("x1b%d" % g), tsel], writes=[tok("psA%d" % pb)])
                    if c2 % 2 == 0:
                        k.op("act", lambda e: e.copy(out=xeT[sb_][:, c2 * 2 * CAP:(c2 + 1) * 2 * CAP], in_=psA[pb][:, 0:2 * CAP]),
                             reads=[tok("psA%d" % pb)], writes=[txe])
                    else:
                        k.op("dve", lambda e: e.tensor_copy(out=xeT[sb_][:, c2 * 2 * CAP:(c2 + 1) * 2 * CAP], in_=psA[pb][:, 0:2 * CAP]),
                             reads=[tok("psA%d" % pb)], writes=[txe])
                for sg in range(2):
                    for jj in range(8):
                        j = g * 8 + jj
                        k.op("pe", lambda e: e.matmul(psS[0:SGM[sg], sb_ * 4 + sg * 2:sb_ * 4 + sg * 2 + 2], sel[sb_][:, jj, sg * 128:sg * 128 + SGM[sg]],
                                                      ghl[:, j, e_, :], start=(jj == 0), stop=(jj == 7)),
                             reads=[tsel, tok("rt%d" % j)], writes=[tok("psS")])
                tgs = tok("gsl%d" % sb_)
                for sg in range(2):
                    k.op("dve", lambda e: e.tensor_reduce(out=gsl[sb_][0:SGM[sg], sg:sg + 1], in_=psS[0:SGM[sg], sb_ * 4 + sg * 2:sb_ * 4 + sg * 2 + 2],
                                                          axis=AX.X, op=ALU.add),
                         reads=[tok("psS")], writes=[tgs])

            def gate_up(it):
                e_, g = items[it]
                s = e_ % 2
                sb_ = it % 2
                twgu, txe, tat = tok("wgu%d" % s), tok("xeT%d" % sb_), tok("aT")
                for kk in range(8):
                    pb = kk % 2
                    tb = 0
                    for half in range(2):
                        for c in range(8):
                            k.op("pe", lambda e: e.matmul(psU[pb][:, half * CAP:(half + 1) * CAP],
                                                          wgu[s][:, c, half * 1024 + kk * 128: half * 1024 + (kk + 1) * 128],
                                                          xeT[sb_][:, c * CAP:(c + 1) * CAP], start=(c == 0), stop=(c == 7)),
                                 reads=[twgu, txe], writes=[tok("psU%d" % pb)])
                    tg, ts_, tu = tok("gc%d" % tb), tok("sg%d" % tb), tok("uc%d" % tb)
                    k.op("dve", lambda e: e.tensor_scalar(out=gc[tb][:], in0=psU[pb][:, 0:CAP], scalar1=bgu[:, e_, kk:kk + 1],
                                                          scalar2=7.0, op0=ALU.add, op1=ALU.min),
                         reads=[tok("psU%d" % pb), tc_], writes=[tg])
                    k.op("act", lambda e: e.activation(out=sg_[tb][:], in_=gc[tb][:], func=AF.Sigmoid, scale=1.702),
                         reads=[tg], writes=[ts_])
                    k.op("act", lambda e: e.activation(out=uc[tb][:], in_=psU[pb][:, CAP:2 * CAP], func=AF.Identity,
                                                       bias=bgu[:, e_, 8 + kk:9 + kk], scale=1.0),
                         reads=[tok("psU%d" % pb), tc_], writes=[tu])
                    k.op("dve", lambda e: e.tensor_scalar(out=uc[tb][:], in0=uc[tb][:], scalar1=7.0, scalar2=-7.0,
                                                          op0=ALU.min, op1=ALU.max), reads=[tu], writes=[tu])
                    k.op("dve", lambda e: e.tensor_tensor(out=sg_[tb][:], in0=gc[tb][:], in1=sg_[tb][:], op=ALU.mult),
                         reads=[tg, ts_], writes=[ts_])
                    k.op("dve", lambda e: e.scalar_tensor_tensor(out=aT[:, kk * CAP:(kk + 1) * CAP], in0=uc[tb][:],
                                                                 scalar=1.0, in1=sg_[tb][:], op0=ALU.add, op1=ALU.mult),
                         reads=[tu, ts_], writes=[tat])

            def down(it):
                e_, g = items[it]
                s = e_ % 2
                sb_ = it % 2
                twd, tat, tgs = tok("wd%d" % s), tok("aT"), tok("gsl%d" % sb_)
                for sg in range(2):
                    for half in range(2):
                        pb = half
                        tys = tok("ysb%d" % half)
                        for kk in range(8):
                            k.op("pe", lambda e: e.matmul(psD[pb][0:SGM[sg], :], aT[:, kk * CAP + sg * 128: kk * CAP + sg * 128 + SGM[sg]],
                                                          wd[s][:, kk, half * 512:(half + 1) * 512], start=(kk == 0), stop=(kk == 7)),
                                 reads=[tat, twd], writes=[tok("psD%d" % pb)])
                        k.op("act", lambda e: e.activation(out=ysb[half][0:SGM[sg], :], in_=psD[pb][0:SGM[sg], :],
                                                           func=AF.Copy, scale=gsl[sb_][0:SGM[sg], sg:sg + 1]),
                             reads=[tok("psD%d" % pb), tgs], writes=[tys])
                        row0 = e_ * (NG * CAP) + g * CAP + sg * 128
                        k.dma("sp", lambda e: e.dma_start(out=yscr[row0:row0 + SGM[sg], half * 512:(half + 1) * 512], in_=ysb[half][0:SGM[sg], :]),
                              reads=[tys], writes=[Tok()])

            for e0 in range(2):
                load_wgu(e0)
                load_wd(e0)
            build_sel(0, 0, 0)
            gather(0)
            for it, (e_, g) in enumerate(items):
                if it + 1 < len(items):
                    build_sel(items[it + 1][0], items[it + 1][1], (it + 1) % 2)
                gate_up(it)
                if e_ >= 1 and e_ + 1 < E:
                    load_wgu(e_ + 1, 2 * g, 2 * g + 2)
                if it + 1 < len(items):
                    gather(it + 1)
                down(it)
                if e_ >= 1 and e_ + 1 < E:
                    load_wd(e_ + 1, 2 * g, 2 * g + 2)
            k.barrier()

        with ExitStack() as es2:
            def sb(name, shape, dt):
                return es2.enter_context(nc.sbuf_tensor("c_" + name, shape, dt))

            def ps(name):
                return es2.enter_context(nc.psum_tensor("c_" + name, [128, 512], F32))
            for n in list(tk):
                if n.startswith("ps"):
                    tk.pop(n)
            lng = sb("lng", [128, DM], F32)
            lnb = sb("lnb", [128, DM], F32)
            bdn = sb("bdn", [E, DM], F32)
            xt = [sb("xt%d" % i, [128, DM], F32) for i in range(2)]
            acc = [sb("acc%d" % i, [128, DM], F32) for i in range(2)]
            gtt = [sb("gtt%d" % i, [128, 4, DM], F32) for i in range(2)]
            gT = [sb("gT%d" % i, [E, 128], F32) for i in range(2)]
            sq = sb("sq", [128, DM], F32)
            st = sb("st", [128, NT, 8], F32)
            psT = ps("psT")
            psB = [ps("psB%d" % i) for i in range(2)]
            tk.pop("xt0", None)
            tk.pop("xt1", None)
            k.dma("sp", lambda e: e.dma_start(out=lng[:], in_=D["ln2_g"]), writes=[tc_])
            k.dma("sp", lambda e: e.dma_start(out=lnb[:], in_=D["ln2_b"]), writes=[tc_])
            k.dma("sp", lambda e: e.dma_start(out=bdn[:], in_=D["b_down"]), writes=[tc_])
            nrows = E * NG * CAP
            def A(j):
                b = j % 2
                tgq, txt, tac = [tok("gtt%d_%d" % (b, q_)) for q_ in range(4)], tok("xt%d" % b), tok("acc%d" % b)
                trt = tok("rt%d" % j)
                for q in range(4):
                    k.dma("pool", lambda e: e.indirect_dma_start(
                        out=gtt[b][:, q, :], out_offset=None, in_=yscr[:, :],
                        in_offset=bass.IndirectOffsetOnAxis(ap=idx4[:, j, q:q + 1], axis=0)),
                        reads=[trt, tok("yscr")], writes=[tgq[q]])
                k.dma("sp", lambda e: e.dma_start(out=xt[b][:], in_=x1[j * 128:(j + 1) * 128, :]),
                      reads=[tok("x1dram")], writes=[txt])
                tgT = tok("gT%d" % b)
                k.op("pe", lambda e: e.transpose(psT[0:E, 0:128], gates[:, j, :], ident[:]), reads=[trt, tc_], writes=[tok("psT")])
                k.op("act", lambda e: e.copy(out=gT[b][:], in_=psT[0:E, 0:128]), reads=[tok("psT")], writes=[tgT])
                for half in range(2):
                    k.op("pe", lambda e: e.matmul(psB[half][:], gT[b][:], bdn[:, half * 512:(half + 1) * 512], start=True, stop=False),
                         reads=[tgT, tc_], writes=[tok("psB%d" % half)])
                    for q in range(4):
                        k.op("pe", lambda e: e.matmul(psB[half][:], ident[:], gtt[b][:, q, half * 512:(half + 1) * 512], start=False, stop=(q == 3)),
                             reads=[tgq[q], tc_], writes=[tok("psB%d" % half)])
                yield
                for half in range(2):
                    k.op("dve", lambda e: e.scalar_tensor_tensor(out=acc[b][:, half * 512:(half + 1) * 512], in0=xt[b][:, half * 512:(half + 1) * 512],
                                                                 scalar=float(ALPHA), in1=psB[half][:], op0=ALU.mult, op1=ALU.add),
                         reads=[txt, tok("psB%d" % half)], writes=[tac])
                    yield

            def L(j):
                b = j % 2
                a_, ta, tst = acc[b], tok("acc%d" % b), tok("st%d" % j)
                k.op("dve", lambda e: e.reduce_sum(out=st[:, j, 0:1], in_=a_[:], axis=AX.X), reads=[ta], writes=[tst])
                k.op("dve", lambda e: e.tensor_scalar(out=st[:, j, 1:2], in0=st[:, j, 0:1], scalar1=-1.0 / DM, scalar2=None,
                                                      op0=ALU.mult), reads=[tst], writes=[tst])
                k.op("act", lambda e: e.activation(out=a_[:], in_=a_[:], func=AF.Identity, bias=st[:, j, 1:2], scale=1.0),
                     reads=[tst, ta], writes=[ta])
                k.op("act", lambda e: e.activation(out=sq[:], in_=a_[:], func=AF.Square, accum_out=st[:, j, 2:3]),
                     reads=[ta, tst], writes=[tok("sq"), tst])
                yield
                k.op("dve", lambda e: e.tensor_scalar(out=st[:, j, 3:4], in0=st[:, j, 2:3], scalar1=1.0 / DM, scalar2=LN_EPS,
                                                      op0=ALU.mult, op1=ALU.add), reads=[tst], writes=[tst])
                k.op("act", lambda e: e.activation(out=st[:, j, 4:5], in_=st[:, j, 3:4], func=AF.Sqrt), reads=[tst], writes=[tst])
                yield
                k.op("dve", lambda e: e.reciprocal(out=st[:, j, 5:6], in_=st[:, j, 4:5]), reads=[tst], writes=[tst])
                k.op("dve", lambda e: e.scalar_tensor_tensor(out=a_[:], in0=a_[:], scalar=st[:, j, 5:6], in1=lng[:],
                                                             op0=ALU.mult, op1=ALU.mult), reads=[tst, ta, tc_], writes=[ta])
                k.op("dve", lambda e: e.tensor_tensor(out=a_[:], in0=a_[:], in1=lnb[:], op=ALU.add), reads=[ta, tc_], writes=[ta])
                k.dma("sp", lambda e: e.dma_start(out=out[j * 128:(j + 1) * 128, :], in_=a_[:]),
                      reads=[ta], writes=[Tok()])
                yield

            for _ in A(0):
                pass
            for j in range(NT):
                gl = L(j)
                ga = A(j + 1) if j + 1 < NT else None
                while gl is not None or ga is not None:
                    if ga is not None and next(ga, "done") == "done":
                        ga = None
                    if gl is not None and next(gl, "done") == "done":
                        gl = None
            k.barrier()


def layer_norm_tile(k, a, ta, sq, tsq, st, j, tst, g_bc, b_bc, tc_):
    k.op("dve", lambda e: e.reduce_sum(out=st[:, j, 0:1], in_=a[:], axis=AX.X), reads=[ta], writes=[tst])
    k.op("dve", lambda e: e.tensor_scalar(out=st[:, j, 1:2], in0=st[:, j, 0:1], scalar1=-1.0 / DM, scalar2=None,
                                          op0=ALU.mult), reads=[tst], writes=[tst])
    k.op("act", lambda e: e.activation(out=a[:], in_=a[:], func=AF.Identity, bias=st[:, j, 1:2], scale=1.0),
         reads=[tst, ta], writes=[ta])
    k.op("act", lambda e: e.activation(out=sq[:], in_=a[:], func=AF.Square, accum_out=st[:, j, 2:3]), reads=[ta, tst], writes=[tsq, tst])
    k.op("dve", lambda e: e.tensor_scalar(out=st[:, j, 3:4], in0=st[:, j, 2:3], scalar1=1.0 / DM, scalar2=LN_EPS,
                                          op0=ALU.mult, op1=ALU.add), reads=[tst], writes=[tst])
    k.op("act", lambda e: e.activation(out=st[:, j, 4:5], in_=st[:, j, 3:4], func=AF.Sqrt), reads=[tst], writes=[tst])
    k.op("dve", lambda e: e.reciprocal(out=st[:, j, 5:6], in_=st[:, j, 4:5]), reads=[tst], writes=[tst])
    k.op("dve", lambda e: e.scalar_tensor_tensor(out=a[:], in0=a[:], scalar=st[:, j, 5:6], in1=g_bc[:],
                                                 op0=ALU.mult, op1=ALU.mult), reads=[tst, ta, tc_], writes=[ta])
    k.op("dve", lambda e: e.tensor_tensor(out=a[:], in0=a[:], in1=b_bc[:], op=ALU.add), reads=[ta, tc_], writes=[ta])


NIT = 20
A_QI, A_KI, A_VA, A_WI, A_COLS = 1024, 1536, 1664, 2176, 2184
B_GA, B_GM, B_VM, B_O, B_IF, B_COLS = 512, 1536, 2560, 3072, 3584, 3592


def bc_mid(x, n):
    return bass.AP(x.tensor, x.offset, [list(x.ap[0]), [0, n], list(x.ap[1])])


def bc_in(x, n):
    return bass.AP(x.tensor, x.offset, [list(x.ap[0]), list(x.ap[1]), [0, n]])


def mixer_phase(nc, k, D, dbg=None):
    C = D["consts"]
    tk = {}

    def tok(name):
        if name not in tk:
            tk[name] = Tok()
        return tk[name]

    tc_ = tok("const")
    xTd = D["xT"].rearrange("(c p) t -> p c t", p=128)
    with ExitStack() as es:
        identb = es.enter_context(nc.sbuf_tensor("identb", [128, 128], BF16))
        causT = es.enter_context(nc.sbuf_tensor("causT", [128, 128], BF16))
        k.dma("sp", lambda e: e.dma_start(out=identb[:], in_=C["identb"]), writes=[tc_])
        k.dma("sp", lambda e: e.dma_start(out=causT[:], in_=C["causT"]), writes=[tc_])

        with ExitStack() as es2:
            def sb(name, shape, dt):
                return es2.enter_context(nc.sbuf_tensor("a_" + name, shape, dt))

            def ps(name, dt=F32):
                return es2.enter_context(nc.psum_tensor("a_" + name, [128, 512] if dt == F32 else [128, 1024], dt))
            winA = sb("winA", [128, 8, A_COLS], BF16)
            kaT = sb("kaT", [128, 4, T], BF16)
            vaug = sb("vaug", [128, NT, 8, 66], BF16)
            kiT2 = sb("kiT2", [128, T], BF16)
            scores = [sb("scores%d" % i, [128, T], F32) for i in range(2)]
            mbuf = sb("mbuf", [128, T], BF16)
            maskT = sb("maskT", [128, NT, 128], BF16)
            onesb = sb("onesb", [128, 128], BF16)
            negmask = sb("negmask", [128, 128], F32)
            ebx = [sb("ebx%d" % i, [128, 8, 128], BF16) for i in range(2)]
            ebf = sb("ebf", [128, 8, 128], F32)
            cfx = sb("cfx", [128, 8], F32)
            ftab = sb("ftab", [128, NIT + 1], F32)
            xTa = [sb("xTa%d" % i, [128, 8, 128], BF16) for i in range(2)]
            qaT = [sb("qaT%d" % i, [128, 8, 128], BF16) for i in range(3)]
            qiT = sb("qiT", [128, 8, 128], BF16)
            wi = sb("wi", [128, 8], F32)
            diagw = sb("diagw", [128, 8, 128], BF16)
            R = [sb("R%d" % i, [128, 512], BF16) for i in range(2)]
            Eb = [sb("Eb%d" % i, [128, 8, 128], BF16) for i in range(2)]
            PT = [sb("PT%d" % i, [128, 8, 128], BF16) for i in range(2)]
            tmpE = sb("tmpE", [128, 8, 128], BF16)
            oc = sb("oc", [128, 8, 65], F32)
            bs = sb("bs", [128, 8], F32)
            stp = sb("stp", [128, NIT + 1], F32)
            dummy = sb("dummy", [128, 16], F32)
            self_ctr = [0]
            rden = sb("rden", [128, 8], F32)
            yat = [sb("yat%d" % i, [128, 512], BF16) for i in range(2)]
            psOf = [ps("psOf%d" % i) for i in range(2)]
            psOn = [ps("psOn%d" % i) for i in range(2)]
            Rk = [ps("K%d" % i) for i in range(3)]
            psT = ps("psT", BF16)
            if DBG.get("verbose"):
                print("sweepA sbuf remaining", nc.sbuf_bytes_remaining)

            for cb_, (c0, c1) in enumerate(((0, A_VA), (A_VA, A_COLS))):
                k.dma("pool", lambda e: e.dma_start(out=winA[:, :, c0:c1],
                                                    in_=D["winA"].rearrange("(c p) f -> p c f", p=128)[:, :, c0:c1]),
                      writes=[tok("winA")])
            k.dma("sp", lambda e: e.dma_start(out=onesb[:], in_=C["ones"]), writes=[tc_])
            k.dma("sp", lambda e: e.dma_start(out=negmask[:], in_=C["negmask"]), writes=[tc_])
            k.dma("sp", lambda e: e.dma_start(out=ftab[:], in_=C["ftab"]), writes=[tc_])
            k.dma("sp", lambda e: e.dma_start(out=cfx[:], in_=D["cfar"]), writes=[tc_])
            k.op("act", lambda e: e.activation(out=cfx[:], in_=cfx[:], func=AF.Exp), reads=[tc_], writes=[tc_])
            for i, nm in enumerate(("ebd", "ebp")):
                k.dma("sp", lambda e: e.dma_start(out=ebf[:], in_=D[nm]), writes=[tok("ebf")])
                k.op("act", lambda e: e.activation(out=ebx[i][:], in_=ebf[:], func=AF.Exp), reads=[tok("ebf")], writes=[tc_])
            for i in range(3):
                k.op("dve", lambda e: e.memset(qaT[i][:], 0.0), writes=[tok("qaT%d" % i)])
            k.op("dve", lambda e: e.memset(qiT[:], 0.0), writes=[tok("qiT")])
            tmk = tok("maskT")

            def S1(j):
                b = j % 2
                n = (j + 1) * 128
                txa = tok("xTa%d" % b)
                q3 = j % 3
                k.dma("pool", lambda e: e.dma_start(out=xTa[b][:], in_=xTd[:, :, j * 128:(j + 1) * 128]), writes=[txa])

                def fm_group(bank, chunk0, nch):
                    bv = bank[:, :].rearrange("p (a b) -> p a b", a=4)
                    for cc in range(nch):
                        for c in range(8):
                            k.op("pe", lambda e: e.matmul(bv[:, cc, :], winA[:, c, (chunk0 + cc) * 128:(chunk0 + cc + 1) * 128],
                                                          xTa[b][:, c, :], start=(c == 0), stop=(c == 7)),
                                 reads=[tok("winA"), txa], writes=[tok(bank.name)])
                    return bv
                bv = fm_group(Rk[0], 0, 4)
                k.op("act", lambda e: e.copy(out=qaT[q3][0:64, 0:8:2, :], in_=bv[0:64, :, :]), reads=[tok(Rk[0].name)], writes=[tok("qaT%d" % q3)])
                k.op("act", lambda e: e.copy(out=qaT[q3][64:128, 1:8:2, :], in_=bv[64:128, :, :]), reads=[tok(Rk[0].name)], writes=[tok("qaT%d" % q3)])
                bv = fm_group(Rk[1], 4, 4)
                k.op("act", lambda e: e.copy(out=kaT[:, :, j * 128:(j + 1) * 128], in_=bv[:, :, :]),
                     reads=[tok(Rk[1].name)], writes=[tok("kaT%d" % j)])
                bv = fm_group(Rk[2], 8, 4)
                k.op("act", lambda e: e.copy(out=qiT[0:64, 0:8:2, :], in_=bv[0:64, :, :]), reads=[tok(Rk[2].name)], writes=[tok("qiT")])
                k.op("act", lambda e: e.copy(out=qiT[64:128, 1:8:2, :], in_=bv[64:128, :, :]), reads=[tok(Rk[2].name)], writes=[tok("qiT")])
                bv = fm_group(Rk[0], 12, 1)
                k.op("act", lambda e: e.copy(out=kiT2[:, j * 128:(j + 1) * 128], in_=bv[:, 0, :]),
                     reads=[tok(Rk[0].name)], writes=[tok("kiT%d" % j)])
                for c in range(8):
                    k.op("pe", lambda e: e.matmul(Rk[1][:, :], xTa[b][:, c, :], winA[:, c, A_VA:A_WI], start=(c == 0), stop=(c == 7)),
                         reads=[tok("winA"), txa], writes=[tok(Rk[1].name)])
                k.op("pool", lambda e: e.memset(vaug[:, j, :, 64:66], 1.0), writes=[tok("vaug%d" % j)])
                k.op("act", lambda e: e.copy(out=vaug[:, j, :, 0:64], in_=Rk[1][:, :].rearrange("p (a b) -> p a b", a=8)),
                     reads=[tok(Rk[1].name)], writes=[tok("vaug%d" % j)])
                if j < 2:
                    return
                for c in range(8):
                    k.op("pe", lambda e: e.matmul(Rk[2][:, 0:8], xTa[b][:, c, :], winA[:, c, A_WI:A_COLS], start=(c == 0), stop=(c == 7)),
                         reads=[tok("winA"), txa], writes=[tok(Rk[2].name)])
                k.op("act", lambda e: e.activation(out=wi[:], in_=Rk[2][:, 0:8], func=AF.Copy, scale=float(0.125 * 8 ** -0.5)),
                     reads=[tok(Rk[2].name)], writes=[tok("wi")])
                for h in range(8):
                    k.op("act", lambda e: e.activation(out=diagw[:, h, :], in_=identb[:], func=AF.Copy, scale=wi[:, h:h + 1]),
                         reads=[tok("wi"), tc_], writes=[tok("diagw")])
                tsc = tok("scores%d" % b)
                for blk in range((n + 511) // 512):
                    ncl = min(512, n - blk * 512)
                    kread = [tok("kiT%d" % t_) for t_ in range(blk * 4, min(blk * 4 + 4, j + 1))]
                    def mm2(h):
                        hb = h % 2
                        k.op("pe", lambda e: e.matmul(Rk[2][:, 0:ncl], diagw[:, h, :], R[hb][:, 0:ncl], start=(h == 0), stop=(h == 7)),
                             reads=[tok("diagw"), tok("R%d" % hb)], writes=[tok(Rk[2].name)])
                    for h in range(8):
                        hb = h % 2
                        k.op("pe", lambda e: e.matmul(Rk[hb][:, 0:ncl], qiT[:, h, :],
                                                      kiT2[:, blk * 512:blk * 512 + ncl], start=True, stop=True),
                             reads=[tok("qiT")] + kread, writes=[tok(Rk[hb].name)])
                        k.op("act", lambda e: e.activation(out=R[hb][:, 0:ncl], in_=Rk[hb][:, 0:ncl], func=AF.Relu),
                             reads=[tok(Rk[hb].name)], writes=[tok("R%d" % hb)])
                        if h >= 1:
                            mm2(h - 1)
                    mm2(7)
                    k.op("act", lambda e: e.copy(out=scores[b][:, blk * 512:blk * 512 + ncl], in_=Rk[2][:, 0:ncl]),
                         reads=[tok(Rk[2].name)], writes=[tsc])

            def S2a(j):
                b = j % 2
                n = (j + 1) * 128
                sc = scores[b]
                tsc, tb_ = tok("scores%d" % b), tok("bs")
                k.op("dve", lambda e: e.tensor_reduce(out=bs[:, 0:1], in_=sc[:, 0:n], axis=AX.X, op=ALU.max), reads=[tsc], writes=[tb_])
                k.op("dve", lambda e: e.tensor_reduce(out=bs[:, 1:2], in_=sc[:, 0:256], axis=AX.X, op=ALU.min), reads=[tsc], writes=[tb_])
                k.op("dve", lambda e: e.tensor_tensor(out=bs[:, 2:3], in0=bs[:, 0:1], in1=bs[:, 1:2], op=ALU.subtract), reads=[tb_], writes=[tb_])
                k.op("dve", lambda e: e.tensor_tensor(out=sc[:, j * 128:(j + 1) * 128], in0=sc[:, j * 128:(j + 1) * 128],
                                                      in1=negmask[:], op=ALU.add), reads=[tsc, tc_], writes=[tsc])
                k.op("dve", lambda e: e.tensor_scalar(out=stp[:], in0=ftab[:], scalar1=bs[:, 2:3], scalar2=None, op0=ALU.mult),
                     reads=[tb_, tc_], writes=[tb_])
                k.op("dve", lambda e: e.tensor_tensor(out=bs[:, 3:4], in0=bs[:, 1:2], in1=stp[:, 0:1], op=ALU.add), reads=[tb_], writes=[tb_])
                yield
                nf = DBG.get("fillers", 0)
                so = nf > 0

                def fill():
                    for f_ in range(nf):
                        self_ctr[0] += 1
                        c_ = self_ctr[0] % 16
                        k.op("dve", lambda e: e.memset(dummy[:, c_:c_ + 1], 0.0), self_ok=True)
                for it in range(1, NIT + 1):
                    k.op("dve", lambda e: e.tensor_scalar(out=mbuf[:, 0:n], in0=sc[:, 0:n], scalar1=bs[:, 3:4], scalar2=0.0,
                                                          op0=ALU.is_ge, op1=ALU.add, accum_out=bs[:, 4:5]),
                         reads=[tsc, tb_], writes=[tok("mbuf"), tb_], self_ok=(so and it > 1))
                    fill()
                    k.op("dve", lambda e: e.tensor_scalar(out=bs[:, 5:6], in0=bs[:, 4:5], scalar1=256.0, scalar2=-0.5,
                                                          op0=ALU.is_ge, op1=ALU.add), reads=[tb_], writes=[tb_], self_ok=so)
                    fill()
                    k.op("dve", lambda e: e.scalar_tensor_tensor(out=bs[:, 3:4], in0=stp[:, it:it + 1], scalar=bs[:, 5:6], in1=bs[:, 3:4],
                                                                 op0=ALU.mult, op1=ALU.add), reads=[tb_], writes=[tb_], self_ok=so)
                    fill()
                    yield
                k.op("dve", lambda e: e.scalar_tensor_tensor(out=bs[:, 3:4], in0=stp[:, NIT:NIT + 1], scalar=-0.5, in1=bs[:, 3:4],
                                                             op0=ALU.mult, op1=ALU.add), reads=[tb_], writes=[tb_])
                k.op("dve", lambda e: e.tensor_scalar(out=mbuf[:, 0:n], in0=sc[:, 0:n], scalar1=bs[:, 3:4], scalar2=None,
                                                      op0=ALU.is_ge), reads=[tsc, tb_], writes=[tok("mbuf")])

            def S2b(j):
                for i0 in range(0, j + 1, 8):
                    cnt = min(8, j + 1 - i0)
                    pv = psT[:, :].rearrange("p (a b) -> p a b", a=8)
                    for i in range(i0, i0 + cnt):
                        k.op("pe", lambda e: e.transpose(pv[:, i - i0, :], mbuf[:, i * 128:(i + 1) * 128], identb[:]),
                             reads=[tok("mbuf"), tc_], writes=[tok("psT")])
                    k.op("act", lambda e: e.copy(out=maskT[:, i0:i0 + cnt, :], in_=pv[:, 0:cnt, :]), reads=[tok("psT")], writes=[tmk])

            def S3(j):
                q3 = j % 3

                def qk(i):
                    eb = i % 2
                    for hg in range(2):
                        qb = Rk[(2 * i + hg) % 3]
                        lv = qb[:, :].rearrange("p (a b) -> p a b", a=4)
                        for h4 in range(4):
                            h = hg * 4 + h4
                            k.op("pe", lambda e: e.matmul(lv[:, h4, :], kaT[:, h // 2, i * 128:(i + 1) * 128],
                                                          qaT[q3][:, h, :], start=True, stop=True),
                                 reads=[tok("kaT%d" % i), tok("qaT%d" % q3)], writes=[tok(qb.name)])
                        k.op("act", lambda e: e.activation(out=Eb[eb][:, hg * 4:(hg + 1) * 4, :], in_=lv[:, :, :], func=AF.Exp, scale=0.125),
                             reads=[tok(qb.name)], writes=[tok("Eb%d" % eb)])

                def pv(i):
                    eb = i % 2
                    far = (i <= j - 2)
                    if j >= 2:
                        m_ap, mreads = maskT[:, i, :], [tmk]
                    elif i == j:
                        m_ap, mreads = causT[:], [tc_]
                    else:
                        m_ap, mreads = onesb[:], [tc_]
                    tpt = tok("PT%d" % eb)
                    if far:
                        k.op("dve", lambda e: e.tensor_tensor(out=PT[eb][:], in0=Eb[eb][:], in1=bc_mid(m_ap, 8), op=ALU.mult),
                             reads=[tok("Eb%d" % eb)] + mreads, writes=[tpt])
                    else:
                        kind = 0 if i == j else 1
                        k.op("pool", lambda e: e.tensor_tensor(out=tmpE[:], in0=ebx[kind][:], in1=bc_mid(m_ap, 8), op=ALU.mult),
                             reads=[tc_] + mreads, writes=[tok("tmpE")])
                        k.op("dve", lambda e: e.tensor_tensor(out=PT[eb][:], in0=Eb[eb][:], in1=tmpE[:], op=ALU.mult),
                             reads=[tok("Eb%d" % eb), tok("tmpE")], writes=[tpt])
                    for h in range(8):
                        if far:
                            bank, st_, sp_ = psOf[h // 4], (i == 0 and h % 4 == 0), (i == j - 2 and h % 4 == 3)
                        else:
                            bank, st_, sp_ = psOn[h // 4], (i == max(j - 1, 0) and h % 4 == 0), (i == j and h % 4 == 3)
                        ov = bank[:, 0:260].rearrange("p (a b) -> p a b", a=4)
                        k.op("pe", lambda e: e.matmul(ov[:, h % 4, :], PT[eb][:, h, :], vaug[:, i, h, 0:65], start=st_, stop=sp_),
                             reads=[tpt, tok("vaug%d" % i)], writes=[tok(bank.name)])

                qk(0)
                for i in range(j + 1):
                    if i + 1 <= j:
                        qk(i + 1)
                    pv(i)
                    yield
                toc = tok("oc")
                for hg in range(2):
                    nv = psOn[hg][:, 0:260].rearrange("p (a b) -> p a b", a=4)
                    if j >= 2:
                        fv = psOf[hg][:, 0:260].rearrange("p (a b) -> p a b", a=4)
                        k.op("dve", lambda e: e.tensor_tensor(out=oc[:, hg * 4:(hg + 1) * 4, :], in0=fv[:, :, :],
                                                              in1=bc_in(cfx[:, hg * 4:(hg + 1) * 4], 65), op=ALU.mult),
                             reads=[tok(psOf[hg].name), tc_], writes=[toc])
                        k.op("dve", lambda e: e.tensor_tensor(out=oc[:, hg * 4:(hg + 1) * 4, :], in0=oc[:, hg * 4:(hg + 1) * 4, :],
                                                              in1=nv[:, :, :], op=ALU.add), reads=[tok(psOn[hg].name), toc], writes=[toc])
                    else:
                        k.op("dve", lambda e: e.tensor_copy(out=oc[:, hg * 4:(hg + 1) * 4, :], in_=nv[:, :, :]),
                             reads=[tok(psOn[hg].name)], writes=[toc])
                k.op("dve", lambda e: e.reciprocal(out=rden[:], in_=oc[:, :, 64]), reads=[toc], writes=[tok("rden")])
                yb_ = j % 2
                k.op("dve", lambda e: e.tensor_tensor(out=yat[yb_][:].rearrange("p (a b) -> p a b", a=8), in0=oc[:, :, 0:64],
                                                      in1=bc_in(rden[:], 64), op=ALU.mult),
                     reads=[toc, tok("rden")], writes=[tok("yat%d" % yb_)])
                k.dma("sp", lambda e: e.dma_start(out=D["yatt_scr"][j * 128:(j + 1) * 128, :], in_=yat[yb_][:]),
                      reads=[tok("yat%d" % yb_)], writes=[Tok()])

            def drain(g):
                if g is not None:
                    for _ in g:
                        pass

            ntA = DBG.get("ntA", NT)
            for t in range(-2, ntA):
                ga = S2a(t + 1) if 2 <= t + 1 < ntA else None
                gb = S3(t) if t >= 0 else None
                if ga is not None and gb is not None:
                    ppi = max(1, -(-(t + 1) // 5))
                    next(ga)
                    for it in range(NIT):
                        next(ga)
                        for _ in range(ppi):
                            if next(gb, "done") == "done":
                                gb = None
                                break
                        if gb is None:
                            break
                    drain(ga)
                    drain(gb)
                else:
                    drain(ga)
                    drain(gb)
                if t + 2 < ntA:
                    S1(t + 2)
                if 2 <= t + 1 < ntA:
                    S2b(t + 1)
            k.barrier()

        if DBG.get("noB"):
            return
        with ExitStack() as es2:
            def sb(name, shape, dt):
                return es2.enter_context(nc.sbuf_tensor("b_" + name, shape, dt))

            def ps(name, dt=F32):
                return es2.enter_context(nc.psum_tensor("b_" + name, [128, 512] if dt == F32 else [128, 1024], dt))
            for nme in list(tk):
                if nme.startswith("a_"):
                    tk.pop(nme)
            winB = sb("winB", [128, 8, B_COLS], BF16)
            wba = sb("wba", [128, 4, DM], BF16)
            wbm = sb("wbm", [128, 4, DM], BF16)
            wout = sb("wout", [128, 8, DM], BF16)
            cw = sb("cw", [128, 4, 4], F32)
            cbias = sb("cb", [128, 4], F32)
            ifb = sb("ifb", [128, 8], F32)
            ngb = sb("ngb", [128, 512], F32)
            l1g = sb("l1g", [128, DM], F32)
            l1b = sb("l1b", [128, DM], F32)
            triI = sb("triI", [128, 128], F32)
            onesf = sb("onesf", [128, 128], F32)
            xTa = [sb("xTa%d" % i, [128, 8, 128], BF16) for i in range(2)]
            xt = [sb("xt%d" % i, [128, DM], F32) for i in range(2)]
            qkraw = sb("qkraw", [128, 4, 131], F32)
            cacc = sb("cacc", [128, 4, 128], F32)
            qkm = sb("qkm", [128, 4, 128], BF16)
            qz = sb("qz", [128, 4, 128], BF16)
            sga = sb("sga", [128, 8, 128], F32)
            sgm = sb("sgm", [128, 8, 128], F32)
            vm = sb("vm", [128, 4, 130], BF16)
            so = sb("so", [128, 512], F32)
            gat = sb("gat", [128, 32], F32)
            PTm = sb("PTm", [128, 4, 128], BF16)
            kp2 = sb("kp2", [128, 2, 128], BF16)
            Cst = sb("Cst", [128, 2, 129], F32)
            Cb = sb("Cb", [128, 2, 130], BF16)
            ctmp = sb("ctmp", [128, 129], F32)
            hs = sb("hs", [128, 24], F32)
            hv = sb("hv", [128, 4, 128], F32)
            sq = sb("sq", [128, DM], F32)
            ym = sb("ym", [128, 512], BF16)
            yaT = sb("yaT", [128, 4, 128], BF16)
            ymT = sb("ymT", [128, 4, 128], BF16)
            t1 = sb("t1", [128, 512], F32)
            t2 = sb("t2", [128, 512], F32)
            mixT = sb("mixT", [128, 8, 128], BF16)
            acc = [sb("acc%d" % i, [128, DM], F32) for i in range(2)]
            yab = [sb("yab%d" % i, [128, 512], BF16) for i in range(2)]
            st = sb("st", [128, NT, 8], F32)
            Rb = [ps("K%d" % i) for i in range(7)]
            psTb = ps("psTb", BF16)

            wv = D["winB"].rearrange("(c p) f -> p c f", p=128)
            for (c0, c1) in ((0, 2048), (2048, B_COLS)):
                k.dma("pool", lambda e: e.dma_start(out=winB[:, :, c0:c1], in_=wv[:, :, c0:c1]), writes=[tok("winB")])
            k.dma("pool", lambda e: e.dma_start(out=wba[:], in_=D["wba"].rearrange("(c p) f -> p c f", p=128)), writes=[tc_])
            k.dma("pool", lambda e: e.dma_start(out=wbm[:], in_=D["wbm"].rearrange("(c p) f -> p c f", p=128)), writes=[tc_])
            k.dma("pool", lambda e: e.dma_start(out=wout[:], in_=D["wout"].rearrange("(c p) f -> p c f", p=128)), writes=[tc_])
            for t_, nm in ((cw, "cw"), (cbias, "cb"), (ifb, "ifb"), (ngb, "ng"), (l1g, "ln1_g"), (l1b, "ln1_b")):
                k.dma("sp", lambda e: e.dma_start(out=t_[:], in_=D[nm]), writes=[tc_])
            k.dma("sp", lambda e: e.dma_start(out=triI[:], in_=C["triI"]), writes=[tc_])
            k.dma("sp", lambda e: e.dma_start(out=onesf[:], in_=C["onesf"]), writes=[tc_])
            k.op("dve", lambda e: e.memset(qkraw[:], 0.0), writes=[tok("qkraw")])
            k.op("dve", lambda e: e.memset(vm[:], 1.0), writes=[tok("vm")])
            k.op("dve", lambda e: e.memset(Cst[:], 0.0), writes=[tok("Cst")])
            k.op("dve", lambda e: e.memset(Cb[:], 0.0), writes=[tok("Cb")])
            k.op("dve", lambda e: e.memset(qz[:], 0.0), writes=[tok("qkm")])

            def nm(t_):
                return tok(t_.name)

            for j in range(DBG.get("ntB", NT)):
                b = j % 2
                txa, txt = tok("bxTa%d" % b), tok("bxt%d" % b)
                k.dma("pool", lambda e: e.dma_start(out=xTa[b][:], in_=xTd[:, :, j * 128:(j + 1) * 128]), writes=[txa])
                k.dma("sp", lambda e: e.dma_start(out=xt[b][:], in_=D["x"][j * 128:(j + 1) * 128, :]), writes=[txt])
                k.dma("sp", lambda e: e.dma_start(out=yab[b][:], in_=D["yatt_scr"][j * 128:(j + 1) * 128, :]), writes=[tok("yab%d" % b)])

                def fm_group(bank, chunk0, nch):
                    bv = bank[:, :].rearrange("p (a b) -> p a b", a=4)
                    for cc in range(nch):
                        for c in range(8):
                            k.op("pe", lambda e: e.matmul(bv[:, cc, :], winB[:, c, (chunk0 + cc) * 128:(chunk0 + cc + 1) * 128],
                                                          xTa[b][:, c, :], start=(c == 0), stop=(c == 7)),
                                 reads=[tok("winB"), txa], writes=[nm(bank)])
                    return bv
                bv = fm_group(Rb[0], 0, 4)
                tqr = tok("qkraw")
                k.op("act", lambda e: e.copy(out=qkraw[:, :, 3:131], in_=bv[:, :, :]), reads=[nm(Rb[0])], writes=[tqr])
                for g_, (bk, dst) in enumerate(((Rb[1], sga), (Rb[2], sga), (Rb[3], sgm), (Rb[4], sgm))):
                    bv = fm_group(bk, 4 + 4 * g_, 4)
                    k.op("act", lambda e: e.activation(out=dst[:, (g_ % 2) * 4:(g_ % 2) * 4 + 4, :], in_=bv[:, :, :], func=AF.Sigmoid),
                         reads=[nm(bk)], writes=[tok("sg%d" % g_)])
                for (bk, c0, c1) in ((Rb[5], B_VM, B_O), (Rb[6], B_O, B_IF)):
                    for c in range(8):
                        k.op("pe", lambda e: e.matmul(bk[:, 0:c1 - c0], xTa[b][:, c, :], winB[:, c, c0:c1], start=(c == 0), stop=(c == 7)),
                             reads=[tok("winB"), txa], writes=[nm(bk)])
                    if bk is Rb[5]:
                        k.op("dve", lambda e: e.tensor_copy(out=vm[:, :, 0:128], in_=bk[:, :].rearrange("p (a b) -> p a b", a=4)),
                             reads=[nm(bk)], writes=[tok("vm")])
                    else:
                        k.op("act", lambda e: e.activation(out=so[:], in_=bk[:, :], func=AF.Sigmoid), reads=[nm(bk)], writes=[tok("so")])
                k.op("dve", lambda e: e.tensor_tensor(out=so[:], in0=so[:], in1=ngb[:], op=ALU.mult), reads=[tok("so"), tc_], writes=[tok("so")])
                for c in range(8):
                    k.op("pe", lambda e: e.matmul(Rb[6][:, 0:8], xTa[b][:, c, :], winB[:, c, B_IF:B_COLS], start=(c == 0), stop=(c == 7)),
                         reads=[tok("winB"), txa], writes=[nm(Rb[6])])
                tg = tok("gat")
                k.op("dve", lambda e: e.tensor_tensor(out=gat[:, 0:8], in0=Rb[6][:, 0:8], in1=ifb[:], op=ALU.add), reads=[nm(Rb[6]), tc_], writes=[tg])
                k.op("act", lambda e: e.activation(out=gat[:, 28:32], in_=gat[:, 4:8], func=AF.Exp, scale=-1.0), reads=[tg], writes=[tg])
                k.op("act", lambda e: e.activation(out=gat[:, 8:12], in_=gat[:, 28:32], func=AF.Ln, bias=1.0, scale=1.0), reads=[tg], writes=[tg])
                k.op("pe", lambda e: e.matmul(Rb[6][:, 16:20], triI[:], gat[:, 8:12], start=True, stop=True), reads=[tg, tc_], writes=[nm(Rb[6])])
                k.op("pe", lambda e: e.matmul(Rb[6][:, 24:28], onesf[:], gat[:, 8:12], start=True, stop=True), reads=[tg, tc_], writes=[nm(Rb[6])])
                k.op("act", lambda e: e.activation(out=gat[:, 16:20], in_=Rb[6][:, 16:20], func=AF.Exp, scale=-1.0), reads=[nm(Rb[6])], writes=[tg])
                k.op("act", lambda e: e.activation(out=gat[:, 24:28], in_=Rb[6][:, 24:28], func=AF.Exp, scale=-1.0), reads=[nm(Rb[6])], writes=[tg])
                k.op("dve", lambda e: e.tensor_tensor(out=gat[:, 12:16], in0=Rb[6][:, 16:20], in1=gat[:, 0:4], op=ALU.add), reads=[nm(Rb[6]), tg], writes=[tg])
                k.op("act", lambda e: e.activation(out=gat[:, 20:24], in_=gat[:, 12:16], func=AF.Exp, bias=float(np.log(0.125)), scale=1.0),
                     reads=[tg], writes=[tg])
                tca = tok("cacc")
                for cc in range(4):
                    k.op("dve", lambda e: e.tensor_scalar(out=cacc[:, cc, :], in0=qkraw[:, cc, 3:131], scalar1=cw[:, cc, 3:4], scalar2=cbias[:, cc:cc + 1],
                                                          op0=ALU.mult, op1=ALU.add), reads=[tqr, tc_], writes=[tca])
                    for tap in range(3):
                        k.op("dve", lambda e: e.scalar_tensor_tensor(out=cacc[:, cc, :], in0=qkraw[:, cc, tap:tap + 128], scalar=cw[:, cc, tap:tap + 1],
                                                                     in1=cacc[:, cc, :], op0=ALU.mult, op1=ALU.add), reads=[tqr, tc_, tca], writes=[tca])
                k.op("dve", lambda e: e.tensor_copy(out=qkraw[:, :, 0:3], in_=qkraw[:, :, 128:131]), reads=[tqr], writes=[tqr])
                tq = tok("qkm")
                k.op("act", lambda e: e.activation(out=qkm[:, 2:4, :], in_=cacc[:, 2:4, :], func=AF.Silu), reads=[tca], writes=[tq])
                k.op("act", lambda e: e.activation(out=qz[0:64, 0:4:2, :], in_=cacc[0:64, 0:2, :], func=AF.Silu), reads=[tca], writes=[tq])
                k.op("act", lambda e: e.activation(out=qz[64:128, 1:4:2, :], in_=cacc[64:128, 0:2, :], func=AF.Silu), reads=[tca], writes=[tq])
                sv = Rb[0][:, :].rearrange("p (a b) -> p a b", a=4)
                for h in range(4):
                    hp, hc = (h % 2) * 64, h // 2
                    k.op("pe", lambda e: e.matmul(sv[:, h, :], qkm[:, 2 + hc, :], qz[:, h, :], start=True, stop=True),
                         reads=[tq], writes=[nm(Rb[0])])
                for h in range(4):
                    k.op("dve", lambda e: e.scalar_tensor_tensor(out=PTm[:, h, :], in0=sv[:, h, :], scalar=gat[:, 20 + h:21 + h], in1=causT[:],
                                                                 op0=ALU.mult, op1=ALU.mult), reads=[nm(Rb[0]), tg, tc_], writes=[tok("PTm")])
                for h in range(4):
                    hp, hc = (h % 2) * 64, h // 2
                    bk = Rb[1 + h // 2]
                    ov = bk[:, 0:258].rearrange("p (a b) -> p a b", a=2)
                    k.op("pe", lambda e: e.matmul(ov[:, h % 2, :], PTm[:, h, :], vm[:, h, 0:129], start=True, stop=False),
                         reads=[tok("PTm"), tok("vm")], writes=[nm(bk)])
                    k.op("pe", lambda e: e.matmul(ov[:, h % 2, :], qz[:, h, :], Cb[:, hc, 0:129], start=False, stop=True),
                         reads=[tq, tok("Cb")], writes=[nm(bk)])
                kv = psTb[:, 0:256].rearrange("p (a b) -> p a b", a=2)
                for hc in range(2):
                    k.op("pe", lambda e: e.transpose(kv[:, hc, :], qkm[:, 2 + hc, :], identb[:]), reads=[tq, tc_], writes=[nm(psTb)])
                for h in range(4):
                    hq, hc = (h % 2) * 64, h // 2
                    k.op("dve", lambda e: e.tensor_scalar(out=kp2[:, hc, hq:hq + 64], in0=kv[:, hc, hq:hq + 64], scalar1=gat[:, 20 + h:21 + h],
                                                          scalar2=None, op0=ALU.mult), reads=[nm(psTb), tg], writes=[tok("kp2")])
                for h in range(4):
                    hp, hc = (h % 2) * 64, h // 2
                    bk = Rb[3 + h // 2]
                    cv = bk[:, 0:258].rearrange("p (a b) -> p a b", a=2)
                    k.op("pe", lambda e: e.matmul(cv[:, h % 2, :], kp2[:, hc, :], vm[:, h, 0:129], start=True, stop=True),
                         reads=[tok("kp2"), tok("vm")], writes=[nm(bk)])
                    k.op("dve", lambda e: e.tensor_tensor(out=ctmp[hp:hp + 64, :], in0=cv[hp:hp + 64, h % 2, :], in1=Cst[hp:hp + 64, hc, :], op=ALU.add),
                         reads=[nm(bk), tok("Cst")], writes=[tok("ctmp")])
                    k.op("dve", lambda e: e.tensor_scalar(out=Cst[hp:hp + 64, hc, :], in0=ctmp[hp:hp + 64, :], scalar1=gat[hp:hp + 64, 24 + h:25 + h],
                                                          scalar2=None, op0=ALU.mult), reads=[tok("ctmp"), tg], writes=[tok("Cst")])
                    k.op("act", lambda e: e.copy(out=Cb[hp:hp + 64, hc, 0:129], in_=Cst[hp:hp + 64, hc, :]), reads=[tok("Cst")], writes=[tok("Cb")])
                ths, thv = tok("hs"), tok("hv")
                for h in range(4):
                    bk = Rb[1 + h // 2]
                    ov = bk[:, 0:258].rearrange("p (a b) -> p a b", a=2)
                    k.op("dve", lambda e: e.tensor_scalar(out=hs[:, h:h + 1], in0=ov[:, h % 2, 128:129], scalar1=gat[:, 16 + h:17 + h], scalar2=None,
                                                          op0=ALU.mult), reads=[nm(bk), tg], writes=[ths])
                k.op("act", lambda e: e.activation(out=hs[:, 0:4], in_=hs[:, 0:4], func=AF.Abs), reads=[ths], writes=[ths])
                k.op("dve", lambda e: e.tensor_scalar(out=hs[:, 0:4], in0=hs[:, 0:4], scalar1=1.0, scalar2=None, op0=ALU.max), reads=[ths], writes=[ths])
                k.op("dve", lambda e: e.reciprocal(out=hs[:, 4:8], in_=hs[:, 0:4]), reads=[ths], writes=[ths])
                k.op("dve", lambda e: e.tensor_tensor(out=hs[:, 4:8], in0=hs[:, 4:8], in1=gat[:, 16:20], op=ALU.mult), reads=[ths, tg], writes=[ths])
                for h in range(4):
                    bk = Rb[1 + h // 2]
                    ov = bk[:, 0:258].rearrange("p (a b) -> p a b", a=2)
                    k.op("act", lambda e: e.activation(out=hv[:, h, :], in_=ov[:, h % 2, 0:128], func=AF.Copy, scale=hs[:, 4 + h:5 + h]),
                         reads=[nm(bk), ths], writes=[thv])
                k.op("dve", lambda e: e.tensor_reduce(out=hs[:, 8:12], in_=hv[:], axis=AX.X, op=ALU.add), reads=[thv], writes=[ths])
                k.op("dve", lambda e: e.tensor_scalar(out=hs[:, 8:12], in0=hs[:, 8:12], scalar1=-1.0 / 128, scalar2=None, op0=ALU.mult), reads=[ths], writes=[ths])
                for h in range(4):
                    k.op("act", lambda e: e.activation(out=hv[:, h, :], in_=hv[:, h, :], func=AF.Identity, bias=hs[:, 8 + h:9 + h], scale=1.0),
                         reads=[ths, thv], writes=[thv])
                k.op("act", lambda e: e.activation(out=sq[:, 0:512], in_=hv[:].rearrange("p a b -> p (a b)"), func=AF.Square), reads=[thv], writes=[tok("sq")])
                k.op("dve", lambda e: e.tensor_reduce(out=hs[:, 12:16], in_=sq[:, 0:512].rearrange("p (a b) -> p a b", a=4), axis=AX.X, op=ALU.add),
                     reads=[tok("sq")], writes=[ths])
                k.op("dve", lambda e: e.tensor_scalar(out=hs[:, 12:16], in0=hs[:, 12:16], scalar1=1.0 / 128, scalar2=LN_EPS, op0=ALU.mult, op1=ALU.add),
                     reads=[ths], writes=[ths])
                k.op("act", lambda e: e.activation(out=hs[:, 16:20], in_=hs[:, 12:16], func=AF.Sqrt), reads=[ths], writes=[ths])
                k.op("dve", lambda e: e.reciprocal(out=hs[:, 16:20], in_=hs[:, 16:20]), reads=[ths], writes=[ths])
                tym = tok("ym")
                for h in range(4):
                    k.op("dve", lambda e: e.scalar_tensor_tensor(out=ym[:, h * 128:(h + 1) * 128], in0=hv[:, h, :], scalar=hs[:, 16 + h:17 + h],
                                                                 in1=so[:, h * 128:(h + 1) * 128], op0=ALU.mult, op1=ALU.mult),
                         reads=[thv, ths, tok("so")], writes=[tym])
                if dbg is not None:
                    k.dma("sp", lambda e: e.dma_start(out=dbg["ym"][j * 128:(j + 1) * 128, :], in_=ym[:]), reads=[tym], writes=[tok("dbg")])
                tv = psTb[:, :].rearrange("p (a b) -> p a b", a=8)
                for c in range(4):
                    k.op("pe", lambda e: e.transpose(tv[:, c, :], yab[b][:, c * 128:(c + 1) * 128], identb[:]), reads=[tok("yab%d" % b), tc_], writes=[nm(psTb)])
                k.op("act", lambda e: e.copy(out=yaT[:], in_=tv[:, 0:4, :]), reads=[nm(psTb)], writes=[tok("yaT")])
                for c in range(4):
                    k.op("pe", lambda e: e.transpose(tv[:, 4 + c, :], ym[:, c * 128:(c + 1) * 128], identb[:]), reads=[tym, tc_], writes=[nm(psTb)])
                k.op("act", lambda e: e.copy(out=ymT[:], in_=tv[:, 4:8, :]), reads=[nm(psTb)], writes=[tok("ymT")])
                for half in range(2):
                    for (bk, w_, y_, tn) in ((Rb[half], wba, yaT, "yaT"), (Rb[2 + half], wbm, ymT, "ymT")):
                        mv = bk[:, :].rearrange("p (a b) -> p a b", a=4)
                        for c4 in range(4):
                            c8 = half * 4 + c4
                            for kc in range(4):
                                k.op("pe", lambda e: e.matmul(mv[:, c4, :], w_[:, kc, c8 * 128:(c8 + 1) * 128], y_[:, kc, :], start=(kc == 0), stop=(kc == 3)),
                                     reads=[tc_, tok(tn)], writes=[nm(bk)])
                    k.op("dve", lambda e: e.tensor_tensor(out=t1[:], in0=sga[:, half * 4:half * 4 + 4, :].rearrange("p a b -> p (a b)"), in1=Rb[half][:, :], op=ALU.mult),
                         reads=[tok("sg0"), tok("sg1"), nm(Rb[half])], writes=[tok("t1")])
                    k.op("dve", lambda e: e.tensor_tensor(out=t2[:], in0=sgm[:, half * 4:half * 4 + 4, :].rearrange("p a b -> p (a b)"), in1=Rb[2 + half][:, :], op=ALU.mult),
                         reads=[tok("sg2"), tok("sg3"), nm(Rb[2 + half])], writes=[tok("t2")])
                    k.op("dve", lambda e: e.tensor_tensor(out=mixT[:, half * 4:half * 4 + 4, :].rearrange("p a b -> p (a b)"), in0=t1[:], in1=t2[:], op=ALU.add),
                         reads=[tok("t1"), tok("t2")], writes=[tok("mixT")])
                tac = tok("bacc%d" % b)
                for half in range(2):
                    bk = Rb[4 + half]
                    for c8 in range(8):
                        k.op("pe", lambda e: e.matmul(bk[:, :], mixT[:, c8, :], wout[:, c8, half * 512:(half + 1) * 512], start=(c8 == 0), stop=(c8 == 7)),
                             reads=[tok("mixT"), tc_], writes=[nm(bk)])
                    k.op("dve", lambda e: e.scalar_tensor_tensor(out=acc[b][:, half * 512:(half + 1) * 512], in0=xt[b][:, half * 512:(half + 1) * 512],
                                                                 scalar=float(ALPHA), in1=bk[:, :], op0=ALU.mult, op1=ALU.add),
                         reads=[txt, nm(bk)], writes=[tac])
                layer_norm_tile(k, acc[b], tac, sq, tok("sq"), st, j, tok("bst%d" % j), l1g, l1b, tc_)
                k.dma("sp", lambda e: e.dma_start(out=D["x1"][j * 128:(j + 1) * 128, :], in_=acc[b][:]), reads=[tac], writes=[Tok()])
            k.barrier()


def t5_bucket_np(n):
    n = np.maximum(n, 0)
    nf = np.maximum(n, 1).astype(np.float32)
    large = 16 + (np.log(nf / np.float32(16)) / np.float32(np.log(128 / 16)) * np.float32(16)).astype(np.int32)
    large = np.minimum(large, 31)
    return np.where(n < 16, n, large)


def mixer_consts():
    c = {}
    kk = np.arange(128)
    c["identb"] = np.eye(128, dtype=np.float32).astype(ml_dtypes.bfloat16)
    c["causT"] = (kk[:, None] <= kk[None, :]).astype(ml_dtypes.bfloat16)
    c["ones"] = np.ones((128, 128), dtype=ml_dtypes.bfloat16)
    c["negmask"] = np.where(kk[None, :] <= kk[:, None], 0.0, -1e30).astype(np.float32)
    c["triI"] = (kk[:, None] <= kk[None, :]).astype(np.float32)
    c["onesf"] = np.ones((128, 128), dtype=np.float32)
    ft = np.array([0.5] + [2.0 ** -it for it in range(1, NIT + 1)], dtype=np.float32)
    c["ftab"] = np.tile(ft[None, :], (128, 1))
    return c


MIX_CONST_DT = {"ftab": F32, "identb": BF16, "causT": BF16, "ones": BF16, "negmask": F32, "triI": F32, "onesf": F32}

MIX_IN = {"xT": [DM, T], "x": [T, DM], "winA": [DM, A_COLS], "winB": [DM, B_COLS], "wba": [512, DM], "wbm": [512, DM],
          "wout": [DM, DM], "cw": [128, 4, 4], "cb": [128, 4], "ifb": [128, 8], "ng": [128, 512], "ln1_g": [128, DM],
          "ln1_b": [128, DM], "ebd": [128, 8, 128], "ebp": [128, 8, 128], "cfar": [128, 8]}


def declare_mixer_inputs(nc, D):
    for n, shp in MIX_IN.items():
        D[n] = nc.dram_tensor(n, shp, F32, kind="ExternalInput").ap()
    mc = mixer_consts()
    D.setdefault("consts", {})
    for n, a in mc.items():
        if n not in D["consts"]:
            D["consts"][n] = nc.dram_tensor("c_" + n, list(a.shape), MIX_CONST_DT[n], kind="ExternalInput").ap()


def mixer_host_shared(w_in, conv_w, conv_b, mlstm_i_bias, mlstm_f_bias, mlstm_norm_g, w_branch_attn, w_branch_mlstm,
                      w_out, ln1_g, ln1_b, rel_bias):
    m = {}
    for n, a in mixer_consts().items():
        m["c_" + n] = a
    w = w_in[0]
    sp = np.cumsum([0, 512, 512, 512, 512, 64, 8, 256, 256, 512, 4, 4, 512, 1024, 1024])
    seg = {n: w[:, sp[i]:sp[i + 1]] for i, n in enumerate(
        ["q_a", "k_a", "v_a", "q_i", "k_i", "w_i", "q_m", "k_m", "v_m", "i_m", "f_m", "o_m", "g_a", "g_m"])}
    m["winA"] = np.ascontiguousarray(np.concatenate([seg["q_a"], seg["k_a"], seg["q_i"], seg["k_i"], seg["k_i"], seg["v_a"], seg["w_i"]], axis=1))
    m["winB"] = np.ascontiguousarray(np.concatenate([seg["q_m"], seg["k_m"], seg["g_a"], seg["g_m"], seg["v_m"], seg["o_m"], seg["i_m"], seg["f_m"]], axis=1))
    m["wba"] = np.ascontiguousarray(w_branch_attn[0])
    m["wbm"] = np.ascontiguousarray(w_branch_mlstm[0])
    m["wout"] = np.ascontiguousarray(w_out[0])
    m["cw"] = np.ascontiguousarray(conv_w[0].reshape(4, 4, 128).transpose(2, 1, 0))
    m["cb"] = np.ascontiguousarray(conv_b[0].reshape(4, 128).T)
    m["ifb"] = np.ascontiguousarray(np.broadcast_to(np.concatenate([mlstm_i_bias[0], mlstm_f_bias[0]])[None, :], (128, 8)))
    m["ng"] = np.ascontiguousarray(np.broadcast_to(mlstm_norm_g[0][None, :], (128, 512)))
    m["ln1_g"] = np.ascontiguousarray(np.broadcast_to(ln1_g[0][None, :], (128, DM)))
    m["ln1_b"] = np.ascontiguousarray(np.broadcast_to(ln1_b[0][None, :], (128, DM)))
    kk = np.arange(128)
    dd = kk[None, :] - kk[:, None]
    bd = t5_bucket_np(dd)
    bp = t5_bucket_np(dd + 128)
    m["ebd"] = np.ascontiguousarray(rel_bias[:, bd].transpose(1, 0, 2))
    m["ebp"] = np.ascontiguousarray(rel_bias[:, bp].transpose(1, 0, 2))
    m["cfar"] = np.ascontiguousarray(np.broadcast_to(rel_bias[:, 31][None, :], (128, 8)))
    return m


def build_mixer_only():
    nc = bass.Bass("TRN2", target_bir_lowering=False)
    D = {}
    declare_mixer_inputs(nc, D)
    D["x1"] = nc.dram_tensor("x1", [T, DM], F32, kind="ExternalOutput").ap()
    dbg = {"yatt": nc.dram_tensor("dbg_yatt", [T, 512], BF16, kind="ExternalOutput").ap(),
           "ym": nc.dram_tensor("dbg_ym", [T, 512], BF16, kind="ExternalOutput").ap()}
    D["yatt_scr"] = dbg["yatt"]
    with ExitStack() as es:
        k = K(nc, es)
        mixer_phase(nc, k, D, dbg)
        k.finish("sp")
    return nc


def host_consts():
    c = {}
    c["identf"] = np.eye(128, dtype=np.float32)
    c["iota"] = np.tile(np.arange(CAP, dtype=np.float32)[None, :], (128, 1))
    rb = (np.arange(E, dtype=np.float32)[None, :] * (NG * CAP) + (np.arange(NT, dtype=np.float32)[:, None] // 8) * CAP + 1.0)
    c["rowbase"] = np.ascontiguousarray(np.broadcast_to(rb[None, :, :], (128, NT, E))).astype(np.float32)
    kk = np.arange(128)
    c["tri"] = (kk[:, None] < kk[None, :]).astype(ml_dtypes.bfloat16)
    c["ones"] = np.ones((128, 128), dtype=ml_dtypes.bfloat16)
    return c


CONST_DT = {"identf": F32, "iota": F32, "rowbase": F32, "tri": BF16, "ones": BF16}


def build_moe_only():
    nc = bass.Bass("TRN2", target_bir_lowering=False)
    D = {}
    D["x1"] = nc.dram_tensor("x1", [T, DM], F32, kind="ExternalInput").ap()
    D["out"] = nc.dram_tensor("out", [T, DM], F32, kind="ExternalOutput").ap()
    D["yscr"] = nc.dram_tensor("yscr", [E * NG * CAP, DM], F32, kind="Internal").ap()
    declare_moe_inputs(nc, D)
    es = ExitStack()
    with es:
        k = K(nc, es)
        moe_phase(nc, k, D)
        k.finish("sp")
    return nc


def declare_moe_inputs(nc, D):
    hc = host_consts()
    D["consts"] = {n: nc.dram_tensor("c_" + n, list(a.shape), CONST_DT[n], kind="ExternalInput").ap() for n, a in hc.items()}
    D["w_router"] = nc.dram_tensor("w_router", [128, 8, E], F32, kind="ExternalInput").ap()
    D["b_router"] = nc.dram_tensor("b_router", [128, E], F32, kind="ExternalInput").ap()
    D["b_gate_up"] = nc.dram_tensor("b_gate_up", [128, E, 16], F32, kind="ExternalInput").ap()
    D["ln2_g"] = nc.dram_tensor("ln2_g", [128, DM], F32, kind="ExternalInput").ap()
    D["ln2_b"] = nc.dram_tensor("ln2_b", [128, DM], F32, kind="ExternalInput").ap()
    D["w_gate_up"] = nc.dram_tensor("w_gate_up", [E, DM, 2048], F32, kind="ExternalInput").ap()
    D["w_down"] = nc.dram_tensor("w_down", [E, DM, DM], F32, kind="ExternalInput").ap()
    D["b_down"] = nc.dram_tensor("b_down", [E, DM], F32, kind="ExternalInput").ap()


def moe_host_inputs(w_router, b_router, w_gate_up, b_gate_up, w_down, b_down, ln2_g, ln2_b):
    m = {}
    for n, a in host_consts().items():
        m["c_" + n] = a
    m["w_router"] = np.ascontiguousarray(w_router[0].reshape(8, 128, E).transpose(1, 0, 2))
    m["b_router"] = np.ascontiguousarray(np.broadcast_to(b_router[0][None, :], (128, E)))
    m["b_gate_up"] = np.ascontiguousarray(b_gate_up[0].reshape(E, 16, 128).transpose(2, 0, 1))
    m["ln2_g"] = np.ascontiguousarray(np.broadcast_to(ln2_g[0][None, :], (128, DM)))
    m["ln2_b"] = np.ascontiguousarray(np.broadcast_to(ln2_b[0][None, :], (128, DM)))
    m["w_gate_up"] = np.ascontiguousarray(w_gate_up[0])
    m["w_down"] = np.ascontiguousarray(w_down[0])
    m["b_down"] = np.ascontiguousarray(b_down[0])
    return m


def build_full():
    nc = bass.Bass("TRN2", target_bir_lowering=False)
    D = {}
    declare_mixer_inputs(nc, D)
    hc = host_consts()
    for n, a in hc.items():
        if n not in D["consts"]:
            D["consts"][n] = nc.dram_tensor("c_" + n, list(a.shape), CONST_DT[n], kind="ExternalInput").ap()
    D["w_router"] = nc.dram_tensor("w_router", [128, 8, E], F32, kind="ExternalInput").ap()
    D["b_router"] = nc.dram_tensor("b_router", [128, E], F32, kind="ExternalInput").ap()
    D["b_gate_up"] = nc.dram_tensor("b_gate_up", [128, E, 16], F32, kind="ExternalInput").ap()
    D["ln2_g"] = nc.dram_tensor("ln2_g", [128, DM], F32, kind="ExternalInput").ap()
    D["ln2_b"] = nc.dram_tensor("ln2_b", [128, DM], F32, kind="ExternalInput").ap()
    D["w_gate_up"] = nc.dram_tensor("w_gate_up", [E, DM, 2048], F32, kind="ExternalInput").ap()
    D["w_down"] = nc.dram_tensor("w_down", [E, DM, DM], F32, kind="ExternalInput").ap()
    D["b_down"] = nc.dram_tensor("b_down", [E, DM], F32, kind="ExternalInput").ap()
    D["x1"] = nc.dram_tensor("x1", [T, DM], F32, kind="Internal").ap()
    D["yscr"] = nc.dram_tensor("yscr", [E * NG * CAP, DM], F32, kind="Internal").ap()
    D["yatt_scr"] = nc.dram_tensor("yatt_scr", [T, 512], BF16, kind="Internal").ap()
    D["out"] = nc.dram_tensor("out", [T, DM], F32, kind="ExternalOutput").ap()
    with ExitStack() as es:
        k = K(nc, es)
        mixer_phase(nc, k, D, None)
        moe_phase(nc, k, D)
        k.finish("sp")
    return nc


def kernel(x, w_in, conv_w, conv_b, mlstm_i_bias, mlstm_f_bias, mlstm_norm_g, w_branch_attn, w_branch_mlstm, w_out,
           ln1_g, ln1_b, w_router, b_router, w_gate_up, b_gate_up, w_down, b_down, ln2_g, ln2_b, rel_bias):
    f = lambda a: np.asarray(a, dtype=np.float32)
    x = f(x)
    shared = mixer_host_shared(f(w_in), f(conv_w), f(conv_b), f(mlstm_i_bias), f(mlstm_f_bias), f(mlstm_norm_g),
                               f(w_branch_attn), f(w_branch_mlstm), f(w_out), f(ln1_g), f(ln1_b), f(rel_bias))
    shared.update(moe_host_inputs(f(w_router), f(b_router), f(w_gate_up), f(b_gate_up), f(w_down), f(b_down), f(ln2_g), f(ln2_b)))
    nc = build_full()
    in_maps = []
    for c in range(NCORES):
        m = dict(shared)
        m["x"] = np.ascontiguousarray(x[c])
        m["xT"] = np.ascontiguousarray(x[c].T)
        in_maps.append(m)
    res = run_bass_kernel_spmd(nc, in_maps, core_ids=list(range(NCORES)))
    return np.stack([np.asarray(r["out"], dtype=np.float32) for r in res.results], axis=0)
```
